# Optimizing a Trainium2 kernel written in Bass

```python
import math
import jax
import jax.numpy as jnp
from jax import lax
import numpy as np


D_MODEL = 2048
BATCH = 1
SEQ = 8192
DEPTH = 2

GRID_W = 64
CTX_LEN = 256
EPS = 1e-6

DA_HEADS = 8
DA_QK_DIM = 64
DA_V_DIM = 2 * DA_QK_DIM
DA_WIDTH = DA_HEADS * DA_V_DIM
DA_SCALE = DA_QK_DIM ** -0.5
ROPE_BASE = 10000.0
Q_BLOCK = 128

HY_WIDTH = 1024
HY_ORDER = 2
HY_SHORT = 3
HY_EMB = 33
HY_BANDS = (HY_EMB - 1) // 2
HY_FFN = 64
HY_MIN_DECAY = math.log(1e-2) / 1.5
HY_MAX_DECAY = math.log(1e-2) / 0.3

CV_WIDTH = 1024
CV_KERNEL = 31

N_BRANCH = 3

N_EXPERTS = 32
TOP_K = 4
D_EXPERT = 1024
SWIGLU_LIMIT = 7.0
SWIGLU_ALPHA = 1.702
MOE_BLOCK = 128

Q_COLS = 2 * DA_HEADS * DA_QK_DIM
K_COLS = Q_COLS
V_COLS = DA_WIDTH
HY_COLS = (HY_ORDER + 1) * HY_WIDTH
CV_COLS = 2 * CV_WIDTH
GATE_COLS = N_BRANCH * D_MODEL
OFF_Q = 0
OFF_K = OFF_Q + Q_COLS
OFF_V = OFF_K + K_COLS
OFF_HY = OFF_V + V_COLS
OFF_CV = OFF_HY + HY_COLS
OFF_GATE = OFF_CV + CV_COLS
IN_COLS = OFF_GATE + GATE_COLS

kernel_name = 'hybrid_diffattn_hyena_conformer_moe_dit'


def rmsnorm(x, g):
    xf = x.astype(jnp.float32)
    y = xf * lax.rsqrt(jnp.mean(xf * xf, axis=-1, keepdims=True) + EPS)
    return (y * g.astype(jnp.float32)).astype(x.dtype)


def layernorm(x, g, b):
    xf = x.astype(jnp.float32)
    mu = jnp.mean(xf, axis=-1, keepdims=True)
    var = jnp.mean(jnp.square(xf - mu), axis=-1, keepdims=True)
    y = (xf - mu) * lax.rsqrt(var + EPS)
    return (y * g.astype(jnp.float32) + b.astype(jnp.float32)).astype(x.dtype)


def depthwise_conv(x, w, b):
    k = w.shape[0]
    y = lax.conv_general_dilated(x, w[:, None, :].astype(x.dtype), (1,), [((k - 1) // 2, k // 2)],
                                 dimension_numbers=('NWC', 'WIO', 'NWC'),
                                 feature_group_count=x.shape[-1])
    return y + b.astype(x.dtype)


def axial_rope_tables(n_lat):
    rows = n_lat // GRID_W
    row = jnp.repeat(jnp.arange(rows, dtype=jnp.float32), GRID_W)
    col = jnp.tile(jnp.arange(GRID_W, dtype=jnp.float32), rows)
    half = DA_QK_DIM // 2
    inv = ROPE_BASE ** (-jnp.arange(0, half, 2, dtype=jnp.float32) / half)
    ar = row[:, None] * inv
    ac = col[:, None] * inv
    return (jnp.cos(ar), jnp.sin(ar), jnp.cos(ac), jnp.sin(ac))


def rope_axis(x, cos, sin):
    x1, x2 = jnp.split(x, 2, axis=-1)
    cos = cos[None, :, None, :]
    sin = sin[None, :, None, :]
    return jnp.concatenate([x1 * cos - x2 * sin, x2 * cos + x1 * sin], axis=-1)


def apply_axial_rope(x, tabs):
    xf = x.astype(jnp.float32)
    half = DA_QK_DIM // 2
    xr = rope_axis(xf[..., :half], tabs[0], tabs[1])
    xc = rope_axis(xf[..., half:], tabs[2], tabs[3])
    return jnp.concatenate([xr, xc], axis=-1).astype(x.dtype)


def diff_attend(q, k, v, lam):
    s = jnp.einsum('bqhd,bkhd->bhqk', q, k, preferred_element_type=jnp.float32) * DA_SCALE
    pr = jax.nn.softmax(s, axis=-1)
    b, _, nq, nk = pr.shape
    pr = pr.reshape(b, DA_HEADS, 2, nq, nk)
    w = pr[:, :, 0] - lam * pr[:, :, 1]
    return jnp.einsum('bhqk,bkhd->bqhd', w.astype(v.dtype), v)


def diff_head_out(o, g, lam_init):
    o = rmsnorm(o, g) * (1.0 - lam_init)
    return o.reshape(o.shape[0], o.shape[1], DA_WIDTH)


def hyena_filters(n, p):
    f32 = jnp.float32
    t = jnp.linspace(0.0, 1.0, n, dtype=f32)[:, None]
    w = 2.0 * math.pi * jnp.arange(n, dtype=f32)[:, None] / n
    bands = jnp.linspace(1e-4, HY_BANDS - 1, HY_BANDS, dtype=f32)[None, :]
    z = jnp.concatenate([t, jnp.cos(bands * w), -jnp.sin(bands * w)], axis=-1)
    freq = p['hy_freq'].astype(f32)
    h = jnp.sin(freq * (z @ p['hy_w1'].astype(f32) + p['hy_b1'].astype(f32)))
    h = jnp.sin(freq * (h @ p['hy_w2'].astype(f32) + p['hy_b2'].astype(f32)))
    h = (h @ p['hy_w3'].astype(f32)).reshape(n, HY_ORDER, 2, HY_WIDTH)
    deltas = jnp.abs(jnp.linspace(HY_MIN_DECAY, HY_MAX_DECAY, HY_WIDTH, dtype=f32))
    h = h * jnp.exp(-t * deltas)[:, None, None, :]
    h = h / jnp.sum(jnp.abs(h), axis=(0, 2), keepdims=True)
    return jnp.concatenate([h[:, :, 0], h[::-1, :, 1]], axis=0)


def long_conv(z, kf, bias):
    n = z.shape[1]
    zf = jnp.fft.rfft(z.astype(jnp.float32), n=2 * n, axis=1)
    kff = jnp.fft.rfft(kf, n=2 * n, axis=0)
    y = jnp.fft.irfft(zf * kff[None], n=2 * n, axis=1)[:, :n]
    return (y + z.astype(jnp.float32) * bias.astype(jnp.float32)).astype(z.dtype)


def hyena_branch(u, p):
    u = depthwise_conv(u, p['hy_short_w'], p['hy_short_b'])
    x1, x2, v = jnp.split(u, 3, axis=-1)
    kf = hyena_filters(u.shape[1], p)
    z = x1 * long_conv(v, kf[:, 0], p['hy_bias'][0])
    z = x2 * long_conv(z, kf[:, 1], p['hy_bias'][1])
    return z


def conv_branch(u, p):
    a, g = jnp.split(u, 2, axis=-1)
    y = a * jax.nn.sigmoid(g)
    y = depthwise_conv(y, p['cv_dw_w'], p['cv_dw_b'])
    y = layernorm(y, p['cv_ln_g'], p['cv_ln_b'])
    return jax.nn.silu(y)


def merge_branches(proj, a, hy, cv, p):
    b, n, _ = proj.shape
    g = jax.nn.sigmoid(proj[..., OFF_GATE:] + p['b_gate']).reshape(b, n, N_BRANCH, D_MODEL)
    y = (g[:, :, 0] * (a @ p['w_da_out']) + g[:, :, 1] * (hy @ p['w_hy_out'])
         + g[:, :, 2] * (cv @ p['w_cv_out']))
    return y @ p['w_out']


def token_mixer(h_lat, h_ctx, p, layer_idx, need_ctx):
    b, s, _ = h_lat.shape
    lc = h_ctx.shape[1]
    lam_init = 0.8 - 0.6 * math.exp(-0.3 * layer_idx)
    lp = p['da_lambda'].astype(jnp.float32)
    lam = jnp.exp(jnp.sum(lp[0] * lp[1])) - jnp.exp(jnp.sum(lp[2] * lp[3])) + lam_init

    proj = h_lat @ p['w_in']
    tabs = axial_rope_tables(s)
    q = apply_axial_rope(proj[..., OFF_Q:OFF_K].reshape(b, s, 2 * DA_HEADS, DA_QK_DIM), tabs)
    k = apply_axial_rope(proj[..., OFF_K:OFF_V].reshape(b, s, 2 * DA_HEADS, DA_QK_DIM), tabs)
    v = proj[..., OFF_V:OFF_HY].reshape(b, s, DA_HEADS, DA_V_DIM)

    if need_ctx:
        proj_c = h_ctx @ p['w_in']
        kv_c = proj_c[..., OFF_K:OFF_HY]
    else:
        kv_c = h_ctx @ p['w_in'][:, OFF_K:OFF_HY]
    k_c = kv_c[..., :K_COLS].reshape(b, lc, 2 * DA_HEADS, DA_QK_DIM)
    v_c = kv_c[..., K_COLS:].reshape(b, lc, DA_HEADS, DA_V_DIM)

    k_all = jnp.concatenate([k_c, k], axis=1)
    v_all = jnp.concatenate([v_c, v], axis=1)
    n_blk = s // Q_BLOCK
    q_blocks = jnp.moveaxis(q.reshape(b, n_blk, Q_BLOCK, 2 * DA_HEADS, DA_QK_DIM), 1, 0)
    o = lax.map(lambda qb: diff_attend(qb, k_all, v_all, lam), q_blocks)
    o = jnp.moveaxis(o, 0, 1).reshape(b, s, DA_HEADS, DA_V_DIM)
    a_lat = diff_head_out(o, p['da_subln_g'], lam_init)
    y_lat = merge_branches(proj, a_lat, hyena_branch(proj[..., OFF_HY:OFF_CV], p),
                           conv_branch(proj[..., OFF_CV:OFF_GATE], p), p)
    if not need_ctx:
        return y_lat, None

    q_c = proj_c[..., OFF_Q:OFF_K].reshape(b, lc, 2 * DA_HEADS, DA_QK_DIM)
    a_ctx = diff_head_out(diff_attend(q_c, k_c, v_c, lam), p['da_subln_g'], lam_init)
    y_ctx = merge_branches(proj_c, a_ctx, hyena_branch(proj_c[..., OFF_HY:OFF_CV], p),
                           conv_branch(proj_c[..., OFF_CV:OFF_GATE], p), p)
    return y_lat, y_ctx


def moe(h, p):
    n, d = h.shape
    logits = (h @ p['w_router']).astype(jnp.float32) + p['b_router'].astype(jnp.float32)
    top_val, top_idx = lax.top_k(logits, TOP_K)
    gates = jax.nn.softmax(top_val, axis=-1)
    flat_e = top_idx.reshape(-1)
    order = jnp.argsort(flat_e)
    sorted_e = flat_e[order]
    tok = order // TOP_K
    w_sorted = gates.reshape(-1)[order]
    counts = jnp.bincount(flat_e, length=N_EXPERTS)
    padded = (counts + MOE_BLOCK - 1) // MOE_BLOCK * MOE_BLOCK
    pad_end = jnp.cumsum(padded)
    pad_start = pad_end - padded
    grp_start = jnp.cumsum(counts) - counts
    rank = jnp.arange(n * TOP_K) - grp_start[sorted_e]
    dest = pad_start[sorted_e] + rank
    n_blocks = -(-(n * TOP_K) // MOE_BLOCK) + N_EXPERTS
    cap = n_blocks * MOE_BLOCK
    row_tok = jnp.zeros((cap,), jnp.int32).at[dest].set(tok.astype(jnp.int32))
    row_w = jnp.zeros((cap,), jnp.float32).at[dest].set(w_sorted)
    block_e = jnp.minimum(jnp.searchsorted(pad_end, jnp.arange(n_blocks) * MOE_BLOCK, side='right'),
                          N_EXPERTS - 1)
    xs = h[row_tok].reshape(n_blocks, MOE_BLOCK, d)

    def expert_block(args):
        xb, e = args
        gu = xb @ p['w_gu'][e] + p['b_gu'][e]
        gate = jnp.minimum(gu[:, :D_EXPERT], SWIGLU_LIMIT)
        up = jnp.clip(gu[:, D_EXPERT:], -SWIGLU_LIMIT, SWIGLU_LIMIT)
        act = gate * jax.nn.sigmoid(SWIGLU_ALPHA * gate) * (up + 1.0)
        return act @ p['w_dn'][e] + p['b_dn'][e]

    ys = lax.map(expert_block, (xs, block_e)).reshape(cap, d)
    out = jnp.zeros((n, d), jnp.float32).at[row_tok].add(ys.astype(jnp.float32) * row_w[:, None])
    return out.astype(h.dtype)


def trunk_layer(x, ctx, c, c_ctx, p, layer_idx, need_ctx):
    b, s, d = x.shape
    mod = jax.nn.silu(c) @ p['w_ada'] + p['b_ada']
    mod_c = (jax.nn.silu(c_ctx) @ p['w_ada'] + p['b_ada'])[None]
    sh1, sc1, g1, sh2, sc2, g2 = [m[:, None, :] for m in jnp.split(mod, 6, axis=-1)]
    csh1, csc1, cg1, csh2, csc2, cg2 = [m[:, None, :] for m in jnp.split(mod_c, 6, axis=-1)]
    ng = p['norm_g']

    h_lat = rmsnorm(x, ng[0]) * (1.0 + sc1) + sh1
    h_ctx = rmsnorm(ctx, ng[0]) * (1.0 + csc1) + csh1
    m_lat, m_ctx = token_mixer(h_lat, h_ctx, p, layer_idx, need_ctx)
    x = x + g1 * rmsnorm(m_lat, ng[1])
    f_lat_in = rmsnorm(x, ng[2]) * (1.0 + sc2) + sh2
    if need_ctx:
        ctx = ctx + cg1 * rmsnorm(m_ctx, ng[1])
        f_ctx_in = rmsnorm(ctx, ng[2]) * (1.0 + csc2) + csh2
        tokens = jnp.concatenate([f_ctx_in.reshape(-1, d), f_lat_in.reshape(-1, d)], axis=0)
        f = moe(tokens, p)
        n_c = ctx.shape[0] * ctx.shape[1]
        f_ctx = f[:n_c].reshape(ctx.shape)
        f_lat = f[n_c:].reshape(x.shape)
        ctx = ctx + cg2 * rmsnorm(f_ctx, ng[3])
    else:
        f_lat = moe(f_lat_in.reshape(-1, d), p).reshape(x.shape)
    x = x + g2 * rmsnorm(f_lat, ng[3])
    return x, ctx


def setup_inputs(seed: int = 0) -> dict:
    key = jax.random.key(seed)
    ks = iter(jax.random.split(key, 40))
    f32 = jnp.float32

    def nrm(shape, scale):
        return scale * jax.random.normal(next(ks), shape, f32)

    d, nl = D_MODEL, DEPTH
    return {
        'x': nrm((BATCH, SEQ, d), 1.0),
        'c': nrm((BATCH, d), 1.0),
        'ctx': nrm((BATCH, CTX_LEN, d), 1.0),
        'c_ctx': nrm((d,), 1.0),
        'w_ada': nrm((nl, d, 6 * d), 0.5 * d ** -0.5),
        'b_ada': nrm((nl, 6 * d), 0.01),
        'norm_g': 1.0 + nrm((nl, 4, d), 0.01),
        'w_in': nrm((nl, d, IN_COLS), d ** -0.5),
        'b_gate': nrm((nl, GATE_COLS), 0.01),
        'da_lambda': nrm((nl, 4, DA_QK_DIM), 0.1),
        'da_subln_g': 1.0 + nrm((nl, DA_V_DIM), 0.01),
        'w_da_out': nrm((nl, DA_WIDTH, d), DA_WIDTH ** -0.5),
        'hy_short_w': nrm((nl, HY_SHORT, HY_COLS), HY_SHORT ** -0.5),
        'hy_short_b': nrm((nl, HY_COLS), 0.01),
        'hy_w1': nrm((nl, HY_EMB, HY_FFN), HY_EMB ** -0.5),
        'hy_b1': nrm((nl, HY_FFN), 0.01),
        'hy_w2': nrm((nl, HY_FFN, HY_FFN), HY_FFN ** -0.5),
        'hy_b2': nrm((nl, HY_FFN), 0.01),
        'hy_freq': 1.0 + nrm((nl, HY_FFN), 0.01),
        'hy_w3': nrm((nl, HY_FFN, HY_ORDER * 2 * HY_WIDTH), HY_FFN ** -0.5),
        'hy_bias': nrm((nl, HY_ORDER, HY_WIDTH), 0.5),
        'w_hy_out': nrm((nl, HY_WIDTH, d), HY_WIDTH ** -0.5),
        'cv_dw_w': nrm((nl, CV_KERNEL, CV_WIDTH), CV_KERNEL ** -0.5),
        'cv_dw_b': nrm((nl, CV_WIDTH), 0.01),
        'cv_ln_g': 1.0 + nrm((nl, CV_WIDTH), 0.01),
        'cv_ln_b': nrm((nl, CV_WIDTH), 0.01),
        'w_cv_out': nrm((nl, CV_WIDTH, d), CV_WIDTH ** -0.5),
        'w_out': nrm((nl, d, d), d ** -0.5),
        'w_router': nrm((nl, d, N_EXPERTS), d ** -0.5),
        'b_router': nrm((nl, N_EXPERTS), 0.01),
        'w_gu': nrm((nl, N_EXPERTS, d, 2 * D_EXPERT), d ** -0.5),
        'b_gu': nrm((nl, N_EXPERTS, 2 * D_EXPERT), 0.01),
        'w_dn': nrm((nl, N_EXPERTS, D_EXPERT, d), D_EXPERT ** -0.5),
        'b_dn': nrm((nl, N_EXPERTS, d), 0.01),
    }


def reference(x, c, ctx, c_ctx, w_ada, b_ada, norm_g, w_in, b_gate, da_lambda, da_subln_g, w_da_out,
              hy_short_w, hy_short_b, hy_w1, hy_b1, hy_w2, hy_b2, hy_freq, hy_w3, hy_bias, w_hy_out,
              cv_dw_w, cv_dw_b, cv_ln_g, cv_ln_b, w_cv_out, w_out, w_router, b_router,
              w_gu, b_gu, w_dn, b_dn):
    for l in range(DEPTH):
        p = {
            'w_ada': w_ada[l], 'b_ada': b_ada[l], 'norm_g': norm_g[l],
            'w_in': w_in[l], 'b_gate': b_gate[l],
            'da_lambda': da_lambda[l], 'da_subln_g': da_subln_g[l], 'w_da_out': w_da_out[l],
            'hy_short_w': hy_short_w[l], 'hy_short_b': hy_short_b[l],
            'hy_w1': hy_w1[l], 'hy_b1': hy_b1[l], 'hy_w2': hy_w2[l], 'hy_b2': hy_b2[l],
            'hy_freq': hy_freq[l], 'hy_w3': hy_w3[l], 'hy_bias': hy_bias[l], 'w_hy_out': w_hy_out[l],
            'cv_dw_w': cv_dw_w[l], 'cv_dw_b': cv_dw_b[l], 'cv_ln_g': cv_ln_g[l], 'cv_ln_b': cv_ln_b[l],
            'w_cv_out': w_cv_out[l], 'w_out': w_out[l],
            'w_router': w_router[l], 'b_router': b_router[l],
            'w_gu': w_gu[l], 'b_gu': b_gu[l], 'w_dn': w_dn[l], 'b_dn': b_dn[l],
        }
        x, ctx = trunk_layer(x, ctx, c, c_ctx, p, l, l < DEPTH - 1)
    return x
```

```python
import math
from contextlib import ExitStack
import numpy as np
import ml_dtypes
import concourse.bass as bass
import concourse.mybir as mybir
from concourse.bass_utils import run_bass_kernel_spmd

F32 = mybir.dt.float32
BF16 = mybir.dt.bfloat16
AF = mybir.ActivationFunctionType
ALU = mybir.AluOpType
AX = mybir.AxisListType

NCORES = 8
D = 2048
SEQ = 8192
LC = 256
DEPTH = 2
EPS = 1e-6
IN_COLS = 14336
OFF_Q, OFF_K, OFF_V, OFF_HY, OFF_CV, OFF_GATE = 0, 1024, 2048, 3072, 6144, 8192
NE = 32
DE = 1024


class Buf:
    __slots__ = ("name", "w", "r", "multi")

    def __init__(self, name="", multi=False):
        self.name = name
        self.w = []
        self.r = []
        self.multi = multi


class Sched:
    ENG = ("pe", "dve", "act", "pool", "sp")

    def __init__(self, nc, es, n_dma_sems=24):
        self.nc = nc
        self.es = es
        self.ops = []
        self.n_dma_sems = n_dma_sems
        self.last = {}
        self.dmas_since = []
        self.pending = {}

    def barrier(self):
        deps = set(self.last.values()) | set(self.dmas_since)
        self.dmas_since = []
        for e in self.ENG:
            self.pending[e] = set(deps) | self.pending.get(e, set())

    def _deps(self, reads, writes):
        deps = set()
        for b in reads:
            deps.update(b.w)
        for b in writes:
            if not (b.multi and not b.r):
                deps.update(b.w)
            deps.update(b.r)
        return deps

    def _record(self, idx, reads, writes):
        for b in writes:
            if b.multi and not b.r:
                b.w.append(idx)
            else:
                b.w = [idx]
            b.r = []
        for b in reads:
            b.r.append(idx)

    def op(self, eng, fn, reads=(), writes=()):
        deps = self._deps(reads, writes) | self.pending.pop(eng, set())
        idx = len(self.ops)
        self.last[eng] = idx
        self.ops.append(dict(eng=eng, fn=fn, deps=deps, kind="c"))
        self._record(idx, reads, writes)
        return idx

    def do(self, eng, reads, writes, method, *a, **kw):
        return self.op(eng, lambda e: getattr(e, method)(*a, **kw), reads=reads, writes=writes)

    def dma(self, q, out, in_, reads=(), writes=(), **kw):
        deps = self._deps(reads, writes) | self.pending.pop(q, set())
        idx = len(self.ops)
        self.last[q] = idx
        self.dmas_since.append(idx)
        self.ops.append(dict(eng=q, fn=lambda e: e.dma_start(out=out, in_=in_, **kw), deps=deps, kind="d"))
        self._record(idx, reads, writes)
        return idx

    def emit(self, final_waits=()):
        nc = self.nc
        engs = {"pe": nc.tensor, "dve": nc.vector, "act": nc.scalar, "pool": nc.gpsimd, "sp": nc.sync}
        ops = self.ops
        for i, o in enumerate(ops):
            best = {}
            red = set()
            for d in o["deps"]:
                po = ops[d]
                if po["kind"] == "c":
                    if po["eng"] == "pe" and o["eng"] == "pe" and o["kind"] == "c":
                        continue
                    if best.get(po["eng"], -1) < d:
                        best[po["eng"]] = d
                else:
                    red.add(d)
            red.update(best.values())
            o["deps"] = red
        needed = [False] * len(ops)
        for o in ops:
            for d in o["deps"]:
                if ops[d]["kind"] == "c":
                    needed[d] = True
        for d in final_waits:
            if ops[d]["kind"] == "c":
                needed[d] = True
        csem = {e: self.es.enter_context(nc.semaphore("cs_" + e)) for e in self.ENG}
        dsem = [self.es.enter_context(nc.semaphore("ds%d" % i)) for i in range(self.n_dma_sems)]
        ccnt = {e: 0 for e in self.ENG}
        dcnt = [0] * self.n_dma_sems
        ev = [None] * len(ops)
        known = {e: {} for e in self.ENG}
        nd = 0
        nwaits = 0

        def wait(e, key, val):
            nonlocal nwaits
            if known[e].get(key, 0) >= val:
                return
            sem = csem[key] if isinstance(key, str) else dsem[key]
            engs[e].wait_ge(sem, val)
            known[e][key] = val
            nwaits += 1

        for i, o in enumerate(ops):
            e = o["eng"]
            for d in sorted(o["deps"]):
                key, val = ev[d]
                wait(e, key, val)
            if o["kind"] == "c":
                ins = o["fn"](engs[e])
                if needed[i]:
                    ccnt[e] += 1
                    ins.then_inc(csem[e], 1)
                    ev[i] = (e, ccnt[e])
            else:
                k = nd % self.n_dma_sems
                nd += 1
                wait(e, k, dcnt[k])
                ins = o["fn"](engs[e])
                dcnt[k] += 16
                ins.then_inc(dsem[k], 16)
                ev[i] = (k, dcnt[k])
        for d in final_waits:
            key, val = ev[d]
            wait("sp", key, val)
        self.stats = dict(n_ops=len(ops), ccnt=dict(ccnt), nwaits=nwaits)


class T:
    def __init__(self, t, name):
        self.t = t
        self.b = Buf(name)

    def __getitem__(self, k):
        return self.t[k]


class Prog:
    def __init__(self, name="k"):
        self.nc = bass.Bass("TRN2", target_bir_lowering=False)
        self.es = ExitStack()
        self.S = Sched(self.nc, self.es)
        self.outs = []
        self.n = 0
        self.stack = [self.es]

    def push(self):
        self.stack.append(ExitStack())

    def pop(self):
        self.S.barrier()
        self.stack.pop().close()

    def din(self, name, shape, dt=F32):
        return self.nc.dram_tensor(name, list(shape), dt, kind="ExternalInput").ap()

    def dout(self, name, shape, dt=F32):
        return self.nc.dram_tensor(name, list(shape), dt, kind="ExternalOutput").ap()

    def dscratch(self, name, shape, dt=F32):
        return T(self.nc.dram_tensor(name, list(shape), dt, kind="Internal").ap(), name)

    def sb(self, name, shape, dt=F32):
        self.n += 1
        return T(self.stack[-1].enter_context(self.nc.sbuf_tensor("%s_%d" % (name, self.n), list(shape), dt)), name)

    def ps(self, name, shape=(128, 512), dt=F32):
        self.n += 1
        return T(self.stack[-1].enter_context(self.nc.psum_tensor("%s_%d" % (name, self.n), list(shape), dt)), name)

    def finish(self):
        self.S.emit(final_waits=self.outs)
        self.es.close()
        return self.nc


def run(prog_nc, in_maps):
    import time
    t0 = time.time()
    res = run_bass_kernel_spmd(prog_nc, in_maps, core_ids=list(range(NCORES)))
    print("[run] launch %.1fs" % (time.time() - t0), flush=True)
    return res.results


def fm(vec, nchunk):
    return np.ascontiguousarray(np.asarray(vec).reshape(nchunk, 128).T)


def build_k0():
    P = Prog()
    S = P.S
    ccT = P.din("ccT", [128, 32])
    w = P.din("w", [DEPTH, 128, 16, 1536])
    bT = P.din("bT", [128, DEPTH * 12])
    out = P.dout("modT", [128, DEPTH * 12 * 2])
    cc = P.sb("cc", [128, 32])
    sl = P.sb("sl", [128, 32])
    bt = P.sb("bt", [128, DEPTH * 12])
    res = P.sb("res", [128, DEPTH * 12 * 2])
    S.dma("sp", cc[:], ccT, writes=[cc.b])
    S.dma("sp", bt[:], bT, writes=[bt.b])
    S.op("act", lambda e: e.activation(out=sl[:], in_=cc[:], func=AF.Silu), reads=[cc.b], writes=[sl.b])
    wts = []
    for l in range(DEPTH):
        for kg in range(4):
            wt = P.sb("w", [128, 4, 1536])
            S.dma("sp", wt[:], w[l, :, kg * 4:(kg + 1) * 4, :], writes=[wt.b])
            wts.append(wt)
    pss = [P.ps("ps", [128, 2]) for _ in range(4)]
    n = 0
    for l in range(DEPTH):
        for j in range(12):
            ps = pss[n % 4]
            for k in range(16):
                wt = wts[l * 4 + k // 4]
                S.op("pe", lambda e, wt=wt, k=k, j=j, ps=ps: e.matmul(
                    ps[:], wt[:, k % 4, j * 128:(j + 1) * 128], sl[:, 2 * k:2 * k + 2],
                    start=(k == 0), stop=(k == 15)), reads=[wt.b, sl.b], writes=[ps.b])
            idx = l * 12 + j
            S.op("dve", lambda e, ps=ps, idx=idx: e.tensor_scalar(
                res[:, 2 * idx:2 * idx + 2], ps[:], bt[:, idx:idx + 1], None, ALU.add),
                reads=[ps.b, bt.b], writes=[res.b])
            n += 1
    P.outs.append(S.dma("sp", out, res[:], reads=[res.b]))
    return P.finish()


def run_k0(inp):
    cc = np.stack([inp["c"][0], inp["c_ctx"]], axis=-1)
    ccT = np.ascontiguousarray(cc.reshape(16, 128, 2).transpose(1, 0, 2).reshape(128, 32))
    in_maps = []
    for i in range(NCORES):
        w = inp["w_ada"][:, :, i * 1536:(i + 1) * 1536]
        w = np.ascontiguousarray(w.reshape(DEPTH, 16, 128, 1536).transpose(0, 2, 1, 3))
        b = inp["b_ada"][:, i * 1536:(i + 1) * 1536].reshape(DEPTH, 12, 128)
        bT = np.ascontiguousarray(b.transpose(2, 0, 1).reshape(128, DEPTH * 12))
        in_maps.append({"ccT": ccT, "w": w, "bT": bT})
    res = run(build_k0(), in_maps)
    modT = np.zeros((DEPTH, 128, 96, 2), np.float32)
    for i in range(NCORES):
        r = res[i]["modT"].reshape(128, DEPTH, 12, 2)
        for l in range(DEPTH):
            modT[l, :, i * 12:(i + 1) * 12, :] = r[:, l]
    return modT


NT1 = 1280
GROUPS1 = [(0, 256), (256, 768), (768, 1280)]


def rope_tables(core):
    t = core * 1024 + np.arange(1024)
    row = (t // 64).astype(np.float32)
    col = (t % 64).astype(np.float32)
    inv = (10000.0 ** (-np.arange(0, 32, 2, dtype=np.float32) / 32)).astype(np.float32)
    cosT = np.zeros((128, 1024), np.float32)
    sinT = np.zeros((128, 1024), np.float32)
    prot = np.zeros((128, 128), np.float32)
    for p in range(128):
        d = p % 64
        pos = row if d < 32 else col
        e = d % 32
        ang = (pos * inv[e % 16]).astype(np.float32)
        cosT[p] = np.cos(ang)
        sinT[p] = -np.sin(ang) if e < 16 else np.sin(ang)
        partner = p + 16 if e < 16 else p - 16
        prot[partner, p] = 1.0
    return cosT, sinT, prot


def rms_rstd(S, P, xt, junk, ss, rstd, width=D):
    S.op("act", lambda e: e.activation(out=junk[:], in_=xt[:], func=AF.Square, accum_out=ss[:]),
         reads=[xt.b], writes=[junk.b, ss.b])
    S.op("dve", lambda e: e.tensor_scalar(rstd[:], ss[:], 1.0 / width, EPS, ALU.mult, ALU.add),
         reads=[ss.b], writes=[rstd.b])
    S.op("act", lambda e: e.activation(out=rstd[:], in_=rstd[:], func=AF.Sqrt),
         reads=[rstd.b], writes=[rstd.b])
    S.op("dve", lambda e: e.reciprocal(rstd[:], rstd[:]),
         reads=[rstd.b], writes=[rstd.b])


def build_k1():
    P = Prog()
    S = P.S
    xin = P.din("xin", [NT1, D])
    modT = P.din("modT", [128, 96 * 2])
    ng0T = P.din("ng0T", [128, 16])
    w_in = P.din("w_in", [D, IN_COLS])
    cosd = P.din("cosT", [128, 1024])
    sind = P.din("sinT", [128, 1024])
    protd = P.din("prot", [128, 128])
    bgd = P.din("bgT", [128, 48])
    identd = P.din("ident", [128, 128], BF16)
    qkT = P.dout("qkT", [2048, NT1], BF16)
    vout = P.dout("v", [NT1, 1024], BF16)
    restT = P.dout("restT", [IN_COLS - 3072, NT1])

    mod = P.sb("mod", [128, 192]); ng0 = P.sb("ng0", [128, 16])
    cos = P.sb("cos", [128, 1024]); sin = P.sb("sin", [128, 1024])
    prot = P.sb("prot", [128, 128]); bg = P.sb("bg", [128, 48]); ident = P.sb("ident", [128, 128], BF16)
    for t_, d_ in ((mod, modT), (ng0, ng0T), (cos, cosd), (sin, sind), (prot, protd), (bg, bgd), (ident, identd)):
        S.dma("sp", t_[:], d_, writes=[t_.b])
    s1 = P.sb("s1", [128, 32])
    sh1 = P.sb("sh1", [128, 32])
    modv = mod.t[:].rearrange("p (c j) -> p c j", j=2)
    for j in range(2):
        S.op("dve", lambda e, j=j: e.tensor_scalar(s1[:, j * 16:(j + 1) * 16], modv[:, 16:32, j], 1.0, None, ALU.add),
             reads=[mod.b], writes=[s1.b])
        S.op("dve", lambda e, j=j: e.tensor_tensor(s1[:, j * 16:(j + 1) * 16], s1[:, j * 16:(j + 1) * 16], ng0[:], ALU.mult),
             reads=[s1.b, ng0.b], writes=[s1.b])
        S.op("dve", lambda e, j=j: e.tensor_copy(sh1[:, j * 16:(j + 1) * 16], modv[:, 0:16, j]),
             reads=[mod.b], writes=[sh1.b])

    hT = P.sb("hT", [128, 16, NT1], BF16)
    hTb = [Buf("hT%d" % g, multi=True) for g in range(3)]
    grp_of_tile = lambda tt: 0 if tt < 2 else (1 if tt < 6 else 2)
    xts = [P.sb("xt", [128, D]) for _ in range(2)]
    junk = P.sb("junk", [128, D])
    xns = [P.sb("xn", [128, D], BF16) for _ in range(2)]
    sss = [P.sb("ss", [128, 1]) for _ in range(2)]
    rstds = [P.sb("rstd", [128, 1]) for _ in range(2)]
    tps = [P.ps("tp", [128, 512], BF16) for _ in range(2)]
    ntp = 0
    for tt in range(NT1 // 128):
        xt, xn, ss, rstd = xts[tt % 2], xns[tt % 2], sss[tt % 2], rstds[tt % 2]
        j = 1 if tt < 2 else 0
        S.dma("sp", xt[:], xin[tt * 128:(tt + 1) * 128, :], writes=[xt.b])
        rms_rstd(S, P, xt, junk, ss, rstd)
        S.op("act", lambda e, xt=xt, xn=xn, rstd=rstd: e.activation(out=xn[:], in_=xt[:], func=AF.Copy, scale=rstd[:, 0:1]),
             reads=[xt.b, rstd.b], writes=[xn.b])
        for k4 in range(4):
            tp = tps[ntp % 2]; ntp += 1
            for kk in range(4):
                k = k4 * 4 + kk
                S.op("pe", lambda e, tp=tp, kk=kk, k=k, xn=xn: e.transpose(tp[:, kk * 128:(kk + 1) * 128], xn[:, k * 128:(k + 1) * 128], ident[:]),
                     reads=[xn.b, ident.b], writes=[tp.b])
            for kk in range(4):
                k = k4 * 4 + kk
                S.op("dve", lambda e, tp=tp, kk=kk, k=k, j=j, tt=tt: e.tensor_scalar(
                    hT[:, k, tt * 128:(tt + 1) * 128], tp[:, kk * 128:(kk + 1) * 128],
                    s1[:, j * 16 + k:j * 16 + k + 1], sh1[:, j * 16 + k:j * 16 + k + 1], ALU.mult, ALU.add),
                    reads=[tp.b, s1.b, sh1.b], writes=[hTb[grp_of_tile(tt)]])

    wv = w_in.rearrange("(k p) c -> p k c", p=128)
    wts = [P.sb("wt", [128, 16, 512], BF16) for _ in range(3)]
    accs = [P.ps("acc") for _ in range(4)]
    rots = [P.ps("rot") for _ in range(2)]
    stg = [P.sb("stg", [128, 512]) for _ in range(4)]
    stgb = [P.sb("stgb", [128, 512], BF16) for _ in range(3)]
    tmp1 = [P.sb("tmp1", [128, 512]) for _ in range(2)]
    na = 0; ns = 0; nsb = 0; nr = 0
    for cb in range(IN_COLS // 512):
        wt = wts[cb % 3]
        S.dma("pool", wt[:], wv[:, :, cb * 512:(cb + 1) * 512], writes=[wt.b])
        if cb in (4, 5):
            for tt in range(NT1 // 128):
                acc = accs[na % 4]; na += 1
                for k in range(16):
                    S.op("pe", lambda e, acc=acc, k=k, tt=tt, wt=wt: e.matmul(
                        acc[:], hT[:, k, tt * 128:(tt + 1) * 128], wt[:, k, :], start=(k == 0), stop=(k == 15)),
                        reads=[hTb[grp_of_tile(tt)], wt.b], writes=[acc.b])
                sb_ = stgb[nsb % 3]; nsb += 1
                S.op("act", lambda e, acc=acc, sb_=sb_: e.activation(out=sb_[:], in_=acc[:], func=AF.Copy),
                     reads=[acc.b], writes=[sb_.b])
                P.outs.append(S.dma("sp", vout[tt * 128:(tt + 1) * 128, (cb - 4) * 512:(cb - 3) * 512], sb_[:], reads=[sb_.b]))
            continue
        for c4 in range(4):
            ch = cb * 4 + c4
            for g, (g0, g1) in enumerate(GROUPS1):
                n = g1 - g0
                acc = accs[na % 4]; na += 1
                for k in range(16):
                    S.op("pe", lambda e, acc=acc, k=k, c4=c4, wt=wt, g0=g0, g1=g1, n=n: e.matmul(
                        acc[:, 0:n], wt[:, k, c4 * 128:(c4 + 1) * 128], hT[:, k, g0:g1], start=(k == 0), stop=(k == 15)),
                        reads=[hTb[g], wt.b], writes=[acc.b])
                if ch < 16:
                    sb_ = stgb[nsb % 3]; nsb += 1
                    if g == 0:
                        S.op("act", lambda e, acc=acc, sb_=sb_, n=n: e.activation(out=sb_[:, 0:n], in_=acc[:, 0:n], func=AF.Copy),
                             reads=[acc.b], writes=[sb_.b])
                    else:
                        a = stg[ns % 4]; ns += 1
                        S.op("act", lambda e, acc=acc, a=a, n=n: e.activation(out=a[:, 0:n], in_=acc[:, 0:n], func=AF.Copy),
                             reads=[acc.b], writes=[a.b])
                        rot = rots[nr % 2]; t1 = tmp1[nr % 2]; nr += 1
                        S.op("pe", lambda e, rot=rot, a=a, n=n: e.matmul(rot[:, 0:n], prot[:], a[:, 0:n], start=True, stop=True),
                             reads=[prot.b, a.b], writes=[rot.b])
                        c0 = g0 - 256
                        S.op("dve", lambda e, t1=t1, a=a, n=n, c0=c0: e.tensor_tensor(t1[:, 0:n], a[:, 0:n], cos[:, c0:c0 + n], ALU.mult),
                             reads=[a.b, cos.b], writes=[t1.b])
                        S.op("dve", lambda e, rot=rot, a=a, n=n, c0=c0: e.tensor_tensor(a[:, 0:n], rot[:, 0:n], sin[:, c0:c0 + n], ALU.mult),
                             reads=[rot.b, sin.b], writes=[a.b])
                        S.op("dve", lambda e, t1=t1, a=a, sb_=sb_, n=n: e.tensor_tensor(sb_[:, 0:n], t1[:, 0:n], a[:, 0:n], ALU.add),
                             reads=[t1.b, a.b], writes=[sb_.b])
                    P.outs.append(S.dma("sp", qkT[ch * 128:(ch + 1) * 128, g0:g1], sb_[:, 0:n], reads=[sb_.b]))
                else:
                    a = stg[ns % 4]; ns += 1
                    if ch < 64:
                        S.op("act", lambda e, acc=acc, a=a, n=n: e.activation(out=a[:, 0:n], in_=acc[:, 0:n], func=AF.Copy),
                             reads=[acc.b], writes=[a.b])
                    else:
                        gi = ch - 64
                        S.op("act", lambda e, acc=acc, a=a, n=n, gi=gi: e.activation(
                            out=a[:, 0:n], in_=acc[:, 0:n], func=AF.Sigmoid, bias=bg[:, gi:gi + 1]),
                            reads=[acc.b, bg.b], writes=[a.b])
                    r0 = (ch - 24) * 128
                    P.outs.append(S.dma("sp", restT[r0:r0 + 128, g0:g1], a[:, 0:n], reads=[a.b]))
    return P.finish()


def run_k1(inp, l, x, ctx, modT):
    ident = np.eye(128, dtype=np.float32).astype(ml_dtypes.bfloat16)
    in_maps = []
    for i in range(NCORES):
        cosT, sinT, prot = rope_tables(i)
        in_maps.append({
            "xin": np.ascontiguousarray(np.concatenate([ctx, x[i * 1024:(i + 1) * 1024]], axis=0)),
            "modT": np.ascontiguousarray(modT[l].reshape(128, 192)),
            "ng0T": fm(inp["norm_g"][l, 0], 16),
            "w_in": inp["w_in"][l],
            "cosT": cosT, "sinT": sinT, "prot": prot,
            "bgT": fm(inp["b_gate"][l], 48),
            "ident": ident,
        })
    return run(build_k1(), in_maps)


NTOK = LC + SEQ
NKT = NTOK // 128


def build_k2():
    P = Prog()
    S = P.S
    qd = P.din("qT2", [128, NTOK], BF16)
    kd = P.din("kT2", [128, NTOK], BF16)
    vd = P.din("vh", [NTOK, 128], BF16)
    lamd = P.din("lamp", [1, 256])
    lcd = P.din("lamc", [128, 2])
    gd = P.din("subg", [1, 128])
    identd = P.din("ident", [128, 128], BF16)
    aout = P.dout("aT", [128, NTOK], BF16)

    q = P.sb("q", [128, NTOK], BF16); k = P.sb("k", [128, NTOK], BF16)
    v = P.sb("v", [128, NKT, 129], BF16)
    lamp = P.sb("lamp", [128, 256]); lamc = P.sb("lamc", [128, 2]); g = P.sb("g", [128, 128])
    ident = P.sb("ident", [128, 128], BF16)
    S.dma("sp", q[:], qd, writes=[q.b])
    S.dma("sp", k[:], kd, writes=[k.b])
    S.op("pool", lambda e: e.memset(v[:, :, 128:129], 1.0), writes=[v.b])
    S.dma("sp", v[:, :, 0:128], vd.rearrange("(t p) d -> p t d", p=128), writes=[v.b])
    S.dma("sp", lamp[:], lamd.partition_broadcast(128), writes=[lamp.b])
    S.dma("sp", g[:], gd.partition_broadcast(128), writes=[g.b])
    S.dma("sp", lamc[:], lcd, writes=[lamc.b])
    S.dma("sp", ident[:], identd, writes=[ident.b])
    pr = P.sb("pr", [128, 128]); sm = P.sb("sm", [128, 2]); nlam = P.sb("nlam", [128, 1])
    S.op("dve", lambda e: e.tensor_tensor(pr[:, 0:64], lamp[:, 0:64], lamp[:, 64:128], ALU.mult), reads=[lamp.b], writes=[pr.b])
    S.op("dve", lambda e: e.tensor_tensor(pr[:, 64:128], lamp[:, 128:192], lamp[:, 192:256], ALU.mult), reads=[lamp.b, pr.b], writes=[pr.b])
    S.op("dve", lambda e: e.tensor_reduce(sm[:], pr[:].rearrange("p (a b) -> p a b", a=2), AX.X, ALU.add), reads=[pr.b], writes=[sm.b])
    S.op("act", lambda e: e.activation(out=sm[:], in_=sm[:], func=AF.Exp), reads=[sm.b], writes=[sm.b])
    S.op("dve", lambda e: e.tensor_tensor(nlam[:], sm[:, 1:2], sm[:, 0:1], ALU.subtract), reads=[sm.b], writes=[nlam.b])
    S.op("dve", lambda e: e.tensor_tensor(nlam[:], nlam[:], lamc[:, 0:1], ALU.subtract), reads=[nlam.b, lamc.b], writes=[nlam.b])
    S.op("dve", lambda e: e.tensor_scalar(g[:], g[:], lamc[:, 1:2], None, ALU.mult), reads=[g.b, lamc.b], writes=[g.b])

    sps = [P.ps("s") for _ in range(2)]
    ops_ = [[P.ps("o", [128, 512]) for _ in range(2)] for _ in range(2)]
    tps = P.ps("tp", [128, 512], BF16)
    pts = [P.sb("pT", [128, 512], BF16) for _ in range(3)]
    o1 = P.sb("o1", [128, 128]); dif = P.sb("dif", [128, 128]); junk = P.sb("junk", [128, 128])
    rs = P.sb("rs", [128, 2]); ss = P.sb("ss", [128, 1]); rstd = P.sb("rstd", [128, 1])
    ab = P.sb("ab", [128, 512], BF16); aTs = [P.sb("aTs", [128, 512], BF16) for _ in range(2)]
    nsp = 0; npt = 0
    groups = [(0, 256, 2)] + [(256 + 512 * gi, 256 + 512 * (gi + 1), NKT) for gi in range(SEQ // 512)]
    for gi, (q0, q1, nkt) in enumerate(groups):
        nq = q1 - q0
        nsub = nq // 128
        for kt in range(nkt):
            for j in range(2):
                sp = sps[nsp % 2]; nsp += 1
                pT = pts[npt % 3]; npt += 1
                S.op("pe", lambda e, sp=sp, j=j, kt=kt, q0=q0, q1=q1, nq=nq: e.matmul(
                    sp[:, 0:nq], k[j * 64:(j + 1) * 64, kt * 128:(kt + 1) * 128], q[j * 64:(j + 1) * 64, q0:q1],
                    start=True, stop=True), reads=[k.b, q.b], writes=[sp.b])
                S.op("act", lambda e, sp=sp, pT=pT, nq=nq: e.activation(out=pT[:, 0:nq], in_=sp[:, 0:nq], func=AF.Exp, scale=0.125),
                     reads=[sp.b], writes=[pT.b])
                for qs in range(nsub):
                    o_ = ops_[j][qs // 2]
                    S.op("pe", lambda e, o_=o_, qs=qs, pT=pT, kt=kt, nkt=nkt: e.matmul(
                        o_[:, (qs % 2) * 129:(qs % 2) * 129 + 129], pT[:, qs * 128:(qs + 1) * 128], v[:, kt, :],
                        start=(kt == 0 and qs % 2 == 0), stop=(kt == nkt - 1), skip_group_check=True),
                        reads=[pT.b, v.b], writes=[o_.b])
        for qs in range(nsub):
            oa = ops_[0][qs // 2]; ob = ops_[1][qs // 2]; h_ = qs % 2
            S.op("dve", lambda e, oa=oa, h_=h_: e.reciprocal(rs[:, 0:1], oa[:, h_ * 129 + 128:h_ * 129 + 129]), reads=[oa.b], writes=[rs.b])
            S.op("dve", lambda e, ob=ob, h_=h_: e.reciprocal(rs[:, 1:2], ob[:, h_ * 129 + 128:h_ * 129 + 129]), reads=[ob.b, rs.b], writes=[rs.b])
            S.op("dve", lambda e: e.tensor_tensor(rs[:, 1:2], rs[:, 1:2], nlam[:], ALU.mult), reads=[rs.b, nlam.b], writes=[rs.b])
            S.op("dve", lambda e, oa=oa, h_=h_: e.tensor_scalar(o1[:], oa[:, h_ * 129:h_ * 129 + 128], rs[:, 0:1], None, ALU.mult),
                 reads=[oa.b, rs.b], writes=[o1.b])
            S.op("dve", lambda e, ob=ob, h_=h_: e.scalar_tensor_tensor(dif[:], ob[:, h_ * 129:h_ * 129 + 128], rs[:, 1:2], o1[:], ALU.mult, ALU.add),
                 reads=[ob.b, rs.b, o1.b], writes=[dif.b])
            rms_rstd(S, P, dif, junk, ss, rstd, width=128)
            S.op("dve", lambda e: e.tensor_scalar(dif[:], dif[:], rstd[:, 0:1], None, ALU.mult), reads=[dif.b, rstd.b], writes=[dif.b])
            S.op("dve", lambda e, qs=qs: e.tensor_tensor(ab[:, qs * 128:(qs + 1) * 128], dif[:], g[:], ALU.mult),
                 reads=[dif.b, g.b], writes=[ab.b])
            S.op("pe", lambda e, qs=qs: e.transpose(tps[:, qs * 128:(qs + 1) * 128], ab[:, qs * 128:(qs + 1) * 128], ident[:]),
                 reads=[ab.b, ident.b], writes=[tps.b])
        aT = aTs[gi % 2]
        S.op("act", lambda e, aT=aT, nq=nq: e.activation(out=aT[:, 0:nq], in_=tps[:, 0:nq], func=AF.Copy), reads=[tps.b], writes=[aT.b])
        P.outs.append(S.dma("sp", aout[:, q0:q1], aT[:, 0:nq], reads=[aT.b]))
    return P.finish()


def lam_init_of(l):
    return 0.8 - 0.6 * math.exp(-0.3 * l)


def run_k2(inp, l, r1):
    ident = np.eye(128, dtype=np.float32).astype(ml_dtypes.bfloat16)
    qk = np.concatenate([r1[0]["qkT"][:, :LC]] + [r1[i]["qkT"][:, LC:] for i in range(NCORES)], axis=1)
    v = np.concatenate([r1[0]["v"][:LC]] + [r1[i]["v"][LC:] for i in range(NCORES)], axis=0)
    li = lam_init_of(l)
    lamc = np.tile(np.array([[li, 1.0 - li]], np.float32), (128, 1))
    in_maps = []
    for h in range(NCORES):
        in_maps.append({
            "qT2": np.ascontiguousarray(qk[h * 128:(h + 1) * 128]),
            "kT2": np.ascontiguousarray(qk[1024 + h * 128:1024 + (h + 1) * 128]),
            "vh": np.ascontiguousarray(v[:, h * 128:(h + 1) * 128]),
            "lamp": np.ascontiguousarray(inp["da_lambda"][l].reshape(1, 256)),
            "lamc": lamc,
            "subg": np.ascontiguousarray(inp["da_subln_g"][l].reshape(1, 128)),
            "ident": ident,
        })
    res = run(build_k2(), in_maps)
    return np.concatenate([res[h]["aT"] for h in range(NCORES)], axis=0)


HY_EMB = 33
TWO_PI = 2.0 * math.pi


def hyena_tables(n, core):
    m = np.arange(2 * n)
    pos = np.where(m < n, n - 1 - m, m - n)
    t = np.linspace(0.0, 1.0, n, dtype=np.float32)[pos]
    w = (2.0 * math.pi * pos.astype(np.float32) / n).astype(np.float32)
    bands = np.linspace(1e-4, 15, 16, dtype=np.float32)
    z = np.concatenate([t[None, :], np.cos(bands[:, None] * w[None, :]), -np.sin(bands[:, None] * w[None, :])], axis=0)
    lo = math.log(1e-2) / 1.5
    hi = math.log(1e-2) / 0.3
    deltas = np.abs(np.linspace(lo, hi, 1024, dtype=np.float32))[core * 128:(core + 1) * 128]
    dec = np.exp(-t[None, :] * deltas[:, None])
    return z.astype(np.float32), dec.astype(np.float32)


def build_k3(dbg=False):
    P = Prog()
    S = P.S
    N, NC_ = SEQ, LC
    ud = P.din("uT", [3, 128, NTOK])
    swd = P.din("swT", [128, 9]); sbd = P.din("sbT", [128, 3])
    zL = P.din("zL", [HY_EMB, 2 * N]); zC = P.din("zC", [HY_EMB, 2 * NC_])
    dL = P.din("dL", [128, 2 * N]); dC = P.din("dC", [128, 2 * NC_])
    w1d = P.din("w1", [HY_EMB, 64]); w2d = P.din("w2", [64, 64]); w3d = P.din("w3c", [64, 512])
    pd = P.din("mlp", [64, 3])
    hbd = P.din("hbT", [128, 2])
    identd = P.din("identF", [128, 128]); jmd = P.din("jm", [128, 128])
    out = P.dout("hyT", [128, NTOK])
    kdL = [P.dscratch("kdL%d" % o, [128, 2 * N], BF16) for o in range(2)]
    kdC = [P.dscratch("kdC%d" % o, [128, 2 * NC_], BF16) for o in range(2)]

    sw = P.sb("sw", [128, 9]); sbb = P.sb("sbb", [128, 3]); hb = P.sb("hb", [128, 2])
    ident = P.sb("ident", [128, 128]); jm = P.sb("jm", [128, 128])
    w1 = P.sb("w1", [HY_EMB, 64]); w2 = P.sb("w2", [64, 64]); w3 = P.sb("w3", [64, 512]); mp = P.sb("mp", [64, 3])
    for t_, d_ in ((sw, swd), (sbb, sbd), (hb, hbd), (ident, identd), (jm, jmd), (w1, w1d), (w2, w2d), (w3, w3d), (mp, pd)):
        S.dma("sp", t_[:], d_, writes=[t_.b])
    fb = P.sb("fb", [64, 2])
    S.op("dve", lambda e: e.tensor_scalar(fb[:], mp[:, 0:2], mp[:, 2:3], None, ALU.mult), reads=[mp.b], writes=[fb.b])

    def filters(n, zd, dd, kd):
        W = 2 * n
        P.push()
        kf = [P.sb("kf", [128, W]) for _ in range(2)]
        kfb = [Buf("kfb%d" % o, multi=True) for o in range(2)]
        nb = max(1, W // 512)
        bw = W // nb
        zts = [P.sb("zt", [HY_EMB, bw]) for _ in range(2)]
        dts = [P.sb("dt", [128, bw]) for _ in range(2)]
        hs = [[P.sb("h", [64, bw]) for _ in range(2)] for _ in range(2)]
        t1 = P.sb("t1", [64, bw]); t2 = P.sb("t2", [64, bw])
        pa = [P.ps("pa") for _ in range(2)]
        pb = [P.ps("pb") for _ in range(2)]

        def sin_layer(ps, h, li):
            S.op("dve", lambda e: e.tensor_scalar(h[:], ps[0:64, 0:bw], mp[:, 2:3], fb[:, li:li + 1], ALU.mult, ALU.add),
                 reads=[ps.b, mp.b, fb.b], writes=[h.b])
            S.op("dve", lambda e: e.tensor_scalar(t1[:], h[:], math.pi, -TWO_PI, ALU.is_gt, ALU.mult), reads=[h.b], writes=[t1.b])
            S.op("dve", lambda e: e.tensor_scalar(t2[:], h[:], -math.pi, TWO_PI, ALU.is_lt, ALU.mult), reads=[h.b], writes=[t2.b])
            S.op("dve", lambda e: e.tensor_tensor(t1[:], t1[:], t2[:], ALU.add), reads=[t1.b, t2.b], writes=[t1.b])
            S.op("dve", lambda e: e.tensor_tensor(h[:], h[:], t1[:], ALU.add), reads=[h.b, t1.b], writes=[h.b])
            S.op("act", lambda e: e.activation(out=h[:], in_=h[:], func=AF.Sin), reads=[h.b], writes=[h.b])

        for b in range(nb):
            zt = zts[b % 2]; dt_ = dts[b % 2]; c0 = b * bw
            S.dma("sp", zt[:], zd[:, c0:c0 + bw], writes=[zt.b])
            S.dma("sp", dt_[:], dd[:, c0:c0 + bw], writes=[dt_.b])
            p1 = pa[b % 2]; h1 = hs[0][b % 2]; h2 = hs[1][b % 2]
            S.op("pe", lambda e, p1=p1, zt=zt: e.matmul(p1[0:64, 0:bw], w1[:], zt[:], start=True, stop=True),
                 reads=[w1.b, zt.b], writes=[p1.b])
            sin_layer(p1, h1, 0)
            S.op("pe", lambda e, p1=p1, h1=h1: e.matmul(p1[0:64, 0:bw], w2[:], h1[:], start=True, stop=True),
                 reads=[w2.b, h1.b], writes=[p1.b])
            sin_layer(p1, h2, 1)
            for o in range(2):
                if bw <= n:
                    segs = [(0, bw, 0 if c0 < n else 1)]
                else:
                    segs = [(0, n, 0), (n, bw, 1)]
                p3 = pb[o]
                for (a0, a1, dr) in segs:
                    od = o * 2 + dr
                    S.op("pe", lambda e, p3=p3, h2=h2, od=od, a0=a0, a1=a1: e.matmul(
                        p3[:, a0:a1], w3[:, od * 128:(od + 1) * 128], h2[:, a0:a1], start=True, stop=True),
                        reads=[w3.b, h2.b], writes=[p3.b])
                S.op("dve", lambda e, p3=p3, o=o, dt_=dt_, c0=c0: e.tensor_tensor(kf[o][:, c0:c0 + bw], p3[:, 0:bw], dt_[:], ALU.mult),
                     reads=[p3.b, dt_.b], writes=[kfb[o]])
        cw = min(W, 2048)
        ncw = W // cw
        junk = P.sb("junk", [128, cw]); part = P.sb("part", [128, 8]); tot = P.sb("tot", [128, 1])
        obs = [P.sb("ob", [128, cw], BF16) for _ in range(2)]
        for o in range(2):
            for i in range(ncw):
                S.op("act", lambda e, o=o, i=i: e.activation(out=junk[:], in_=kf[o][:, i * cw:(i + 1) * cw], func=AF.Abs, accum_out=part[:, i:i + 1]),
                     reads=[kfb[o]], writes=[junk.b, part.b])
            S.op("dve", lambda e: e.tensor_reduce(tot[:], part[:, 0:ncw], AX.X, ALU.add), reads=[part.b], writes=[tot.b])
            S.op("dve", lambda e: e.reciprocal(tot[:], tot[:]), reads=[tot.b], writes=[tot.b])
            for i in range(ncw):
                ob = obs[i % 2]
                S.op("act", lambda e, o=o, i=i, ob=ob: e.activation(out=ob[:], in_=kf[o][:, i * cw:(i + 1) * cw], func=AF.Copy, scale=tot[:, 0:1]),
                     reads=[kfb[o], tot.b], writes=[ob.b])
                S.dma("sp", kd[o][:, i * cw:(i + 1) * cw], ob[:], reads=[ob.b], writes=[kd[o].b])
        P.pop()

    filters(N, zL, dL, kdL)
    filters(NC_, zC, dC, kdC)
    if dbg == 1:
        dbo = P.dout("dbg", [128, 2 * NC_], BF16)
        dbt = P.sb("dbt", [128, 2 * NC_], BF16)
        S.dma("sp", dbt[:], kdC[0][:], reads=[kdC[0].b], writes=[dbt.b])
        P.outs.append(S.dma("sp", dbo, dbt[:], reads=[dbt.b]))

    A = P.sb("A", [128, NTOK])
    B = P.sb("B", [128, NTOK])
    U = P.sb("U", [128, 2048])
    Zt = P.sb("Zt", [128, 128, 64], BF16)
    Yt = P.sb("Yt", [128, 64, 128])
    Ytb = Buf("Ytb", multi=True)
    Gs = [P.sb("G", [128, 2 * N - 128], BF16) for _ in range(2)]
    tmp = P.sb("tmp", [128, 512])
    tpi = [P.ps("tpi") for _ in range(2)]
    yb = [P.ps("yb") for _ in range(2)]
    tpo = [P.ps("tpo") for _ in range(2)]
    SEGS = [(0, NC_), (NC_, NTOK)]

    def short_conv(which, dst):
        CH = 2046
        for (s0, s1) in SEGS:
            c = s0
            while c < s1:
                e_ = min(c + CH, s1)
                lo = max(c - 1, s0); hi = min(e_ + 1, s1)
                nl = hi - lo
                S.dma("sp", U[:, 0:nl], ud[which, :, lo:hi], writes=[U.b])
                off = c - lo
                nn = e_ - c
                S.op("dve", lambda e, off=off, nn=nn, c=c: e.tensor_scalar(
                    dst[:, c:c + nn], U[:, off:off + nn], sw[:, which * 3 + 1:which * 3 + 2], sbb[:, which:which + 1], ALU.mult, ALU.add),
                    reads=[U.b, sw.b, sbb.b], writes=[dst.b])
                tl = c if off == 1 else c + 1
                S.op("dve", lambda e, tl=tl, e_=e_, lo=lo: e.scalar_tensor_tensor(
                    dst[:, tl:e_], U[:, tl - 1 - lo:e_ - 1 - lo], sw[:, which * 3:which * 3 + 1], dst[:, tl:e_], ALU.mult, ALU.add),
                    reads=[U.b, sw.b, dst.b], writes=[dst.b])
                tr = e_ if hi == e_ + 1 else e_ - 1
                S.op("dve", lambda e, c=c, tr=tr, lo=lo: e.scalar_tensor_tensor(
                    dst[:, c:tr], U[:, c + 1 - lo:tr + 1 - lo], sw[:, which * 3 + 2:which * 3 + 3], dst[:, c:tr], ALU.mult, ALU.add),
                    reads=[U.b, sw.b, dst.b], writes=[dst.b])
                c = e_

    ng = [0]

    def long_conv(o, X, gate, order=(1, 0), dbg_stop=False):
        for si in order:
            (s0, s1), kd = SEGS[si], (kdC[o], kdL[o])[si]
            n = s1 - s0
            nbk = n // 128
            for J0 in range(0, nbk, 4):
                nj = min(4, nbk - J0)
                tp = tpi[(J0 // 4) % 2]
                for jj in range(nj):
                    J = J0 + jj
                    S.op("pe", lambda e, tp=tp, jj=jj, J=J, s0=s0: e.transpose(tp[:, jj * 128:(jj + 1) * 128], X[:, s0 + J * 128:s0 + (J + 1) * 128], ident[:]),
                         reads=[X.b, ident.b], writes=[tp.b])
                eng = "act" if (J0 // 4) % 2 else "dve"
                src = tp[:, 0:nj * 128].rearrange("p (j c) -> p j c", j=nj)
                dst = Zt[:, :, J0:J0 + nj].rearrange("p c j -> p j c")
                if eng == "act":
                    S.op("act", lambda e, src=src, dst=dst: e.activation(out=dst, in_=src, func=AF.Copy), reads=[tp.b], writes=[Zt.b])
                else:
                    S.op("dve", lambda e, src=src, dst=dst: e.tensor_copy(dst, src), reads=[tp.b], writes=[Zt.b])
            for c in range(128):
                G = Gs[ng[0] % 2]; ng[0] += 1
                gw = 2 * n - 128
                S.dma("sp", G[:, 0:gw], bass.AP(kd.t.tensor, c * 2 * n, [[1, 128], [1, gw]]), reads=[kd.b], writes=[G.b])
                ybk = yb[(c // 8) % 2]
                a0 = (c % 8) * 64
                deltas = [0] + [d for d in range(-(nbk - 1), nbk) if d != 0]
                for di, dl in enumerate(deltas):
                    if dl >= 0:
                        i0, i1, j0, j1 = dl, nbk, 0, nbk - dl
                    else:
                        i0, i1, j0, j1 = 0, nbk + dl, -dl, nbk
                    S.op("pe", lambda e, ybk=ybk, a0=a0, i0=i0, i1=i1, j0=j0, j1=j1, G=G, n=n, dl=dl, c=c, di=di, nd=len(deltas): e.matmul(
                        ybk[:, a0 + i0:a0 + i1], G[:, n - 128 - 128 * dl:n - 128 * dl], Zt[:, c, j0:j1],
                        start=(di == 0), stop=(di == nd - 1), skip_group_check=True),
                        reads=[G.b, Zt.b], writes=[ybk.b])
                if c % 8 == 7:
                    c0 = c - 7
                    src = ybk[:, 0:512].rearrange("p (c i) -> p c i", c=8)[:, :, 0:nbk]
                    dst = Yt[:, 0:nbk, c0:c0 + 8].rearrange("p i c -> p c i")
                    if (c // 8) % 2:
                        S.op("act", lambda e, src=src, dst=dst: e.activation(out=dst, in_=src, func=AF.Copy), reads=[ybk.b], writes=[Ytb])
                    else:
                        S.op("dve", lambda e, src=src, dst=dst: e.tensor_copy(dst, src), reads=[ybk.b], writes=[Ytb])
            if dbg_stop:
                d1 = P.dout("dbgZ", [128, 128 * 64], BF16); d2 = P.dout("dbgY", [128, 64 * 128])
                P.outs.append(S.dma("sp", d1, Zt[:].rearrange("p c j -> p (c j)"), reads=[Zt.b]))
                P.outs.append(S.dma("sp", d2, Yt[:].rearrange("p i c -> p (i c)"), reads=[Ytb]))
                return
            for I0 in range(0, nbk, 4):
                ni = min(4, nbk - I0)
                tp = tpo[(I0 // 4) % 2]
                for ii in range(ni):
                    S.op("pe", lambda e, tp=tp, ii=ii, I0=I0: e.matmul(tp[:, ii * 128:(ii + 1) * 128], Yt[:, I0 + ii, :], jm[:], start=True, stop=True),
                         reads=[Ytb, jm.b], writes=[tp.b])
                t0 = s0 + I0 * 128; wd = ni * 128
                S.op("dve", lambda e, tp=tp, t0=t0, wd=wd: e.scalar_tensor_tensor(
                    tmp[:, 0:wd], X[:, t0:t0 + wd], hb[:, o:o + 1], tp[:, 0:wd], ALU.mult, ALU.add),
                    reads=[X.b, hb.b, tp.b], writes=[tmp.b])
                S.op("dve", lambda e, t0=t0, wd=wd: e.tensor_tensor(X[:, t0:t0 + wd], tmp[:, 0:wd], gate[:, t0:t0 + wd], ALU.mult),
                     reads=[tmp.b, gate.b, X.b], writes=[X.b])

    short_conv(2, A)
    short_conv(0, B)
    if dbg == 4:
        for c in range(3):
            G = Gs[c % 2]
            S.dma("sp", G[:, 0:2 * N - 128], bass.AP(kdL[0].t.tensor, c * 2 * N, [[1, 128], [1, 2 * N - 128]]), reads=[kdL[0].b], writes=[G.b])
            dbo = P.dout("dbgG%d" % c, [128, 2 * N - 128], BF16)
            P.outs.append(S.dma("sp", dbo, G[:, 0:2 * N - 128], reads=[G.b]))
        dbk = P.dout("dbgK", [128, 2 * N], BF16)
        P.outs.append(S.dma("sp", dbk, kdL[0][:], reads=[kdL[0].b]))
        return P.finish()
    if dbg == 2:
        dbo = P.dout("dbgA", [128, NTOK]); dbo2 = P.dout("dbgB", [128, NTOK])
        P.outs.append(S.dma("sp", dbo, A[:], reads=[A.b]))
        P.outs.append(S.dma("sp", dbo2, B[:], reads=[B.b]))
        return P.finish()
    if dbg == 5:
        long_conv(0, A, B, order=(0, 1), dbg_stop=True)
        return P.finish()
    long_conv(0, A, B)
    if dbg == 3:
        dbo = P.dout("dbgA", [128, NTOK])
        P.outs.append(S.dma("sp", dbo, A[:], reads=[A.b]))
        return P.finish()
    short_conv(1, B)
    long_conv(1, A, B)
    P.outs.append(S.dma("sp", out, A[:], reads=[A.b]))
    return P.finish()


def run_k3(inp, l, restT_full, dbg=False):
    identF = np.eye(128, dtype=np.float32)
    jm = np.ascontiguousarray(identF[::-1])
    in_maps = []
    for c in range(NCORES):
        zl, dl = hyena_tables(SEQ, c)
        zc, dc = hyena_tables(LC, c)
        u = np.stack([restT_full[j * 1024 + c * 128:j * 1024 + (c + 1) * 128] for j in range(3)], axis=0)
        sw = np.stack([inp["hy_short_w"][l][:, j * 1024 + c * 128:j * 1024 + (c + 1) * 128].T for j in range(3)], axis=1)
        sb_ = np.stack([inp["hy_short_b"][l][j * 1024 + c * 128:j * 1024 + (c + 1) * 128] for j in range(3)], axis=1)
        w3 = inp["hy_w3"][l].reshape(64, 2, 2, 1024)[:, :, :, c * 128:(c + 1) * 128].reshape(64, 512)
        mlp = np.stack([inp["hy_b1"][l], inp["hy_b2"][l], inp["hy_freq"][l]], axis=1)
        in_maps.append({
            "uT": np.ascontiguousarray(u), "swT": np.ascontiguousarray(sw.reshape(128, 9)), "sbT": np.ascontiguousarray(sb_),
            "zL": zl, "zC": zc, "dL": dl, "dC": dc,
            "w1": np.ascontiguousarray(inp["hy_w1"][l]), "w2": np.ascontiguousarray(inp["hy_w2"][l]), "w3c": np.ascontiguousarray(w3),
            "mlp": np.ascontiguousarray(mlp), "hbT": np.ascontiguousarray(inp["hy_bias"][l][:, c * 128:(c + 1) * 128].T),
            "identF": identF, "jm": jm,
        })
    res = run(build_k3(dbg), in_maps)
    if dbg:
        return res
    return np.concatenate([res[c]["hyT"] for c in range(NCORES)], axis=0)


CW4 = 1340


def build_k4():
    P = Prog()
    S = P.S
    gTd = P.din("gT", [6144, NT1])
    cvd = P.din("cvin", [2048, CW4])
    aTd = P.din("aT", [1024, NT1], BF16)
    hyd = P.din("hyT", [1024, NT1])
    xind = P.din("xin", [NT1, D])
    wbr = [P.din(n_, [1024, D]) for n_ in ("w_da", "w_hy", "w_cv")]
    woutd = P.din("w_out", [D, D])
    wrd = P.din("w_r", [D, 32]); brd = P.din("b_r", [1, 32])
    cvpd = P.din("cvp", [128, 8 * 34])
    modd = P.din("modT", [128, 192]); ng2d = P.din("ng2T", [128, 16])
    g1rd = P.din("g1row", [2, D]); ng1rd = P.din("ng1row", [1, D])
    identd = P.din("identF", [128, 128]); onesd = P.din("onesF", [128, 128])
    x1o = P.dout("x1", [NT1, D]); fTo = P.dout("fT", [D, NT1], BF16); wgo = P.dout("wg", [NT1, 32])

    ident = P.sb("ident", [128, 128]); ones = P.sb("ones", [128, 128])
    mod = P.sb("mod", [128, 192]); ng2 = P.sb("ng2", [128, 16]); cvp = P.sb("cvp", [128, 8, 34])
    S.dma("sp", ident[:], identd, writes=[ident.b]); S.dma("sp", ones[:], onesd, writes=[ones.b])
    S.dma("sp", mod[:], modd, writes=[mod.b]); S.dma("sp", ng2[:], ng2d, writes=[ng2.b])
    S.dma("sp", cvp[:].rearrange("p a b -> p (a b)"), cvpd, writes=[cvp.b])
    s2 = P.sb("s2", [128, 32]); sh2 = P.sb("sh2", [128, 32])
    modv = mod.t[:].rearrange("p (c j) -> p c j", j=2)
    for j in range(2):
        S.do("dve", [mod.b], [s2.b], "tensor_scalar", s2[:, j * 16:(j + 1) * 16], modv[:, 64:80, j], 1.0, None, ALU.add)
        S.do("dve", [s2.b, ng2.b], [s2.b], "tensor_tensor", s2[:, j * 16:(j + 1) * 16], s2[:, j * 16:(j + 1) * 16], ng2[:], ALU.mult)
        S.do("dve", [mod.b], [sh2.b], "tensor_copy", sh2[:, j * 16:(j + 1) * 16], modv[:, 48:64, j])

    ypT = P.sb("ypT", [128, 16, NT1], BF16)
    ypTb = [Buf("ypT%d" % g, multi=True) for g in range(3)]
    P.push()
    cvT = P.sb("cvT", [128, 8, NT1], BF16)
    cvTb = Buf("cvTb", multi=True)

    P.push()
    C = P.sb("C", [128, 8, NT1])
    Cb = [Buf("C%d" % ch) for ch in range(8)]
    ats = [P.sb("at", [128, CW4]) for _ in range(2)]
    gts = [P.sb("gt", [128, CW4]) for _ in range(2)]
    for ch in range(8):
        at = ats[ch % 2]; gt = gts[ch % 2]
        S.dma("sp", at[:], cvd[ch * 128:(ch + 1) * 128, :], writes=[at.b])
        S.dma("sp", gt[:], cvd[1024 + ch * 128:1024 + (ch + 1) * 128, :], writes=[gt.b])
        S.do("act", [gt.b], [gt.b], "activation", out=gt[:], in_=gt[:], func=AF.Sigmoid)
        S.do("dve", [at.b, gt.b], [at.b], "tensor_tensor", at[:], at[:], gt[:], ALU.mult)
        eng = "dve"
        for (o0, n, i0) in ((0, 256, 0), (256, 1024, 286)):
            acc = C[:, ch, o0:o0 + n]
            S.do(eng, [at.b, cvp.b], [Cb[ch]], "tensor_scalar", acc, at[:, i0:i0 + n], cvp[:, ch, 0:1], cvp[:, ch, 31:32], ALU.mult, ALU.add)
            for k in range(1, 31):
                S.do(eng, [at.b, cvp.b, Cb[ch]], [Cb[ch]], "scalar_tensor_tensor", acc, at[:, i0 + k:i0 + k + n], cvp[:, ch, k:k + 1], acc, ALU.mult, ALU.add)
    sps = P.ps("sps"); qps = P.ps("qps")
    sq = [P.sb("sq", [128, 512]) for _ in range(2)]
    mean = P.sb("mean", [128, 512]); rstd = P.sb("rstdc", [128, 512]); msq = P.sb("msq", [128, 512])
    tt_ = [P.sb("tt", [128, 512]) for _ in range(2)]
    for g, (g0, g1) in enumerate(GROUPS1):
        n = g1 - g0
        for ch in range(8):
            S.do("pe", [ones.b, Cb[ch]], [sps.b], "matmul", sps[:, 0:n], ones[:], C[:, ch, g0:g1], start=(ch == 0), stop=(ch == 7))
        for ch in range(8):
            q_ = sq[ch % 2]
            S.do("act", [Cb[ch]], [q_.b], "activation", out=q_[:, 0:n], in_=C[:, ch, g0:g1], func=AF.Square)
            S.do("pe", [ones.b, q_.b], [qps.b], "matmul", qps[:, 0:n], ones[:], q_[:, 0:n], start=(ch == 0), stop=(ch == 7))
        S.do("dve", [sps.b], [mean.b], "tensor_scalar", mean[:, 0:n], sps[:, 0:n], 1.0 / 1024, None, ALU.mult)
        S.do("dve", [mean.b], [msq.b], "tensor_tensor", msq[:, 0:n], mean[:, 0:n], mean[:, 0:n], ALU.mult)
        S.do("dve", [qps.b, msq.b], [rstd.b], "scalar_tensor_tensor", rstd[:, 0:n], qps[:, 0:n], 1.0 / 1024, msq[:, 0:n], ALU.mult, ALU.subtract)
        S.do("dve", [rstd.b], [rstd.b], "tensor_scalar", rstd[:, 0:n], rstd[:, 0:n], EPS, None, ALU.add)
        S.do("act", [rstd.b], [rstd.b], "activation", out=rstd[:, 0:n], in_=rstd[:, 0:n], func=AF.Sqrt)
        S.do("dve", [rstd.b], [rstd.b], "reciprocal", rstd[:, 0:n], rstd[:, 0:n])
        for ch in range(8):
            t_ = tt_[ch % 2]
            S.do("dve", [Cb[ch], mean.b], [t_.b], "tensor_tensor", t_[:, 0:n], C[:, ch, g0:g1], mean[:, 0:n], ALU.subtract)
            S.do("dve", [t_.b, rstd.b], [t_.b], "tensor_tensor", t_[:, 0:n], t_[:, 0:n], rstd[:, 0:n], ALU.mult)
            S.do("act", [t_.b, cvp.b], [cvTb], "activation", out=cvT[:, ch, g0:g1], in_=t_[:, 0:n], func=AF.Silu,
                 scale=cvp[:, ch, 32:33], bias=cvp[:, ch, 33:34])
    P.pop()

    P.push()
    aT = P.sb("aT", [128, 8, NT1], BF16); hyT = P.sb("hyT", [128, 8, NT1], BF16)
    S.dma("sp", aT[:], aTd.rearrange("(k p) t -> p k t", p=128), writes=[aT.b])
    S.dma("pool", hyT[:], hyd.rearrange("(k p) t -> p k t", p=128), writes=[hyT.b])
    acts = [(aT, aT.b), (hyT, hyT.b), (cvT, cvTb)]
    wbs = [[P.sb("wb", [128, 8, 512], BF16) for _ in range(3)] for _ in range(2)]
    gbs = [[P.sb("gb", [128, NT1]) for _ in range(3)] for _ in range(2)]
    pacc = [P.ps("pacc") for _ in range(6)]
    m1 = P.sb("m1", [128, 512]); m2 = P.sb("m2", [128, 512])
    npa = 0
    for cb in range(4):
        wset = wbs[cb % 2]
        for b in range(3):
            S.dma("pool", wset[b][:], wbr[b].rearrange("(k p) c -> p k c", p=128)[:, :, cb * 512:(cb + 1) * 512], writes=[wset[b].b])
        for c4 in range(4):
            j = cb * 4 + c4
            gset = gbs[j % 2]
            for b in range(3):
                S.dma("sp", gset[b][:], gTd[(b * 16 + j) * 128:(b * 16 + j + 1) * 128, :], writes=[gset[b].b])
            for g, (g0, g1) in enumerate(GROUPS1):
                n = g1 - g0
                pp = []
                for b in range(3):
                    pa_ = pacc[npa % 6]; npa += 1
                    at_, ab_ = acts[b]
                    for k in range(8):
                        S.do("pe", [wset[b].b, ab_], [pa_.b], "matmul", pa_[:, 0:n], wset[b][:, k, c4 * 128:(c4 + 1) * 128], at_[:, k, g0:g1],
                             start=(k == 0), stop=(k == 7))
                    pp.append(pa_)
                S.do("dve", [pp[0].b, gset[0].b], [m1.b], "tensor_tensor", m1[:, 0:n], pp[0][:, 0:n], gset[0][:, g0:g1], ALU.mult)
                S.do("dve", [pp[1].b, gset[1].b], [m2.b], "tensor_tensor", m2[:, 0:n], pp[1][:, 0:n], gset[1][:, g0:g1], ALU.mult)
                S.do("dve", [m1.b, m2.b], [m1.b], "tensor_tensor", m1[:, 0:n], m1[:, 0:n], m2[:, 0:n], ALU.add)
                S.do("dve", [pp[2].b, gset[2].b], [m2.b], "tensor_tensor", m2[:, 0:n], pp[2][:, 0:n], gset[2][:, g0:g1], ALU.mult)
                S.do("dve", [m1.b, m2.b], [ypTb[g]], "tensor_tensor", ypT[:, j, g0:g1], m1[:, 0:n], m2[:, 0:n], ALU.add)
    P.pop()
    P.pop()

    P.push()
    wout = P.sb("wout", [128, 16, D], BF16)
    woutb = [Buf("wout%d" % i) for i in range(4)]
    for cb in range(4):
        S.dma("pool", wout[:, :, cb * 512:(cb + 1) * 512], woutd.rearrange("(k p) c -> p k c", p=128)[:, :, cb * 512:(cb + 1) * 512], writes=[woutb[cb]])
    g1r = [P.sb("g1r", [128, D]) for _ in range(2)]
    ng1r = P.sb("ng1r", [128, D])
    S.dma("sp", ng1r[:], ng1rd.partition_broadcast(128), writes=[ng1r.b])
    for j in range(2):
        S.dma("sp", g1r[j][:], g1rd[j:j + 1, :].partition_broadcast(128), writes=[g1r[j].b])
        S.do("dve", [g1r[j].b, ng1r.b], [g1r[j].b], "tensor_tensor", g1r[j][:], g1r[j][:], ng1r[:], ALU.mult)
    wr = P.sb("wr", [128, 16, 32]); br = P.sb("br", [128, 32])
    S.dma("sp", wr[:], wrd.rearrange("(k p) e -> p k e", p=128), writes=[wr.b])
    S.dma("sp", br[:], brd.partition_broadcast(128), writes=[br.b])
    accs = [P.ps("acc") for _ in range(4)]
    tps = [P.ps("tp") for _ in range(2)]
    lps = P.ps("lps", [128, 32])
    xts = [P.sb("xt", [128, D]) for _ in range(2)]
    x1ts = [P.sb("x1t", [128, D]) for _ in range(2)]
    xn = P.sb("xn", [128, D]); junk = xn
    ssp = P.sb("ssp", [128, 4]); ss = P.sb("ss", [128, 1]); rstd1 = P.sb("rstd1", [128, 1]); rstd2 = P.sb("rstd2", [128, 1])
    t5 = P.sb("t5", [128, 512])
    f32T = [P.sb("f32T", [128, 16, 128]) for _ in range(2)]
    fbT = [P.sb("fbT", [128, 16, 128], BF16) for _ in range(2)]
    lg = P.sb("lg", [128, 32]); top8 = P.sb("top8", [128, 8]); msk = P.sb("msk", [128, 32]); ex = P.sb("ex", [128, 32])
    nmx = P.sb("nmx", [128, 1]); sm = P.sb("sm", [128, 1]); wgt = [P.sb("wgt", [128, 32]) for _ in range(2)]
    fTv = fTo.rearrange("(k p) t -> p k t", p=128)
    ntp = 0
    for tt in range(NT1 // 128):
        j = 1 if tt < 2 else 0
        g = 0 if tt < 2 else (1 if tt < 6 else 2)
        xt = xts[tt % 2]; x1t = x1ts[tt % 2]
        S.dma("sp", xt[:], xind[tt * 128:(tt + 1) * 128, :], writes=[xt.b])
        for cb in range(4):
            for k in range(16):
                S.do("pe", [ypTb[g], woutb[cb]], [accs[cb].b], "matmul", accs[cb][:], ypT[:, k, tt * 128:(tt + 1) * 128], wout[:, k, cb * 512:(cb + 1) * 512],
                     start=(k == 0), stop=(k == 15))
            S.do("act", [accs[cb].b], [junk.b, ssp.b], "activation", out=junk[:, 0:512], in_=accs[cb][:], func=AF.Square, accum_out=ssp[:, cb:cb + 1])
        S.do("dve", [ssp.b], [ss.b], "tensor_reduce", ss[:], ssp[:], AX.X, ALU.add)
        S.do("dve", [ss.b], [rstd1.b], "tensor_scalar", rstd1[:], ss[:], 1.0 / D, EPS, ALU.mult, ALU.add)
        S.do("act", [rstd1.b], [rstd1.b], "activation", out=rstd1[:], in_=rstd1[:], func=AF.Sqrt)
        S.do("dve", [rstd1.b], [rstd1.b], "reciprocal", rstd1[:], rstd1[:])
        for cb in range(4):
            S.do("dve", [accs[cb].b, rstd1.b, g1r[j].b], [t5.b], "scalar_tensor_tensor", t5[:], accs[cb][:], rstd1[:, 0:1], g1r[j][:, cb * 512:(cb + 1) * 512], ALU.mult, ALU.mult)
            S.do("dve", [t5.b, xt.b], [x1t.b], "tensor_tensor", x1t[:, cb * 512:(cb + 1) * 512], t5[:], xt[:, cb * 512:(cb + 1) * 512], ALU.add)
        P.outs.append(S.dma("sp", x1o[tt * 128:(tt + 1) * 128, :], x1t[:], reads=[x1t.b]))
        S.do("act", [x1t.b], [junk.b, ss.b], "activation", out=junk[:], in_=x1t[:], func=AF.Square, accum_out=ss[:])
        S.do("dve", [ss.b], [rstd2.b], "tensor_scalar", rstd2[:], ss[:], 1.0 / D, EPS, ALU.mult, ALU.add)
        S.do("act", [rstd2.b], [rstd2.b], "activation", out=rstd2[:], in_=rstd2[:], func=AF.Sqrt)
        S.do("dve", [rstd2.b], [rstd2.b], "reciprocal", rstd2[:], rstd2[:])
        S.do("act", [x1t.b, rstd2.b], [xn.b], "activation", out=xn[:], in_=x1t[:], func=AF.Copy, scale=rstd2[:, 0:1])
        f32 = f32T[tt % 2]; fb = fbT[tt % 2]
        for k4 in range(4):
            tp = tps[ntp % 2]; ntp += 1
            for kk in range(4):
                k = k4 * 4 + kk
                S.do("pe", [xn.b, ident.b], [tp.b], "transpose", tp[:, kk * 128:(kk + 1) * 128], xn[:, k * 128:(k + 1) * 128], ident[:])
            for kk in range(4):
                k = k4 * 4 + kk
                S.do("dve", [tp.b, s2.b, sh2.b], [f32.b], "tensor_scalar", f32[:, k, :], tp[:, kk * 128:(kk + 1) * 128],
                     s2[:, j * 16 + k:j * 16 + k + 1], sh2[:, j * 16 + k:j * 16 + k + 1], ALU.mult, ALU.add)
        S.do("act", [f32.b], [fb.b], "activation", out=fb[:], in_=f32[:], func=AF.Copy)
        P.outs.append(S.dma("sp", fTv[:, :, tt * 128:(tt + 1) * 128], fb[:], reads=[fb.b]))
        for k in range(16):
            S.do("pe", [f32.b, wr.b], [lps.b], "matmul", lps[:], f32[:, k, :], wr[:, k, :], start=(k == 0), stop=(k == 15))
        S.do("dve", [lps.b, br.b], [lg.b], "tensor_tensor", lg[:], lps[:], br[:], ALU.add)
        S.do("dve", [lg.b], [top8.b], "max", top8[:], lg[:])
        S.do("dve", [lg.b, top8.b], [msk.b], "tensor_scalar", msk[:], lg[:], top8[:, 3:4], None, ALU.is_ge)
        S.do("dve", [top8.b], [nmx.b], "tensor_scalar", nmx[:], top8[:, 0:1], -1.0, None, ALU.mult)
        S.do("act", [lg.b, nmx.b], [ex.b], "activation", out=ex[:], in_=lg[:], func=AF.Exp, bias=nmx[:, 0:1])
        S.do("dve", [ex.b, msk.b], [ex.b], "tensor_tensor", ex[:], ex[:], msk[:], ALU.mult)
        S.do("dve", [ex.b], [sm.b], "tensor_reduce", sm[:], ex[:], AX.X, ALU.add)
        S.do("dve", [sm.b], [sm.b], "reciprocal", sm[:], sm[:])
        w_ = wgt[tt % 2]
        S.do("dve", [ex.b, sm.b], [w_.b], "tensor_scalar", w_[:], ex[:], sm[:, 0:1], None, ALU.mult)
        P.outs.append(S.dma("sp", wgo[tt * 128:(tt + 1) * 128, :], w_[:], reads=[w_.b]))
    P.pop()
    return P.finish()


def mod_rows(modT_l):
    return np.ascontiguousarray(modT_l.transpose(2, 1, 0).reshape(2, 12288))


def run_k4(inp, l, x, ctx, modT, rest_full, aT_full, hyT_full):
    identF = np.eye(128, dtype=np.float32)
    onesF = np.ones((128, 128), np.float32)
    rows = mod_rows(modT[l])
    g1row = np.ascontiguousarray(rows[:, 2 * D:3 * D])
    cvp = np.zeros((128, 8, 34), np.float32)
    cvp[:, :, 0:31] = inp["cv_dw_w"][l].reshape(31, 8, 128).transpose(2, 1, 0)
    cvp[:, :, 31] = fm(inp["cv_dw_b"][l], 8)
    cvp[:, :, 32] = fm(inp["cv_ln_g"][l], 8)
    cvp[:, :, 33] = fm(inp["cv_ln_b"][l], 8)
    cvall = rest_full[3072:5120]
    z15 = np.zeros((2048, 15), np.float32)
    in_maps = []
    for i in range(NCORES):
        t0 = LC + i * 1024
        left = cvall[:, t0 - 15:t0] if i > 0 else z15
        right = cvall[:, t0 + 1024:t0 + 1039] if i < NCORES - 1 else z15
        cvin = np.concatenate([z15, cvall[:, :LC], z15, left, cvall[:, t0:t0 + 1024], right], axis=1)
        sel = lambda a: np.ascontiguousarray(np.concatenate([a[:, :LC], a[:, t0:t0 + 1024]], axis=1))
        in_maps.append({
            "gT": sel(rest_full[5120:]), "cvin": np.ascontiguousarray(cvin),
            "aT": sel(aT_full), "hyT": sel(hyT_full),
            "xin": np.ascontiguousarray(np.concatenate([ctx, x[i * 1024:(i + 1) * 1024]], axis=0)),
            "w_da": inp["w_da_out"][l], "w_hy": inp["w_hy_out"][l], "w_cv": inp["w_cv_out"][l], "w_out": inp["w_out"][l],
            "w_r": inp["w_router"][l], "b_r": np.ascontiguousarray(inp["b_router"][l].reshape(1, 32)),
            "cvp": np.ascontiguousarray(cvp.reshape(128, 8 * 34)),
            "modT": np.ascontiguousarray(modT[l].reshape(128, 192)), "ng2T": fm(inp["norm_g"][l, 2], 16),
            "g1row": g1row, "ng1row": np.ascontiguousarray(inp["norm_g"][l, 1].reshape(1, D)),
            "identF": identF, "onesF": onesF,
        })
    return run(build_k4(), in_maps)


EPC = NE // NCORES
SW_LIMIT = 7.0
SW_ALPHA = 1.702


def build_k5():
    P = Prog()
    S = P.S
    fTd = P.din("fT", [D, NTOK], BF16)
    wg4d = P.din("wg4", [128, NKT * EPC])
    wgTd = P.din("wgT4", [EPC, NTOK])
    wgud = P.din("w_gu", [EPC, D, 2 * DE]); bgud = P.din("b_guT", [128, EPC * 16])
    wdnd = P.din("w_dn", [EPC, DE, D]); bdnd = P.din("b_dn", [EPC, D])
    part = P.dout("part", [NTOK, D])

    wg4 = P.sb("wg4", [128, NKT * EPC]); bgu = P.sb("bgu", [128, EPC * 16]); bdn = P.sb("bdn", [EPC, D])
    S.dma("sp", wg4[:], wg4d, writes=[wg4.b]); S.dma("sp", bgu[:], bgud, writes=[bgu.b]); S.dma("sp", bdn[:], bdnd, writes=[bdn.b])

    gus = [P.dscratch("gus%d" % e, [128, 16 * 2 * DE], BF16) for e in range(EPC)]
    dns = [P.dscratch("dns%d" % e, [128, 8 * D], BF16) for e in range(EPC)]
    for t_ in gus + dns:
        t_.b.multi = True
    P.push()
    st = [P.sb("st", [128, 16, 512], BF16) for _ in range(3)]
    ns = 0
    for e in range(EPC):
        guv0 = gus[e].t.rearrange("p (k c) -> p k c", k=16)
        dnv0 = dns[e].t.rearrange("p (k c) -> p k c", k=8)
        for cb in range(4):
            s_ = st[ns % 3]; ns += 1
            S.dma("pool", s_[:], wgud[e].rearrange("(k p) c -> p k c", p=128)[:, :, cb * 512:(cb + 1) * 512], writes=[s_.b])
            S.dma("sp", guv0[:, :, cb * 512:(cb + 1) * 512], s_[:], reads=[s_.b], writes=[gus[e].b])
        for cb in range(4):
            s_ = st[ns % 3]; ns += 1
            S.dma("pool", s_[:, 0:8, :], wdnd[e].rearrange("(k p) c -> p k c", p=128)[:, :, cb * 512:(cb + 1) * 512], writes=[s_.b])
            S.dma("sp", dnv0[:, :, cb * 512:(cb + 1) * 512], s_[:, 0:8, :], reads=[s_.b], writes=[dns[e].b])
    P.pop()

    fTv = fTd.rearrange("(k p) t -> p k t", p=128)
    fgs = [P.sb("fg", [128, 16, 512], BF16) for _ in range(2)]
    wgts = [P.sb("wgt", [EPC, 512]) for _ in range(2)]
    acc = P.sb("acc", [128, 4, D])
    accb = [[Buf("acc%d_%d" % (t, cb)) for cb in range(4)] for t in range(4)]
    gs = P.sb("gs", [128, 8, 512]); gsb = [Buf("gs%d" % j) for j in range(8)]
    actT = P.sb("actT", [128, 8, 512], BF16); actb = Buf("actT", multi=True)
    wgu_t = [P.sb("wgu", [128, 16, 512], BF16) for _ in range(3)]
    wdn_t = [P.sb("wdn", [128, 8, 512], BF16) for _ in range(3)]
    t1s = [P.sb("t1", [128, 512]) for _ in range(2)]
    sgs = [P.sb("sg", [128, 512]) for _ in range(2)]
    pg = [P.ps("pg") for _ in range(3)]
    pd = [P.ps("pd") for _ in range(3)]
    pb = P.ps("pb")
    npg = 0; npd = 0; nwu = 0; nwd = 0; nt1 = 0
    groups = [(t0, min(t0 + 512, NTOK)) for t0 in range(0, NTOK, 512)]
    for gi, (t0, t1) in enumerate(groups):
        n = t1 - t0
        ntile = n // 128
        fg = fgs[gi % 2]; wgt = wgts[gi % 2]
        S.dma("sp", fg[:, :, 0:n], fTv[:, :, t0:t1], writes=[fg.b])
        S.dma("sp", wgt[:, 0:n], wgTd[:, t0:t1], writes=[wgt.b])
        for tl in range(ntile):
            for cb in range(4):
                S.do("pe", [wgt.b, bdn.b], [pb.b], "matmul", pb[:], wgt[:, tl * 128:(tl + 1) * 128], bdn[:, cb * 512:(cb + 1) * 512], start=True, stop=True)
                S.do("act", [pb.b], [accb[tl][cb]], "activation", out=acc[:, tl, cb * 512:(cb + 1) * 512], in_=pb[:], func=AF.Copy)
        for e in range(EPC):
            guv = gus[e].t.rearrange("p (k c) -> p k c", k=16)
            dnv = dns[e].t.rearrange("p (k c) -> p k c", k=8)
            for cb in range(4):
                W = wgu_t[nwu % 3]; nwu += 1
                S.dma("sp", W[:], guv[:, :, cb * 512:(cb + 1) * 512], reads=[gus[e].b], writes=[W.b])
                for c4 in range(4):
                    ch = cb * 4 + c4
                    ps = pg[npg % 3]; npg += 1
                    for k in range(16):
                        S.do("pe", [W.b, fg.b], [ps.b], "matmul", ps[:, 0:n], W[:, k, c4 * 128:(c4 + 1) * 128], fg[:, k, 0:n], start=(k == 0), stop=(k == 15))
                    bcol = bgu[:, e * 16 + ch:e * 16 + ch + 1]
                    t_ = t1s[nt1 % 2]; sg = sgs[nt1 % 2]; nt1 += 1
                    if ch < 8:
                        S.do("dve", [ps.b, bgu.b], [t_.b], "tensor_scalar", t_[:, 0:n], ps[:, 0:n], bcol, SW_LIMIT, ALU.add, ALU.min)
                        S.do("act", [t_.b], [sg.b], "activation", out=sg[:, 0:n], in_=t_[:, 0:n], func=AF.Sigmoid, scale=SW_ALPHA)
                        S.do("dve", [t_.b, sg.b], [gsb[ch]], "tensor_tensor", gs[:, ch, 0:n], t_[:, 0:n], sg[:, 0:n], ALU.mult)
                    else:
                        j = ch - 8
                        S.do("dve", [ps.b, bgu.b], [t_.b], "tensor_scalar", t_[:, 0:n], ps[:, 0:n], bcol, SW_LIMIT, ALU.add, ALU.min)
                        S.do("dve", [t_.b], [t_.b], "tensor_scalar", t_[:, 0:n], t_[:, 0:n], -SW_LIMIT, 1.0, ALU.max, ALU.add)
                        S.do("dve", [t_.b, gsb[j]], [actb], "tensor_tensor", actT[:, j, 0:n], gs[:, j, 0:n], t_[:, 0:n], ALU.mult)
            for cb in range(4):
                Wd = wdn_t[nwd % 3]; nwd += 1
                S.dma("sp", Wd[:], dnv[:, :, cb * 512:(cb + 1) * 512], reads=[dns[e].b], writes=[Wd.b])
                for tl in range(ntile):
                    ps = pd[npd % 3]; npd += 1
                    for k in range(8):
                        S.do("pe", [actb, Wd.b], [ps.b], "matmul", ps[:], actT[:, k, tl * 128:(tl + 1) * 128], Wd[:, k, :], start=(k == 0), stop=(k == 7))
                    tg = (t0 // 128) + tl
                    a_ = acc[:, tl, cb * 512:(cb + 1) * 512]
                    S.do("dve", [ps.b, wg4.b, accb[tl][cb]], [accb[tl][cb]], "scalar_tensor_tensor", a_, ps[:], wg4[:, tg * EPC + e:tg * EPC + e + 1], a_, ALU.mult, ALU.add)
        rd = [accb[tl][cb] for tl in range(ntile) for cb in range(4)]
        P.outs.append(S.dma("sp", part[t0:t1, :].rearrange("(t p) d -> p t d", p=128), acc[:, 0:ntile, :], reads=rd))
    return P.finish()


def run_k5(inp, l, fT_full, wg_full):
    in_maps = []
    for c in range(NCORES):
        es = slice(c * EPC, (c + 1) * EPC)
        wg = wg_full[:, es]
        wg4 = wg.reshape(NKT, 128, EPC).transpose(1, 0, 2).reshape(128, NKT * EPC)
        bgu = inp["b_gu"][l][es].reshape(EPC, 16, 128).transpose(2, 0, 1).reshape(128, EPC * 16)
        in_maps.append({
            "fT": fT_full, "wg4": np.ascontiguousarray(wg4), "wgT4": np.ascontiguousarray(wg.T),
            "w_gu": inp["w_gu"][l][es], "b_guT": np.ascontiguousarray(bgu),
            "w_dn": inp["w_dn"][l][es], "b_dn": np.ascontiguousarray(inp["b_dn"][l][es]),
        })
    res = run(build_k5(), in_maps)
    return [res[c]["part"] for c in range(NCORES)]


NT6 = 1024 + 32


def build_k6():
    P = Prog()
    S = P.S
    pd_ = P.din("parts", [NCORES, NT6, D])
    x1d = P.din("x1", [NT6, D])
    g2rd = P.din("g2row", [2, D]); ng3rd = P.din("ng3row", [1, D])
    out = P.dout("x2", [NT6, D])
    g2r = [P.sb("g2r", [128, D]) for _ in range(2)]
    ng3r = P.sb("ng3r", [128, D])
    S.dma("sp", ng3r[:], ng3rd.partition_broadcast(128), writes=[ng3r.b])
    for j in range(2):
        S.dma("sp", g2r[j][:], g2rd[j:j + 1, :].partition_broadcast(128), writes=[g2r[j].b])
        S.do("dve", [g2r[j].b, ng3r.b], [g2r[j].b], "tensor_tensor", g2r[j][:], g2r[j][:], ng3r[:], ALU.mult)
    pts = [P.sb("pt", [128, D]) for _ in range(4)]
    fs = [P.sb("f", [128, D]) for _ in range(2)]
    xts = [P.sb("xt", [128, D]) for _ in range(2)]
    junk = P.sb("junk", [128, D])
    ss = P.sb("ss", [128, 1]); rstd = P.sb("rstd", [128, 1])
    npt = 0
    tiles = [(t * 128, 128, 0) for t in range(8)] + [(1024, 32, 1)]
    for ti, (r0, nr, j) in enumerate(tiles):
        f = fs[ti % 2]; xt = xts[ti % 2]
        S.dma("sp", f[0:nr, :], pd_[0, r0:r0 + nr, :], writes=[f.b])
        S.dma("sp", xt[0:nr, :], x1d[r0:r0 + nr, :], writes=[xt.b])
        for c in range(1, NCORES):
            pt = pts[npt % 4]; npt += 1
            S.dma("sp", pt[0:nr, :], pd_[c, r0:r0 + nr, :], writes=[pt.b])
            eng = "dve" if c % 2 else "pool"
            S.do(eng, [f.b, pt.b], [f.b], "tensor_tensor", f[0:nr, :], f[0:nr, :], pt[0:nr, :], ALU.add)
        S.do("act", [f.b], [junk.b, ss.b], "activation", out=junk[0:nr, :], in_=f[0:nr, :], func=AF.Square, accum_out=ss[0:nr, :])
        S.do("dve", [ss.b], [rstd.b], "tensor_scalar", rstd[0:nr, :], ss[0:nr, :], 1.0 / D, EPS, ALU.mult, ALU.add)
        S.do("act", [rstd.b], [rstd.b], "activation", out=rstd[0:nr, :], in_=rstd[0:nr, :], func=AF.Sqrt)
        S.do("dve", [rstd.b], [rstd.b], "reciprocal", rstd[0:nr, :], rstd[0:nr, :])
        S.do("dve", [f.b, rstd.b, g2r[j].b], [f.b], "scalar_tensor_tensor", f[0:nr, :], f[0:nr, :], rstd[0:nr, 0:1], g2r[j][0:nr, :], ALU.mult, ALU.mult)
        S.do("dve", [f.b, xt.b], [xt.b], "tensor_tensor", xt[0:nr, :], xt[0:nr, :], f[0:nr, :], ALU.add)
        P.outs.append(S.dma("sp", out[r0:r0 + nr, :], xt[0:nr, :], reads=[xt.b]))
    return P.finish()


def run_k6(inp, l, parts, x1_lat, x1_ctx, modT):
    rows = mod_rows(modT[l])
    g2row = np.ascontiguousarray(rows[:, 5 * D:6 * D])
    ng3row = np.ascontiguousarray(inp["norm_g"][l, 3].reshape(1, D))
    in_maps = []
    for i in range(NCORES):
        sel = lambda a_lat, a_ctx: np.concatenate([a_lat[i * 1024:(i + 1) * 1024], a_ctx[i * 32:(i + 1) * 32]], axis=0)
        pp = np.stack([sel(p[LC:], p[:LC]) for p in parts], axis=0)
        in_maps.append({"parts": np.ascontiguousarray(pp), "x1": np.ascontiguousarray(sel(x1_lat, x1_ctx)),
                        "g2row": g2row, "ng3row": ng3row})
    res = run(build_k6(), in_maps)
    x2 = np.concatenate([res[i]["x2"][:1024] for i in range(NCORES)], axis=0)
    ctx2 = np.concatenate([res[i]["x2"][1024:] for i in range(NCORES)], axis=0)
    return x2, ctx2


def kernel(**inputs):
    inp = {k: np.asarray(v) for k, v in inputs.items()}
    x = np.ascontiguousarray(inp["x"][0])
    ctx = np.ascontiguousarray(inp["ctx"][0])
    modT = run_k0(inp)
    for l in range(DEPTH):
        r1 = run_k1(inp, l, x, ctx, modT)
        aT = run_k2(inp, l, r1)
        rest_full = np.concatenate([r1[0]["restT"][:, :LC]] + [r1[i]["restT"][:, LC:] for i in range(NCORES)], axis=1)
        del r1
        hyT = run_k3(inp, l, rest_full)
        r4 = run_k4(inp, l, x, ctx, modT, rest_full, aT, hyT)
        del rest_full
        fT = np.ascontiguousarray(np.concatenate([r4[0]["fT"][:, :LC]] + [r4[i]["fT"][:, LC:] for i in range(NCORES)], axis=1))
        wg = np.concatenate([r4[0]["wg"][:LC]] + [r4[i]["wg"][LC:] for i in range(NCORES)], axis=0)
        x1_lat = np.concatenate([r4[i]["x1"][LC:] for i in range(NCORES)], axis=0)
        x1_ctx = r4[0]["x1"][:LC]
        del r4
        parts = run_k5(inp, l, fT, wg)
        x, ctx = run_k6(inp, l, parts, x1_lat, x1_ctx, modT)
        del parts
    return np.ascontiguousarray(x[None].astype(np.float32))
```

```python
import math
from contextlib import ExitStack
import numpy as np
import ml_dtypes
import concourse.bass as bass
import concourse.mybir as mybir
from concourse.bass_utils import run_bass_kernel_spmd

F32 = mybir.dt.float32
BF16 = mybir.dt.bfloat16
AF = mybir.ActivationFunctionType
ALU = mybir.AluOpType
AX = mybir.AxisListType

NCORES = 8
D = 2048
SEQ = 8192
LC = 256
DEPTH = 2
EPS = 1e-6
IN_COLS = 14336
OFF_Q, OFF_K, OFF_V, OFF_HY, OFF_CV, OFF_GATE = 0, 1024, 2048, 3072, 6144, 8192
NE = 32
DE = 1024


class Buf:
    __slots__ = ("name", "w", "r", "multi")

    def __init__(self, name="", multi=False):
        self.name = name
        self.w = []
        self.r = []
        self.multi = multi


class Sched:
    ENG = ("pe", "dve", "act", "pool", "sp")

    def __init__(self, nc, es, n_dma_sems=24):
        self.nc = nc
        self.es = es
        self.ops = []
        self.n_dma_sems = n_dma_sems
        self.last = {}
        self.dmas_since = []
        self.pending = {}

    def barrier(self):
        deps = set(self.last.values()) | set(self.dmas_since)
        self.dmas_since = []
        for e in self.ENG:
            self.pending[e] = set(deps) | self.pending.get(e, set())

    def _deps(self, reads, writes):
        deps = set()
        for b in reads:
            deps.update(b.w)
        for b in writes:
            if not (b.multi and not b.r):
                deps.update(b.w)
            deps.update(b.r)
        return deps

    def _record(self, idx, reads, writes):
        for b in writes:
            if b.multi and not b.r:
                b.w.append(idx)
            else:
                b.w = [idx]
            b.r = []
        for b in reads:
            b.r.append(idx)

    def op(self, eng, fn, reads=(), writes=()):
        deps = self._deps(reads, writes) | self.pending.pop(eng, set())
        idx = len(self.ops)
        self.last[eng] = idx
        self.ops.append(dict(eng=eng, fn=fn, deps=deps, kind="c"))
        self._record(idx, reads, writes)
        return idx

    def do(self, eng, reads, writes, method, *a, **kw):
        return self.op(eng, lambda e: getattr(e, method)(*a, **kw), reads=reads, writes=writes)

    def dma(self, q, out, in_, reads=(), writes=(), **kw):
        deps = self._deps(reads, writes) | self.pending.pop(q, set())
        idx = len(self.ops)
        self.last[q] = idx
        self.dmas_since.append(idx)
        self.ops.append(dict(eng=q, fn=lambda e: e.dma_start(out=out, in_=in_, **kw), deps=deps, kind="d"))
        self._record(idx, reads, writes)
        return idx

    def emit(self, final_waits=()):
        nc = self.nc
        engs = {"pe": nc.tensor, "dve": nc.vector, "act": nc.scalar, "pool": nc.gpsimd, "sp": nc.sync}
        ops = self.ops
        for i, o in enumerate(ops):
            best = {}
            red = set()
            for d in o["deps"]:
                po = ops[d]
                if po["kind"] == "c":
                    if po["eng"] == "pe" and o["eng"] == "pe" and o["kind"] == "c":
                        continue
                    if best.get(po["eng"], -1) < d:
                        best[po["eng"]] = d
                else:
                    red.add(d)
            red.update(best.values())
            o["deps"] = red
        needed = [False] * len(ops)
        for o in ops:
            for d in o["deps"]:
                if ops[d]["kind"] == "c":
                    needed[d] = True
        for d in final_waits:
            if ops[d]["kind"] == "c":
                needed[d] = True
        csem = {e: self.es.enter_context(nc.semaphore("cs_" + e)) for e in self.ENG}
        dsem = [self.es.enter_context(nc.semaphore("ds%d" % i)) for i in range(self.n_dma_sems)]
        ccnt = {e: 0 for e in self.ENG}
        dcnt = [0] * self.n_dma_sems
        ev = [None] * len(ops)
        known = {e: {} for e in self.ENG}
        nd = 0
        nwaits = 0

        def wait(e, key, val):
            nonlocal nwaits
            if known[e].get(key, 0) >= val:
                return
            sem = csem[key] if isinstance(key, str) else dsem[key]
            engs[e].wait_ge(sem, val)
            known[e][key] = val
            nwaits += 1

        for i, o in enumerate(ops):
            e = o["eng"]
            for d in sorted(o["deps"]):
                key, val = ev[d]
                wait(e, key, val)
            if o["kind"] == "c":
                ins = o["fn"](engs[e])
                if needed[i]:
                    ccnt[e] += 1
                    ins.then_inc(csem[e], 1)
                    ev[i] = (e, ccnt[e])
            else:
                k = nd % self.n_dma_sems
                nd += 1
                wait(e, k, dcnt[k])
                ins = o["fn"](engs[e])
                dcnt[k] += 16
                ins.then_inc(dsem[k], 16)
                ev[i] = (k, dcnt[k])
        for d in final_waits:
            key, val = ev[d]
            wait("sp", key, val)
        self.stats = dict(n_ops=len(ops), ccnt=dict(ccnt), nwaits=nwaits)


class T:
    def __init__(self, t, name):
        self.t = t
        self.b = Buf(name)

    def __getitem__(self, k):
        return self.t[k]


class Prog:
    def __init__(self, name="k"):
        self.nc = bass.Bass("TRN2", target_bir_lowering=False)
        self.es = ExitStack()
        self.S = Sched(self.nc, self.es)
        self.outs = []
        self.n = 0
        self.stack = [self.es]

    def push(self):
        self.stack.append(ExitStack())

    def pop(self):
        self.S.barrier()
        self.stack.pop().close()

    def din(self, name, shape, dt=F32):
        return self.nc.dram_tensor(name, list(shape), dt, kind="ExternalInput").ap()

    def dout(self, name, shape, dt=F32):
        return self.nc.dram_tensor(name, list(shape), dt, kind="ExternalOutput").ap()

    def dscratch(self, name, shape, dt=F32):
        return T(self.nc.dram_tensor(name, list(shape), dt, kind="Internal").ap(), name)

    def sb(self, name, shape, dt=F32):
        self.n += 1
        return T(self.stack[-1].enter_context(self.nc.sbuf_tensor("%s_%d" % (name, self.n), list(shape), dt)), name)

    def ps(self, name, shape=(128, 512), dt=F32):
        self.n += 1
        return T(self.stack[-1].enter_context(self.nc.psum_tensor("%s_%d" % (name, self.n), list(shape), dt)), name)

    def finish(self):
        self.S.emit(final_waits=self.outs)
        self.es.close()
        return self.nc


def run(prog_nc, in_maps):
    import time
    t0 = time.time()
    res = run_bass_kernel_spmd(prog_nc, in_maps, core_ids=list(range(NCORES)))
    print("[run] launch %.1fs" % (time.time() - t0), flush=True)
    return res.results


def fm(vec, nchunk):
    return np.ascontiguousarray(np.asarray(vec).reshape(nchunk, 128).T)


def build_k0():
    P = Prog()
    S = P.S
    ccT = P.din("ccT", [128, 32])
    w = P.din("w", [DEPTH, 128, 16, 1536])
    bT = P.din("bT", [128, DEPTH * 12])
    out = P.dout("modT", [128, DEPTH * 12 * 2])
    cc = P.sb("cc", [128, 32])
    sl = P.sb("sl", [128, 32])
    bt = P.sb("bt", [128, DEPTH * 12])
    res = P.sb("res", [128, DEPTH * 12 * 2])
    S.dma("sp", cc[:], ccT, writes=[cc.b])
    S.dma("sp", bt[:], bT, writes=[bt.b])
    S.op("act", lambda e: e.activation(out=sl[:], in_=cc[:], func=AF.Silu), reads=[cc.b], writes=[sl.b])
    wts = []
    for l in range(DEPTH):
        for kg in range(4):
            wt = P.sb("w", [128, 4, 1536])
            S.dma("sp", wt[:], w[l, :, kg * 4:(kg + 1) * 4, :], writes=[wt.b])
            wts.append(wt)
    pss = [P.ps("ps", [128, 2]) for _ in range(4)]
    n = 0
    for l in range(DEPTH):
        for j in range(12):
            ps = pss[n % 4]
            for k in range(16):
                wt = wts[l * 4 + k // 4]
                S.op("pe", lambda e, wt=wt, k=k, j=j, ps=ps: e.matmul(
                    ps[:], wt[:, k % 4, j * 128:(j + 1) * 128], sl[:, 2 * k:2 * k + 2],
                    start=(k == 0), stop=(k == 15)), reads=[wt.b, sl.b], writes=[ps.b])
            idx = l * 12 + j
            S.op("dve", lambda e, ps=ps, idx=idx: e.tensor_scalar(
                res[:, 2 * idx:2 * idx + 2], ps[:], bt[:, idx:idx + 1], None, ALU.add),
                reads=[ps.b, bt.b], writes=[res.b])
            n += 1
    P.outs.append(S.dma("sp", out, res[:], reads=[res.b]))
    return P.finish()


def run_k0(inp):
    cc = np.stack([inp["c"][0], inp["c_ctx"]], axis=-1)
    ccT = np.ascontiguousarray(cc.reshape(16, 128, 2).transpose(1, 0, 2).reshape(128, 32))
    in_maps = []
    for i in range(NCORES):
        w = inp["w_ada"][:, :, i * 1536:(i + 1) * 1536]
        w = np.ascontiguousarray(w.reshape(DEPTH, 16, 128, 1536).transpose(0, 2, 1, 3))
        b = inp["b_ada"][:, i * 1536:(i + 1) * 1536].reshape(DEPTH, 12, 128)
        bT = np.ascontiguousarray(b.transpose(2, 0, 1).reshape(128, DEPTH * 12))
        in_maps.append({"ccT": ccT, "w": w, "bT": bT})
    res = run(build_k0(), in_maps)
    modT = np.zeros((DEPTH, 128, 96, 2), np.float32)
    for i in range(NCORES):
        r = res[i]["modT"].reshape(128, DEPTH, 12, 2)
        for l in range(DEPTH):
            modT[l, :, i * 12:(i + 1) * 12, :] = r[:, l]
    return modT


NT1 = 1280
GROUPS1 = [(0, 256), (256, 768), (768, 1280)]


def rope_tables(core):
    t = core * 1024 + np.arange(1024)
    row = (t // 64).astype(np.float32)
    col = (t % 64).astype(np.float32)
    inv = (10000.0 ** (-np.arange(0, 32, 2, dtype=np.float32) / 32)).astype(np.float32)
    cosT = np.zeros((128, 1024), np.float32)
    sinT = np.zeros((128, 1024), np.float32)
    prot = np.zeros((128, 128), np.float32)
    for p in range(128):
        d = p % 64
        pos = row if d < 32 else col
        e = d % 32
        ang = (pos * inv[e % 16]).astype(np.float32)
        cosT[p] = np.cos(ang)
        sinT[p] = -np.sin(ang) if e < 16 else np.sin(ang)
        partner = p + 16 if e < 16 else p - 16
        prot[partner, p] = 1.0
    return cosT, sinT, prot


def rms_rstd(S, P, xt, junk, ss, rstd, width=D):
    S.op("act", lambda e: e.activation(out=junk[:], in_=xt[:], func=AF.Square, accum_out=ss[:]),
         reads=[xt.b], writes=[junk.b, ss.b])
    S.op("dve", lambda e: e.tensor_scalar(rstd[:], ss[:], 1.0 / width, EPS, ALU.mult, ALU.add),
         reads=[ss.b], writes=[rstd.b])
    S.op("act", lambda e: e.activation(out=rstd[:], in_=rstd[:], func=AF.Sqrt),
         reads=[rstd.b], writes=[rstd.b])
    S.op("dve", lambda e: e.reciprocal(rstd[:], rstd[:]),
         reads=[rstd.b], writes=[rstd.b])


def build_k1():
    P = Prog()
    S = P.S
    xin = P.din("xin", [NT1, D])
    modT = P.din("modT", [128, 96 * 2])
    ng0T = P.din("ng0T", [128, 16])
    w_in = P.din("w_in", [D, IN_COLS])
    cosd = P.din("cosT", [128, 1024])
    sind = P.din("sinT", [128, 1024])
    protd = P.din("prot", [128, 128])
    bgd = P.din("bgT", [128, 48])
    identd = P.din("ident", [128, 128], BF16)
    qkT = P.dout("qkT", [2048, NT1], BF16)
    vout = P.dout("v", [NT1, 1024], BF16)
    restT = P.dout("restT", [IN_COLS - 3072, NT1])

    mod = P.sb("mod", [128, 192]); ng0 = P.sb("ng0", [128, 16])
    cos = P.sb("cos", [128, 1024]); sin = P.sb("sin", [128, 1024])
    prot = P.sb("prot", [128, 128]); bg = P.sb("bg", [128, 48]); ident = P.sb("ident", [128, 128], BF16)
    for t_, d_ in ((mod, modT), (ng0, ng0T), (cos, cosd), (sin, sind), (prot, protd), (bg, bgd), (ident, identd)):
        S.dma("sp", t_[:], d_, writes=[t_.b])
    s1 = P.sb("s1", [128, 32])
    sh1 = P.sb("sh1", [128, 32])
    modv = mod.t[:].rearrange("p (c j) -> p c j", j=2)
    for j in range(2):
        S.op("dve", lambda e, j=j: e.tensor_scalar(s1[:, j * 16:(j + 1) * 16], modv[:, 16:32, j], 1.0, None, ALU.add),
             reads=[mod.b], writes=[s1.b])
        S.op("dve", lambda e, j=j: e.tensor_tensor(s1[:, j * 16:(j + 1) * 16], s1[:, j * 16:(j + 1) * 16], ng0[:], ALU.mult),
             reads=[s1.b, ng0.b], writes=[s1.b])
        S.op("dve", lambda e, j=j: e.tensor_copy(sh1[:, j * 16:(j + 1) * 16], modv[:, 0:16, j]),
             reads=[mod.b], writes=[sh1.b])

    hT = P.sb("hT", [128, 16, NT1], BF16)
    hTb = [Buf("hT%d" % g, multi=True) for g in range(3)]
    grp_of_tile = lambda tt: 0 if tt < 2 else (1 if tt < 6 else 2)
    xts = [P.sb("xt", [128, D]) for _ in range(2)]
    junk = P.sb("junk", [128, D])
    xns = [P.sb("xn", [128, D], BF16) for _ in range(2)]
    sss = [P.sb("ss", [128, 1]) for _ in range(2)]
    rstds = [P.sb("rstd", [128, 1]) for _ in range(2)]
    tps = [P.ps("tp", [128, 512], BF16) for _ in range(2)]
    ntp = 0
    for tt in range(NT1 // 128):
        xt, xn, ss, rstd = xts[tt % 2], xns[tt % 2], sss[tt % 2], rstds[tt % 2]
        j = 1 if tt < 2 else 0
        S.dma("sp", xt[:], xin[tt * 128:(tt + 1) * 128, :], writes=[xt.b])
        rms_rstd(S, P, xt, junk, ss, rstd)
        S.op("act", lambda e, xt=xt, xn=xn, rstd=rstd: e.activation(out=xn[:], in_=xt[:], func=AF.Copy, scale=rstd[:, 0:1]),
             reads=[xt.b, rstd.b], writes=[xn.b])
        for k4 in range(4):
            tp = tps[ntp % 2]; ntp += 1
            for kk in range(4):
                k = k4 * 4 + kk
                S.op("pe", lambda e, tp=tp, kk=kk, k=k, xn=xn: e.transpose(tp[:, kk * 128:(kk + 1) * 128], xn[:, k * 128:(k + 1) * 128], ident[:]),
                     reads=[xn.b, ident.b], writes=[tp.b])
            for kk in range(4):
                k = k4 * 4 + kk
                S.op("dve", lambda e, tp=tp, kk=kk, k=k, j=j, tt=tt: e.tensor_scalar(
                    hT[:, k, tt * 128:(tt + 1) * 128], tp[:, kk * 128:(kk + 1) * 128],
                    s1[:, j * 16 + k:j * 16 + k + 1], sh1[:, j * 16 + k:j * 16 + k + 1], ALU.mult, ALU.add),
                    reads=[tp.b, s1.b, sh1.b], writes=[hTb[grp_of_tile(tt)]])

    wv = w_in.rearrange("(k p) c -> p k c", p=128)
    wts = [P.sb("wt", [128, 16, 512], BF16) for _ in range(3)]
    accs = [P.ps("acc") for _ in range(4)]
    rots = [P.ps("rot") for _ in range(2)]
    stg = [P.sb("stg", [128, 512]) for _ in range(4)]
    stgb = [P.sb("stgb", [128, 512], BF16) for _ in range(3)]
    tmp1 = [P.sb("tmp1", [128, 512]) for _ in range(2)]
    na = 0; ns = 0; nsb = 0; nr = 0
    for cb in range(IN_COLS // 512):
        wt = wts[cb % 3]
        S.dma("pool", wt[:], wv[:, :, cb * 512:(cb + 1) * 512], writes=[wt.b])
        if cb in (4, 5):
            for tt in range(NT1 // 128):
                acc = accs[na % 4]; na += 1
                for k in range(16):
                    S.op("pe", lambda e, acc=acc, k=k, tt=tt, wt=wt: e.matmul(
                        acc[:], hT[:, k, tt * 128:(tt + 1) * 128], wt[:, k, :], start=(k == 0), stop=(k == 15)),
                        reads=[hTb[grp_of_tile(tt)], wt.b], writes=[acc.b])
                sb_ = stgb[nsb % 3]; nsb += 1
                S.op("act", lambda e, acc=acc, sb_=sb_: e.activation(out=sb_[:], in_=acc[:], func=AF.Copy),
                     reads=[acc.b], writes=[sb_.b])
                P.outs.append(S.dma("sp", vout[tt * 128:(tt + 1) * 128, (cb - 4) * 512:(cb - 3) * 512], sb_[:], reads=[sb_.b]))
            continue
        for c4 in range(4):
            ch = cb * 4 + c4
            for g, (g0, g1) in enumerate(GROUPS1):
                n = g1 - g0
                acc = accs[na % 4]; na += 1
                for k in range(16):
                    S.op("pe", lambda e, acc=acc, k=k, c4=c4, wt=wt, g0=g0, g1=g1, n=n: e.matmul(
                        acc[:, 0:n], wt[:, k, c4 * 128:(c4 + 1) * 128], hT[:, k, g0:g1], start=(k == 0), stop=(k == 15)),
                        reads=[hTb[g], wt.b], writes=[acc.b])
                if ch < 16:
                    sb_ = stgb[nsb % 3]; nsb += 1
                    if g == 0:
                        S.op("act", lambda e, acc=acc, sb_=sb_, n=n: e.activation(out=sb_[:, 0:n], in_=acc[:, 0:n], func=AF.Copy),
                             reads=[acc.b], writes=[sb_.b])
                    else:
                        a = stg[ns % 4]; ns += 1
                        S.op("act", lambda e, acc=acc, a=a, n=n: e.activation(out=a[:, 0:n], in_=acc[:, 0:n], func=AF.Copy),
                             reads=[acc.b], writes=[a.b])
                        rot = rots[nr % 2]; t1 = tmp1[nr % 2]; nr += 1
                        S.op("pe", lambda e, rot=rot, a=a, n=n: e.matmul(rot[:, 0:n], prot[:], a[:, 0:n], start=True, stop=True),
                             reads=[prot.b, a.b], writes=[rot.b])
                        c0 = g0 - 256
                        S.op("dve", lambda e, t1=t1, a=a, n=n, c0=c0: e.tensor_tensor(t1[:, 0:n], a[:, 0:n], cos[:, c0:c0 + n], ALU.mult),
                             reads=[a.b, cos.b], writes=[t1.b])
                        S.op("dve", lambda e, rot=rot, a=a, n=n, c0=c0: e.tensor_tensor(a[:, 0:n], rot[:, 0:n], sin[:, c0:c0 + n], ALU.mult),
                             reads=[rot.b, sin.b], writes=[a.b])
                        S.op("dve", lambda e, t1=t1, a=a, sb_=sb_, n=n: e.tensor_tensor(sb_[:, 0:n], t1[:, 0:n], a[:, 0:n], ALU.add),
                             reads=[t1.b, a.b], writes=[sb_.b])
                    P.outs.append(S.dma("sp", qkT[ch * 128:(ch + 1) * 128, g0:g1], sb_[:, 0:n], reads=[sb_.b]))
                else:
                    a = stg[ns % 4]; ns += 1
                    if ch < 64:
                        S.op("act", lambda e, acc=acc, a=a, n=n: e.activation(out=a[:, 0:n], in_=acc[:, 0:n], func=AF.Copy),
                             reads=[acc.b], writes=[a.b])
                    else:
                        gi = ch - 64
                        S.op("act", lambda e, acc=acc, a=a, n=n, gi=gi: e.activation(
                            out=a[:, 0:n], in_=acc[:, 0:n], func=AF.Sigmoid, bias=bg[:, gi:gi + 1]),
                            reads=[acc.b, bg.b], writes=[a.b])
                    r0 = (ch - 24) * 128
                    P.outs.append(S.dma("sp", restT[r0:r0 + 128, g0:g1], a[:, 0:n], reads=[a.b]))
    return P.finish()


def run_k1(inp, l, x, ctx, modT):
    ident = np.eye(128, dtype=np.float32).astype(ml_dtypes.bfloat16)
    in_maps = []
    for i in range(NCORES):
        cosT, sinT, prot = rope_tables(i)
        in_maps.append({
            "xin": np.ascontiguousarray(np.concatenate([ctx, x[i * 1024:(i + 1) * 1024]], axis=0)),
            "modT": np.ascontiguousarray(modT[l].reshape(128, 192)),
            "ng0T": fm(inp["norm_g"][l, 0], 16),
            "w_in": inp["w_in"][l],
            "cosT": cosT, "sinT": sinT, "prot": prot,
            "bgT": fm(inp["b_gate"][l], 48),
            "ident": ident,
        })
    return run(build_k1(), in_maps)


NTOK = LC + SEQ
NKT = NTOK // 128


def build_k2():
    P = Prog()
    S = P.S
    qd = P.din("qT2", [128, NTOK], BF16)
    kd = P.din("kT2", [128, NTOK], BF16)
    vd = P.din("vh", [NTOK, 128], BF16)
    lamd = P.din("lamp", [1, 256])
    lcd = P.din("lamc", [128, 2])
    gd = P.din("subg", [1, 128])
    identd = P.din("ident", [128, 128], BF16)
    aout = P.dout("aT", [128, NTOK], BF16)

    q = P.sb("q", [128, NTOK], BF16); k = P.sb("k", [128, NTOK], BF16)
    v = P.sb("v", [128, NKT, 129], BF16)
    lamp = P.sb("lamp", [128, 256]); lamc = P.sb("lamc", [128, 2]); g = P.sb("g", [128, 128])
    ident = P.sb("ident", [128, 128], BF16)
    S.dma("sp", q[:], qd, writes=[q.b])
    S.dma("sp", k[:], kd, writes=[k.b])
    S.op("pool", lambda e: e.memset(v[:, :, 128:129], 1.0), writes=[v.b])
    S.dma("sp", v[:, :, 0:128], vd.rearrange("(t p) d -> p t d", p=128), writes=[v.b])
    S.dma("sp", lamp[:], lamd.partition_broadcast(128), writes=[lamp.b])
    S.dma("sp", g[:], gd.partition_broadcast(128), writes=[g.b])
    S.dma("sp", lamc[:], lcd, writes=[lamc.b])
    S.dma("sp", ident[:], identd, writes=[ident.b])
    pr = P.sb("pr", [128, 128]); sm = P.sb("sm", [128, 2]); nlam = P.sb("nlam", [128, 1])
    S.op("dve", lambda e: e.tensor_tensor(pr[:, 0:64], lamp[:, 0:64], lamp[:, 64:128], ALU.mult), reads=[lamp.b], writes=[pr.b])
    S.op("dve", lambda e: e.tensor_tensor(pr[:, 64:128], lamp[:, 128:192], lamp[:, 192:256], ALU.mult), reads=[lamp.b, pr.b], writes=[pr.b])
    S.op("dve", lambda e: e.tensor_reduce(sm[:], pr[:].rearrange("p (a b) -> p a b", a=2), AX.X, ALU.add), reads=[pr.b], writes=[sm.b])
    S.op("act", lambda e: e.activation(out=sm[:], in_=sm[:], func=AF.Exp), reads=[sm.b], writes=[sm.b])
    S.op("dve", lambda e: e.tensor_tensor(nlam[:], sm[:, 1:2], sm[:, 0:1], ALU.subtract), reads=[sm.b], writes=[nlam.b])
    S.op("dve", lambda e: e.tensor_tensor(nlam[:], nlam[:], lamc[:, 0:1], ALU.subtract), reads=[nlam.b, lamc.b], writes=[nlam.b])
    S.op("dve", lambda e: e.tensor_scalar(g[:], g[:], lamc[:, 1:2], None, ALU.mult), reads=[g.b, lamc.b], writes=[g.b])

    sps = [P.ps("s") for _ in range(2)]
    ops_ = [[P.ps("o", [128, 512]) for _ in range(2)] for _ in range(2)]
    tps = P.ps("tp", [128, 512], BF16)
    pts = [P.sb("pT", [128, 512], BF16) for _ in range(3)]
    o1 = P.sb("o1", [128, 128]); dif = P.sb("dif", [128, 128]); junk = P.sb("junk", [128, 128])
    rs = P.sb("rs", [128, 2]); ss = P.sb("ss", [128, 1]); rstd = P.sb("rstd", [128, 1])
    ab = P.sb("ab", [128, 512], BF16); aTs = [P.sb("aTs", [128, 512], BF16) for _ in range(2)]
    nsp = 0; npt = 0
    groups = [(0, 256, 2)] + [(256 + 512 * gi, 256 + 512 * (gi + 1), NKT) for gi in range(SEQ // 512)]
    for gi, (q0, q1, nkt) in enumerate(groups):
        nq = q1 - q0
        nsub = nq // 128
        units = [(kt, j) for kt in range(nkt) for j in range(2)]
        bufs = []
        for _ in units:
            bufs.append((sps[nsp % 2], pts[npt % 3])); nsp += 1; npt += 1

        def qk(u):
            kt, j = units[u]; sp, pT = bufs[u]
            S.do("pe", [k.b, q.b], [sp.b], "matmul", sp[:, 0:nq], k[j * 64:(j + 1) * 64, kt * 128:(kt + 1) * 128],
                 q[j * 64:(j + 1) * 64, q0:q1], start=True, stop=True)
            S.do("act", [sp.b], [pT.b], "activation", out=pT[:, 0:nq], in_=sp[:, 0:nq], func=AF.Exp, scale=0.125)

        def pv(u):
            kt, j = units[u]; sp, pT = bufs[u]
            for qs in range(nsub):
                o_ = ops_[j][qs // 2]
                S.do("pe", [pT.b, v.b], [o_.b], "matmul", o_[:, (qs % 2) * 129:(qs % 2) * 129 + 129], pT[:, qs * 128:(qs + 1) * 128], v[:, kt, :],
                     start=(kt == 0 and qs % 2 == 0), stop=(kt == nkt - 1), skip_group_check=True)

        qk(0)
        for u in range(len(units)):
            if u + 1 < len(units):
                qk(u + 1)
            pv(u)
        for qs in range(nsub):
            oa = ops_[0][qs // 2]; ob = ops_[1][qs // 2]; h_ = qs % 2
            S.op("dve", lambda e, oa=oa, h_=h_: e.reciprocal(rs[:, 0:1], oa[:, h_ * 129 + 128:h_ * 129 + 129]), reads=[oa.b], writes=[rs.b])
            S.op("dve", lambda e, ob=ob, h_=h_: e.reciprocal(rs[:, 1:2], ob[:, h_ * 129 + 128:h_ * 129 + 129]), reads=[ob.b, rs.b], writes=[rs.b])
            S.op("dve", lambda e: e.tensor_tensor(rs[:, 1:2], rs[:, 1:2], nlam[:], ALU.mult), reads=[rs.b, nlam.b], writes=[rs.b])
            S.op("dve", lambda e, oa=oa, h_=h_: e.tensor_scalar(o1[:], oa[:, h_ * 129:h_ * 129 + 128], rs[:, 0:1], None, ALU.mult),
                 reads=[oa.b, rs.b], writes=[o1.b])
            S.op("dve", lambda e, ob=ob, h_=h_: e.scalar_tensor_tensor(dif[:], ob[:, h_ * 129:h_ * 129 + 128], rs[:, 1:2], o1[:], ALU.mult, ALU.add),
                 reads=[ob.b, rs.b, o1.b], writes=[dif.b])
            rms_rstd(S, P, dif, junk, ss, rstd, width=128)
            S.op("dve", lambda e: e.tensor_scalar(dif[:], dif[:], rstd[:, 0:1], None, ALU.mult), reads=[dif.b, rstd.b], writes=[dif.b])
            S.op("dve", lambda e, qs=qs: e.tensor_tensor(ab[:, qs * 128:(qs + 1) * 128], dif[:], g[:], ALU.mult),
                 reads=[dif.b, g.b], writes=[ab.b])
            S.op("pe", lambda e, qs=qs: e.transpose(tps[:, qs * 128:(qs + 1) * 128], ab[:, qs * 128:(qs + 1) * 128], ident[:]),
                 reads=[ab.b, ident.b], writes=[tps.b])
        aT = aTs[gi % 2]
        S.op("act", lambda e, aT=aT, nq=nq: e.activation(out=aT[:, 0:nq], in_=tps[:, 0:nq], func=AF.Copy), reads=[tps.b], writes=[aT.b])
        P.outs.append(S.dma("sp", aout[:, q0:q1], aT[:, 0:nq], reads=[aT.b]))
    return P.finish()


def lam_init_of(l):
    return 0.8 - 0.6 * math.exp(-0.3 * l)


def run_k2(inp, l, r1):
    ident = np.eye(128, dtype=np.float32).astype(ml_dtypes.bfloat16)
    qk = np.concatenate([r1[0]["qkT"][:, :LC]] + [r1[i]["qkT"][:, LC:] for i in range(NCORES)], axis=1)
    v = np.concatenate([r1[0]["v"][:LC]] + [r1[i]["v"][LC:] for i in range(NCORES)], axis=0)
    li = lam_init_of(l)
    lamc = np.tile(np.array([[li, 1.0 - li]], np.float32), (128, 1))
    in_maps = []
    for h in range(NCORES):
        in_maps.append({
            "qT2": np.ascontiguousarray(qk[h * 128:(h + 1) * 128]),
            "kT2": np.ascontiguousarray(qk[1024 + h * 128:1024 + (h + 1) * 128]),
            "vh": np.ascontiguousarray(v[:, h * 128:(h + 1) * 128]),
            "lamp": np.ascontiguousarray(inp["da_lambda"][l].reshape(1, 256)),
            "lamc": lamc,
            "subg": np.ascontiguousarray(inp["da_subln_g"][l].reshape(1, 128)),
            "ident": ident,
        })
    res = run(build_k2(), in_maps)
    return np.concatenate([res[h]["aT"] for h in range(NCORES)], axis=0)


HY_EMB = 33
TWO_PI = 2.0 * math.pi


def hyena_tables(n, core):
    m = np.arange(2 * n)
    pos = np.where(m < n, n - 1 - m, m - n)
    t = np.linspace(0.0, 1.0, n, dtype=np.float32)[pos]
    w = (2.0 * math.pi * pos.astype(np.float32) / n).astype(np.float32)
    bands = np.linspace(1e-4, 15, 16, dtype=np.float32)
    z = np.concatenate([t[None, :], np.cos(bands[:, None] * w[None, :]), -np.sin(bands[:, None] * w[None, :])], axis=0)
    lo = math.log(1e-2) / 1.5
    hi = math.log(1e-2) / 0.3
    deltas = np.abs(np.linspace(lo, hi, 1024, dtype=np.float32))[core * 128:(core + 1) * 128]
    dec = np.exp(-t[None, :] * deltas[:, None])
    return z.astype(np.float32), dec.astype(np.float32)


def build_k3(dbg=False):
    P = Prog()
    S = P.S
    N, NC_ = SEQ, LC
    ud = P.din("uT", [3, 128, NTOK])
    swd = P.din("swT", [128, 9]); sbd = P.din("sbT", [128, 3])
    zL = P.din("zL", [HY_EMB, 2 * N]); zC = P.din("zC", [HY_EMB, 2 * NC_])
    dL = P.din("dL", [128, 2 * N]); dC = P.din("dC", [128, 2 * NC_])
    w1d = P.din("w1", [HY_EMB, 64]); w2d = P.din("w2", [64, 64]); w3d = P.din("w3c", [64, 512])
    pd = P.din("mlp", [64, 3])
    hbd = P.din("hbT", [128, 2])
    identd = P.din("identF", [128, 128]); jmd = P.din("jm", [128, 128])
    out = P.dout("hyT", [128, NTOK])
    kdL = [P.dscratch("kdL%d" % o, [128, 2 * N], BF16) for o in range(2)]
    kdC = [P.dscratch("kdC%d" % o, [128, 2 * NC_], BF16) for o in range(2)]

    sw = P.sb("sw", [128, 9]); sbb = P.sb("sbb", [128, 3]); hb = P.sb("hb", [128, 2])
    ident = P.sb("ident", [128, 128]); jm = P.sb("jm", [128, 128])
    w1 = P.sb("w1", [HY_EMB, 64]); w2 = P.sb("w2", [64, 64]); w3 = P.sb("w3", [64, 512]); mp = P.sb("mp", [64, 3])
    for t_, d_ in ((sw, swd), (sbb, sbd), (hb, hbd), (ident, identd), (jm, jmd), (w1, w1d), (w2, w2d), (w3, w3d), (mp, pd)):
        S.dma("sp", t_[:], d_, writes=[t_.b])
    fb = P.sb("fb", [64, 2])
    S.op("dve", lambda e: e.tensor_scalar(fb[:], mp[:, 0:2], mp[:, 2:3], None, ALU.mult), reads=[mp.b], writes=[fb.b])

    def filters(n, zd, dd, kd):
        W = 2 * n
        P.push()
        kf = [P.sb("kf", [128, W]) for _ in range(2)]
        kfb = [Buf("kfb%d" % o, multi=True) for o in range(2)]
        nb = max(1, W // 512)
        bw = W // nb
        zts = [P.sb("zt", [HY_EMB, bw]) for _ in range(2)]
        dts = [P.sb("dt", [128, bw]) for _ in range(2)]
        hs = [[P.sb("h", [64, bw]) for _ in range(2)] for _ in range(2)]
        t1 = P.sb("t1", [64, bw]); t2 = P.sb("t2", [64, bw])
        pa = [P.ps("pa") for _ in range(2)]
        pb = [P.ps("pb") for _ in range(2)]

        def sin_layer(ps, h, li):
            S.op("dve", lambda e: e.tensor_scalar(h[:], ps[0:64, 0:bw], mp[:, 2:3], fb[:, li:li + 1], ALU.mult, ALU.add),
                 reads=[ps.b, mp.b, fb.b], writes=[h.b])
            S.op("dve", lambda e: e.tensor_scalar(t1[:], h[:], math.pi, -TWO_PI, ALU.is_gt, ALU.mult), reads=[h.b], writes=[t1.b])
            S.op("dve", lambda e: e.tensor_scalar(t2[:], h[:], -math.pi, TWO_PI, ALU.is_lt, ALU.mult), reads=[h.b], writes=[t2.b])
            S.op("dve", lambda e: e.tensor_tensor(t1[:], t1[:], t2[:], ALU.add), reads=[t1.b, t2.b], writes=[t1.b])
            S.op("dve", lambda e: e.tensor_tensor(h[:], h[:], t1[:], ALU.add), reads=[h.b, t1.b], writes=[h.b])
            S.op("act", lambda e: e.activation(out=h[:], in_=h[:], func=AF.Sin), reads=[h.b], writes=[h.b])

        for b in range(nb):
            zt = zts[b % 2]; dt_ = dts[b % 2]; c0 = b * bw
            S.dma("sp", zt[:], zd[:, c0:c0 + bw], writes=[zt.b])
            S.dma("sp", dt_[:], dd[:, c0:c0 + bw], writes=[dt_.b])
            p1 = pa[b % 2]; h1 = hs[0][b % 2]; h2 = hs[1][b % 2]
            S.op("pe", lambda e, p1=p1, zt=zt: e.matmul(p1[0:64, 0:bw], w1[:], zt[:], start=True, stop=True),
                 reads=[w1.b, zt.b], writes=[p1.b])
            sin_layer(p1, h1, 0)
            S.op("pe", lambda e, p1=p1, h1=h1: e.matmul(p1[0:64, 0:bw], w2[:], h1[:], start=True, stop=True),
                 reads=[w2.b, h1.b], writes=[p1.b])
            sin_layer(p1, h2, 1)
            for o in range(2):
                if bw <= n:
                    segs = [(0, bw, 0 if c0 < n else 1)]
                else:
                    segs = [(0, n, 0), (n, bw, 1)]
                p3 = pb[o]
                for (a0, a1, dr) in segs:
                    od = o * 2 + dr
                    S.op("pe", lambda e, p3=p3, h2=h2, od=od, a0=a0, a1=a1: e.matmul(
                        p3[:, a0:a1], w3[:, od * 128:(od + 1) * 128], h2[:, a0:a1], start=True, stop=True),
                        reads=[w3.b, h2.b], writes=[p3.b])
                S.op("dve", lambda e, p3=p3, o=o, dt_=dt_, c0=c0: e.tensor_tensor(kf[o][:, c0:c0 + bw], p3[:, 0:bw], dt_[:], ALU.mult),
                     reads=[p3.b, dt_.b], writes=[kfb[o]])
        cw = min(W, 2048)
        ncw = W // cw
        junk = P.sb("junk", [128, cw]); part = P.sb("part", [128, 8]); tot = P.sb("tot", [128, 1])
        obs = [P.sb("ob", [128, cw], BF16) for _ in range(2)]
        for o in range(2):
            for i in range(ncw):
                S.op("act", lambda e, o=o, i=i: e.activation(out=junk[:], in_=kf[o][:, i * cw:(i + 1) * cw], func=AF.Abs, accum_out=part[:, i:i + 1]),
                     reads=[kfb[o]], writes=[junk.b, part.b])
            S.op("dve", lambda e: e.tensor_reduce(tot[:], part[:, 0:ncw], AX.X, ALU.add), reads=[part.b], writes=[tot.b])
            S.op("dve", lambda e: e.reciprocal(tot[:], tot[:]), reads=[tot.b], writes=[tot.b])
            for i in range(ncw):
                ob = obs[i % 2]
                S.op("act", lambda e, o=o, i=i, ob=ob: e.activation(out=ob[:], in_=kf[o][:, i * cw:(i + 1) * cw], func=AF.Copy, scale=tot[:, 0:1]),
                     reads=[kfb[o], tot.b], writes=[ob.b])
                S.dma("sp", kd[o][:, i * cw:(i + 1) * cw], ob[:], reads=[ob.b], writes=[kd[o].b])
        P.pop()

    filters(N, zL, dL, kdL)
    filters(NC_, zC, dC, kdC)
    if dbg == 1:
        dbo = P.dout("dbg", [128, 2 * NC_], BF16)
        dbt = P.sb("dbt", [128, 2 * NC_], BF16)
        S.dma("sp", dbt[:], kdC[0][:], reads=[kdC[0].b], writes=[dbt.b])
        P.outs.append(S.dma("sp", dbo, dbt[:], reads=[dbt.b]))

    A = P.sb("A", [128, NTOK])
    B = P.sb("B", [128, NTOK])
    U = P.sb("U", [128, 2048])
    Zt = P.sb("Zt", [128, 128, 64], BF16)
    Yt = P.sb("Yt", [128, 64, 128])
    Ytb = Buf("Ytb", multi=True)
    Gs = [P.sb("G", [128, 2 * N - 128], BF16) for _ in range(2)]
    tmp = P.sb("tmp", [128, 512])
    tpi = [P.ps("tpi") for _ in range(2)]
    yb = [P.ps("yb") for _ in range(2)]
    tpo = [P.ps("tpo") for _ in range(2)]
    SEGS = [(0, NC_), (NC_, NTOK)]

    def short_conv(which, dst):
        CH = 2046
        for (s0, s1) in SEGS:
            c = s0
            while c < s1:
                e_ = min(c + CH, s1)
                lo = max(c - 1, s0); hi = min(e_ + 1, s1)
                nl = hi - lo
                S.dma("sp", U[:, 0:nl], ud[which, :, lo:hi], writes=[U.b])
                off = c - lo
                nn = e_ - c
                S.op("dve", lambda e, off=off, nn=nn, c=c: e.tensor_scalar(
                    dst[:, c:c + nn], U[:, off:off + nn], sw[:, which * 3 + 1:which * 3 + 2], sbb[:, which:which + 1], ALU.mult, ALU.add),
                    reads=[U.b, sw.b, sbb.b], writes=[dst.b])
                tl = c if off == 1 else c + 1
                S.op("dve", lambda e, tl=tl, e_=e_, lo=lo: e.scalar_tensor_tensor(
                    dst[:, tl:e_], U[:, tl - 1 - lo:e_ - 1 - lo], sw[:, which * 3:which * 3 + 1], dst[:, tl:e_], ALU.mult, ALU.add),
                    reads=[U.b, sw.b, dst.b], writes=[dst.b])
                tr = e_ if hi == e_ + 1 else e_ - 1
                S.op("dve", lambda e, c=c, tr=tr, lo=lo: e.scalar_tensor_tensor(
                    dst[:, c:tr], U[:, c + 1 - lo:tr + 1 - lo], sw[:, which * 3 + 2:which * 3 + 3], dst[:, c:tr], ALU.mult, ALU.add),
                    reads=[U.b, sw.b, dst.b], writes=[dst.b])
                c = e_

    ng = [0]

    def long_conv(o, X, gate, order=(1, 0), dbg_stop=False):
        for si in order:
            (s0, s1), kd = SEGS[si], (kdC[o], kdL[o])[si]
            n = s1 - s0
            nbk = n // 128
            for J0 in range(0, nbk, 4):
                nj = min(4, nbk - J0)
                tp = tpi[(J0 // 4) % 2]
                for jj in range(nj):
                    J = J0 + jj
                    S.op("pe", lambda e, tp=tp, jj=jj, J=J, s0=s0: e.transpose(tp[:, jj * 128:(jj + 1) * 128], X[:, s0 + J * 128:s0 + (J + 1) * 128], ident[:]),
                         reads=[X.b, ident.b], writes=[tp.b])
                eng = "act" if (J0 // 4) % 2 else "dve"
                src = tp[:, 0:nj * 128].rearrange("p (j c) -> p j c", j=nj)
                dst = Zt[:, :, J0:J0 + nj].rearrange("p c j -> p j c")
                if eng == "act":
                    S.op("act", lambda e, src=src, dst=dst: e.activation(out=dst, in_=src, func=AF.Copy), reads=[tp.b], writes=[Zt.b])
                else:
                    S.op("dve", lambda e, src=src, dst=dst: e.tensor_copy(dst, src), reads=[tp.b], writes=[Zt.b])
            for c in range(128):
                G = Gs[ng[0] % 2]; ng[0] += 1
                gw = 2 * n - 128
                S.dma("sp", G[:, 0:gw], bass.AP(kd.t.tensor, c * 2 * n, [[1, 128], [1, gw]]), reads=[kd.b], writes=[G.b])
                ybk = yb[(c // 8) % 2]
                a0 = (c % 8) * 64
                deltas = [0] + [d for d in range(-(nbk - 1), nbk) if d != 0]
                for di, dl in enumerate(deltas):
                    if dl >= 0:
                        i0, i1, j0, j1 = dl, nbk, 0, nbk - dl
                    else:
                        i0, i1, j0, j1 = 0, nbk + dl, -dl, nbk
                    S.op("pe", lambda e, ybk=ybk, a0=a0, i0=i0, i1=i1, j0=j0, j1=j1, G=G, n=n, dl=dl, c=c, di=di, nd=len(deltas): e.matmul(
                        ybk[:, a0 + i0:a0 + i1], G[:, n - 128 - 128 * dl:n - 128 * dl], Zt[:, c, j0:j1],
                        start=(di == 0), stop=(di == nd - 1), skip_group_check=True),
                        reads=[G.b, Zt.b], writes=[ybk.b])
                if c % 8 == 7:
                    c0 = c - 7
                    src = ybk[:, 0:512].rearrange("p (c i) -> p c i", c=8)[:, :, 0:nbk]
                    dst = Yt[:, 0:nbk, c0:c0 + 8].rearrange("p i c -> p c i")
                    if (c // 8) % 2:
                        S.op("act", lambda e, src=src, dst=dst: e.activation(out=dst, in_=src, func=AF.Copy), reads=[ybk.b], writes=[Ytb])
                    else:
                        S.op("dve", lambda e, src=src, dst=dst: e.tensor_copy(dst, src), reads=[ybk.b], writes=[Ytb])
            if dbg_stop:
                d1 = P.dout("dbgZ", [128, 128 * 64], BF16); d2 = P.dout("dbgY", [128, 64 * 128])
                P.outs.append(S.dma("sp", d1, Zt[:].rearrange("p c j -> p (c j)"), reads=[Zt.b]))
                P.outs.append(S.dma("sp", d2, Yt[:].rearrange("p i c -> p (i c)"), reads=[Ytb]))
                return
            for I0 in range(0, nbk, 4):
                ni = min(4, nbk - I0)
                tp = tpo[(I0 // 4) % 2]
                for ii in range(ni):
                    S.op("pe", lambda e, tp=tp, ii=ii, I0=I0: e.matmul(tp[:, ii * 128:(ii + 1) * 128], Yt[:, I0 + ii, :], jm[:], start=True, stop=True),
                         reads=[Ytb, jm.b], writes=[tp.b])
                t0 = s0 + I0 * 128; wd = ni * 128
                S.op("dve", lambda e, tp=tp, t0=t0, wd=wd: e.scalar_tensor_tensor(
                    tmp[:, 0:wd], X[:, t0:t0 + wd], hb[:, o:o + 1], tp[:, 0:wd], ALU.mult, ALU.add),
                    reads=[X.b, hb.b, tp.b], writes=[tmp.b])
                S.op("dve", lambda e, t0=t0, wd=wd: e.tensor_tensor(X[:, t0:t0 + wd], tmp[:, 0:wd], gate[:, t0:t0 + wd], ALU.mult),
                     reads=[tmp.b, gate.b, X.b], writes=[X.b])

    short_conv(2, A)
    short_conv(0, B)
    if dbg == 4:
        for c in range(3):
            G = Gs[c % 2]
            S.dma("sp", G[:, 0:2 * N - 128], bass.AP(kdL[0].t.tensor, c * 2 * N, [[1, 128], [1, 2 * N - 128]]), reads=[kdL[0].b], writes=[G.b])
            dbo = P.dout("dbgG%d" % c, [128, 2 * N - 128], BF16)
            P.outs.append(S.dma("sp", dbo, G[:, 0:2 * N - 128], reads=[G.b]))
        dbk = P.dout("dbgK", [128, 2 * N], BF16)
        P.outs.append(S.dma("sp", dbk, kdL[0][:], reads=[kdL[0].b]))
        return P.finish()
    if dbg == 2:
        dbo = P.dout("dbgA", [128, NTOK]); dbo2 = P.dout("dbgB", [128, NTOK])
        P.outs.append(S.dma("sp", dbo, A[:], reads=[A.b]))
        P.outs.append(S.dma("sp", dbo2, B[:], reads=[B.b]))
        return P.finish()
    if dbg == 5:
        long_conv(0, A, B, order=(0, 1), dbg_stop=True)
        return P.finish()
    long_conv(0, A, B)
    if dbg == 3:
        dbo = P.dout("dbgA", [128, NTOK])
        P.outs.append(S.dma("sp", dbo, A[:], reads=[A.b]))
        return P.finish()
    short_conv(1, B)
    long_conv(1, A, B)
    P.outs.append(S.dma("sp", out, A[:], reads=[A.b]))
    return P.finish()


def run_k3(inp, l, restT_full, dbg=False):
    identF = np.eye(128, dtype=np.float32)
    jm = np.ascontiguousarray(identF[::-1])
    in_maps = []
    for c in range(NCORES):
        zl, dl = hyena_tables(SEQ, c)
        zc, dc = hyena_tables(LC, c)
        u = np.stack([restT_full[j * 1024 + c * 128:j * 1024 + (c + 1) * 128] for j in range(3)], axis=0)
        sw = np.stack([inp["hy_short_w"][l][:, j * 1024 + c * 128:j * 1024 + (c + 1) * 128].T for j in range(3)], axis=1)
        sb_ = np.stack([inp["hy_short_b"][l][j * 1024 + c * 128:j * 1024 + (c + 1) * 128] for j in range(3)], axis=1)
        w3 = inp["hy_w3"][l].reshape(64, 2, 2, 1024)[:, :, :, c * 128:(c + 1) * 128].reshape(64, 512)
        mlp = np.stack([inp["hy_b1"][l], inp["hy_b2"][l], inp["hy_freq"][l]], axis=1)
        in_maps.append({
            "uT": np.ascontiguousarray(u), "swT": np.ascontiguousarray(sw.reshape(128, 9)), "sbT": np.ascontiguousarray(sb_),
            "zL": zl, "zC": zc, "dL": dl, "dC": dc,
            "w1": np.ascontiguousarray(inp["hy_w1"][l]), "w2": np.ascontiguousarray(inp["hy_w2"][l]), "w3c": np.ascontiguousarray(w3),
            "mlp": np.ascontiguousarray(mlp), "hbT": np.ascontiguousarray(inp["hy_bias"][l][:, c * 128:(c + 1) * 128].T),
            "identF": identF, "jm": jm,
        })
    res = run(build_k3(dbg), in_maps)
    if dbg:
        return res
    return np.concatenate([res[c]["hyT"] for c in range(NCORES)], axis=0)


CW4 = 1340


def build_k4():
    P = Prog()
    S = P.S
    gTd = P.din("gT", [6144, NT1])
    cvd = P.din("cvin", [2048, CW4])
    aTd = P.din("aT", [1024, NT1], BF16)
    hyd = P.din("hyT", [1024, NT1])
    xind = P.din("xin", [NT1, D])
    wbr = [P.din(n_, [1024, D]) for n_ in ("w_da", "w_hy", "w_cv")]
    woutd = P.din("w_out", [D, D])
    wrd = P.din("w_r", [D, 32]); brd = P.din("b_r", [1, 32])
    cvpd = P.din("cvp", [128, 8 * 34])
    modd = P.din("modT", [128, 192]); ng2d = P.din("ng2T", [128, 16])
    g1rd = P.din("g1row", [2, D]); ng1rd = P.din("ng1row", [1, D])
    identd = P.din("identF", [128, 128]); onesd = P.din("onesF", [128, 128])
    x1o = P.dout("x1", [NT1, D]); fTo = P.dout("fT", [D, NT1], BF16); wgo = P.dout("wg", [NT1, 32])

    ident = P.sb("ident", [128, 128]); ones = P.sb("ones", [128, 128])
    mod = P.sb("mod", [128, 192]); ng2 = P.sb("ng2", [128, 16]); cvp = P.sb("cvp", [128, 8, 34])
    S.dma("sp", ident[:], identd, writes=[ident.b]); S.dma("sp", ones[:], onesd, writes=[ones.b])
    S.dma("sp", mod[:], modd, writes=[mod.b]); S.dma("sp", ng2[:], ng2d, writes=[ng2.b])
    S.dma("sp", cvp[:].rearrange("p a b -> p (a b)"), cvpd, writes=[cvp.b])
    s2 = P.sb("s2", [128, 32]); sh2 = P.sb("sh2", [128, 32])
    modv = mod.t[:].rearrange("p (c j) -> p c j", j=2)
    for j in range(2):
        S.do("dve", [mod.b], [s2.b], "tensor_scalar", s2[:, j * 16:(j + 1) * 16], modv[:, 64:80, j], 1.0, None, ALU.add)
        S.do("dve", [s2.b, ng2.b], [s2.b], "tensor_tensor", s2[:, j * 16:(j + 1) * 16], s2[:, j * 16:(j + 1) * 16], ng2[:], ALU.mult)
        S.do("dve", [mod.b], [sh2.b], "tensor_copy", sh2[:, j * 16:(j + 1) * 16], modv[:, 48:64, j])

    ypT = P.sb("ypT", [128, 16, NT1], BF16)
    ypTb = [Buf("ypT%d" % g, multi=True) for g in range(3)]
    P.push()
    cvT = P.sb("cvT", [128, 8, NT1], BF16)
    cvTb = Buf("cvTb", multi=True)

    P.push()
    C = P.sb("C", [128, 8, NT1])
    Cb = [Buf("C%d" % ch) for ch in range(8)]
    ats = [P.sb("at", [128, CW4]) for _ in range(2)]
    gts = [P.sb("gt", [128, CW4]) for _ in range(2)]
    for ch in range(8):
        at = ats[ch % 2]; gt = gts[ch % 2]
        S.dma("sp", at[:], cvd[ch * 128:(ch + 1) * 128, :], writes=[at.b])
        S.dma("sp", gt[:], cvd[1024 + ch * 128:1024 + (ch + 1) * 128, :], writes=[gt.b])
        S.do("act", [gt.b], [gt.b], "activation", out=gt[:], in_=gt[:], func=AF.Sigmoid)
        S.do("dve", [at.b, gt.b], [at.b], "tensor_tensor", at[:], at[:], gt[:], ALU.mult)
        eng = "dve"
        for (o0, n, i0) in ((0, 256, 0), (256, 1024, 286)):
            acc = C[:, ch, o0:o0 + n]
            S.do(eng, [at.b, cvp.b], [Cb[ch]], "tensor_scalar", acc, at[:, i0:i0 + n], cvp[:, ch, 0:1], cvp[:, ch, 31:32], ALU.mult, ALU.add)
            for k in range(1, 31):
                S.do(eng, [at.b, cvp.b, Cb[ch]], [Cb[ch]], "scalar_tensor_tensor", acc, at[:, i0 + k:i0 + k + n], cvp[:, ch, k:k + 1], acc, ALU.mult, ALU.add)
    sps = P.ps("sps"); qps = P.ps("qps")
    sq = [P.sb("sq", [128, 512]) for _ in range(2)]
    mean = P.sb("mean", [128, 512]); rstd = P.sb("rstdc", [128, 512]); msq = P.sb("msq", [128, 512])
    tt_ = [P.sb("tt", [128, 512]) for _ in range(2)]
    for g, (g0, g1) in enumerate(GROUPS1):
        n = g1 - g0
        for ch in range(8):
            S.do("pe", [ones.b, Cb[ch]], [sps.b], "matmul", sps[:, 0:n], ones[:], C[:, ch, g0:g1], start=(ch == 0), stop=(ch == 7))
        for ch in range(8):
            q_ = sq[ch % 2]
            S.do("act", [Cb[ch]], [q_.b], "activation", out=q_[:, 0:n], in_=C[:, ch, g0:g1], func=AF.Square)
            S.do("pe", [ones.b, q_.b], [qps.b], "matmul", qps[:, 0:n], ones[:], q_[:, 0:n], start=(ch == 0), stop=(ch == 7))
        S.do("dve", [sps.b], [mean.b], "tensor_scalar", mean[:, 0:n], sps[:, 0:n], 1.0 / 1024, None, ALU.mult)
        S.do("dve", [mean.b], [msq.b], "tensor_tensor", msq[:, 0:n], mean[:, 0:n], mean[:, 0:n], ALU.mult)
        S.do("dve", [qps.b, msq.b], [rstd.b], "scalar_tensor_tensor", rstd[:, 0:n], qps[:, 0:n], 1.0 / 1024, msq[:, 0:n], ALU.mult, ALU.subtract)
        S.do("dve", [rstd.b], [rstd.b], "tensor_scalar", rstd[:, 0:n], rstd[:, 0:n], EPS, None, ALU.add)
        S.do("act", [rstd.b], [rstd.b], "activation", out=rstd[:, 0:n], in_=rstd[:, 0:n], func=AF.Sqrt)
        S.do("dve", [rstd.b], [rstd.b], "reciprocal", rstd[:, 0:n], rstd[:, 0:n])
        for ch in range(8):
            t_ = tt_[ch % 2]
            S.do("dve", [Cb[ch], mean.b], [t_.b], "tensor_tensor", t_[:, 0:n], C[:, ch, g0:g1], mean[:, 0:n], ALU.subtract)
            S.do("dve", [t_.b, rstd.b], [t_.b], "tensor_tensor", t_[:, 0:n], t_[:, 0:n], rstd[:, 0:n], ALU.mult)
            S.do("act", [t_.b, cvp.b], [cvTb], "activation", out=cvT[:, ch, g0:g1], in_=t_[:, 0:n], func=AF.Silu,
                 scale=cvp[:, ch, 32:33], bias=cvp[:, ch, 33:34])
    P.pop()

    P.push()
    aT = P.sb("aT", [128, 8, NT1], BF16); hyT = P.sb("hyT", [128, 8, NT1], BF16)
    S.dma("sp", aT[:], aTd.rearrange("(k p) t -> p k t", p=128), writes=[aT.b])
    S.dma("pool", hyT[:], hyd.rearrange("(k p) t -> p k t", p=128), writes=[hyT.b])
    acts = [(aT, aT.b), (hyT, hyT.b), (cvT, cvTb)]
    wbs = [[P.sb("wb", [128, 8, 512], BF16) for _ in range(3)] for _ in range(2)]
    gbs = [[P.sb("gb", [128, NT1]) for _ in range(3)] for _ in range(2)]
    pacc = [P.ps("pacc") for _ in range(6)]
    m1 = P.sb("m1", [128, 512]); m2 = P.sb("m2", [128, 512])
    npa = 0
    for cb in range(4):
        wset = wbs[cb % 2]
        for b in range(3):
            S.dma("pool", wset[b][:], wbr[b].rearrange("(k p) c -> p k c", p=128)[:, :, cb * 512:(cb + 1) * 512], writes=[wset[b].b])
        for c4 in range(4):
            j = cb * 4 + c4
            gset = gbs[j % 2]
            for b in range(3):
                S.dma("sp", gset[b][:], gTd[(b * 16 + j) * 128:(b * 16 + j + 1) * 128, :], writes=[gset[b].b])
            for g, (g0, g1) in enumerate(GROUPS1):
                n = g1 - g0
                pp = []
                for b in range(3):
                    pa_ = pacc[npa % 6]; npa += 1
                    at_, ab_ = acts[b]
                    for k in range(8):
                        S.do("pe", [wset[b].b, ab_], [pa_.b], "matmul", pa_[:, 0:n], wset[b][:, k, c4 * 128:(c4 + 1) * 128], at_[:, k, g0:g1],
                             start=(k == 0), stop=(k == 7))
                    pp.append(pa_)
                S.do("dve", [pp[0].b, gset[0].b], [m1.b], "tensor_tensor", m1[:, 0:n], pp[0][:, 0:n], gset[0][:, g0:g1], ALU.mult)
                S.do("dve", [pp[1].b, gset[1].b], [m2.b], "tensor_tensor", m2[:, 0:n], pp[1][:, 0:n], gset[1][:, g0:g1], ALU.mult)
                S.do("dve", [m1.b, m2.b], [m1.b], "tensor_tensor", m1[:, 0:n], m1[:, 0:n], m2[:, 0:n], ALU.add)
                S.do("dve", [pp[2].b, gset[2].b], [m2.b], "tensor_tensor", m2[:, 0:n], pp[2][:, 0:n], gset[2][:, g0:g1], ALU.mult)
                S.do("dve", [m1.b, m2.b], [ypTb[g]], "tensor_tensor", ypT[:, j, g0:g1], m1[:, 0:n], m2[:, 0:n], ALU.add)
    P.pop()
    P.pop()

    P.push()
    wout = P.sb("wout", [128, 16, D], BF16)
    woutb = [Buf("wout%d" % i) for i in range(4)]
    for cb in range(4):
        S.dma("pool", wout[:, :, cb * 512:(cb + 1) * 512], woutd.rearrange("(k p) c -> p k c", p=128)[:, :, cb * 512:(cb + 1) * 512], writes=[woutb[cb]])
    g1r = [P.sb("g1r", [128, D]) for _ in range(2)]
    ng1r = P.sb("ng1r", [128, D])
    S.dma("sp", ng1r[:], ng1rd.partition_broadcast(128), writes=[ng1r.b])
    for j in range(2):
        S.dma("sp", g1r[j][:], g1rd[j:j + 1, :].partition_broadcast(128), writes=[g1r[j].b])
        S.do("dve", [g1r[j].b, ng1r.b], [g1r[j].b], "tensor_tensor", g1r[j][:], g1r[j][:], ng1r[:], ALU.mult)
    wr = P.sb("wr", [128, 16, 32]); br = P.sb("br", [128, 32])
    S.dma("sp", wr[:], wrd.rearrange("(k p) e -> p k e", p=128), writes=[wr.b])
    S.dma("sp", br[:], brd.partition_broadcast(128), writes=[br.b])
    accs = [P.ps("acc") for _ in range(4)]
    tps = [P.ps("tp") for _ in range(2)]
    lps = P.ps("lps", [128, 32])
    xts = [P.sb("xt", [128, D]) for _ in range(2)]
    x1ts = [P.sb("x1t", [128, D]) for _ in range(2)]
    xn = P.sb("xn", [128, D]); junk = xn
    ssp = P.sb("ssp", [128, 4]); ss = P.sb("ss", [128, 1]); rstd1 = P.sb("rstd1", [128, 1]); rstd2 = P.sb("rstd2", [128, 1])
    t5 = P.sb("t5", [128, 512])
    f32T = [P.sb("f32T", [128, 16, 128]) for _ in range(2)]
    fbT = [P.sb("fbT", [128, 16, 128], BF16) for _ in range(2)]
    lg = P.sb("lg", [128, 32]); top8 = P.sb("top8", [128, 8]); msk = P.sb("msk", [128, 32]); ex = P.sb("ex", [128, 32])
    nmx = P.sb("nmx", [128, 1]); sm = P.sb("sm", [128, 1]); wgt = [P.sb("wgt", [128, 32]) for _ in range(2)]
    fTv = fTo.rearrange("(k p) t -> p k t", p=128)
    ntp = 0
    for tt in range(NT1 // 128):
        j = 1 if tt < 2 else 0
        g = 0 if tt < 2 else (1 if tt < 6 else 2)
        xt = xts[tt % 2]; x1t = x1ts[tt % 2]
        S.dma("sp", xt[:], xind[tt * 128:(tt + 1) * 128, :], writes=[xt.b])
        for cb in range(4):
            for k in range(16):
                S.do("pe", [ypTb[g], woutb[cb]], [accs[cb].b], "matmul", accs[cb][:], ypT[:, k, tt * 128:(tt + 1) * 128], wout[:, k, cb * 512:(cb + 1) * 512],
                     start=(k == 0), stop=(k == 15))
            S.do("act", [accs[cb].b], [junk.b, ssp.b], "activation", out=junk[:, 0:512], in_=accs[cb][:], func=AF.Square, accum_out=ssp[:, cb:cb + 1])
        S.do("dve", [ssp.b], [ss.b], "tensor_reduce", ss[:], ssp[:], AX.X, ALU.add)
        S.do("dve", [ss.b], [rstd1.b], "tensor_scalar", rstd1[:], ss[:], 1.0 / D, EPS, ALU.mult, ALU.add)
        S.do("act", [rstd1.b], [rstd1.b], "activation", out=rstd1[:], in_=rstd1[:], func=AF.Sqrt)
        S.do("dve", [rstd1.b], [rstd1.b], "reciprocal", rstd1[:], rstd1[:])
        for cb in range(4):
            S.do("dve", [accs[cb].b, rstd1.b, g1r[j].b], [t5.b], "scalar_tensor_tensor", t5[:], accs[cb][:], rstd1[:, 0:1], g1r[j][:, cb * 512:(cb + 1) * 512], ALU.mult, ALU.mult)
            S.do("dve", [t5.b, xt.b], [x1t.b], "tensor_tensor", x1t[:, cb * 512:(cb + 1) * 512], t5[:], xt[:, cb * 512:(cb + 1) * 512], ALU.add)
        P.outs.append(S.dma("sp", x1o[tt * 128:(tt + 1) * 128, :], x1t[:], reads=[x1t.b]))
        S.do("act", [x1t.b], [junk.b, ss.b], "activation", out=junk[:], in_=x1t[:], func=AF.Square, accum_out=ss[:])
        S.do("dve", [ss.b], [rstd2.b], "tensor_scalar", rstd2[:], ss[:], 1.0 / D, EPS, ALU.mult, ALU.add)
        S.do("act", [rstd2.b], [rstd2.b], "activation", out=rstd2[:], in_=rstd2[:], func=AF.Sqrt)
        S.do("dve", [rstd2.b], [rstd2.b], "reciprocal", rstd2[:], rstd2[:])
        S.do("act", [x1t.b, rstd2.b], [xn.b], "activation", out=xn[:], in_=x1t[:], func=AF.Copy, scale=rstd2[:, 0:1])
        f32 = f32T[tt % 2]; fb = fbT[tt % 2]
        for k4 in range(4):
            tp = tps[ntp % 2]; ntp += 1
            for kk in range(4):
                k = k4 * 4 + kk
                S.do("pe", [xn.b, ident.b], [tp.b], "transpose", tp[:, kk * 128:(kk + 1) * 128], xn[:, k * 128:(k + 1) * 128], ident[:])
            for kk in range(4):
                k = k4 * 4 + kk
                S.do("dve", [tp.b, s2.b, sh2.b], [f32.b], "tensor_scalar", f32[:, k, :], tp[:, kk * 128:(kk + 1) * 128],
                     s2[:, j * 16 + k:j * 16 + k + 1], sh2[:, j * 16 + k:j * 16 + k + 1], ALU.mult, ALU.add)
        S.do("act", [f32.b], [fb.b], "activation", out=fb[:], in_=f32[:], func=AF.Copy)
        P.outs.append(S.dma("sp", fTv[:, :, tt * 128:(tt + 1) * 128], fb[:], reads=[fb.b]))
        for k in range(16):
            S.do("pe", [f32.b, wr.b], [lps.b], "matmul", lps[:], f32[:, k, :], wr[:, k, :], start=(k == 0), stop=(k == 15))
        S.do("dve", [lps.b, br.b], [lg.b], "tensor_tensor", lg[:], lps[:], br[:], ALU.add)
        S.do("dve", [lg.b], [top8.b], "max", top8[:], lg[:])
        S.do("dve", [lg.b, top8.b], [msk.b], "tensor_scalar", msk[:], lg[:], top8[:, 3:4], None, ALU.is_ge)
        S.do("dve", [top8.b], [nmx.b], "tensor_scalar", nmx[:], top8[:, 0:1], -1.0, None, ALU.mult)
        S.do("act", [lg.b, nmx.b], [ex.b], "activation", out=ex[:], in_=lg[:], func=AF.Exp, bias=nmx[:, 0:1])
        S.do("dve", [ex.b, msk.b], [ex.b], "tensor_tensor", ex[:], ex[:], msk[:], ALU.mult)
        S.do("dve", [ex.b], [sm.b], "tensor_reduce", sm[:], ex[:], AX.X, ALU.add)
        S.do("dve", [sm.b], [sm.b], "reciprocal", sm[:], sm[:])
        w_ = wgt[tt % 2]
        S.do("dve", [ex.b, sm.b], [w_.b], "tensor_scalar", w_[:], ex[:], sm[:, 0:1], None, ALU.mult)
        P.outs.append(S.dma("sp", wgo[tt * 128:(tt + 1) * 128, :], w_[:], reads=[w_.b]))
    P.pop()
    return P.finish()


def mod_rows(modT_l):
    return np.ascontiguousarray(modT_l.transpose(2, 1, 0).reshape(2, 12288))


def run_k4(inp, l, x, ctx, modT, rest_full, aT_full, hyT_full):
    identF = np.eye(128, dtype=np.float32)
    onesF = np.ones((128, 128), np.float32)
    rows = mod_rows(modT[l])
    g1row = np.ascontiguousarray(rows[:, 2 * D:3 * D])
    cvp = np.zeros((128, 8, 34), np.float32)
    cvp[:, :, 0:31] = inp["cv_dw_w"][l].reshape(31, 8, 128).transpose(2, 1, 0)
    cvp[:, :, 31] = fm(inp["cv_dw_b"][l], 8)
    cvp[:, :, 32] = fm(inp["cv_ln_g"][l], 8)
    cvp[:, :, 33] = fm(inp["cv_ln_b"][l], 8)
    cvall = rest_full[3072:5120]
    z15 = np.zeros((2048, 15), np.float32)
    in_maps = []
    for i in range(NCORES):
        t0 = LC + i * 1024
        left = cvall[:, t0 - 15:t0] if i > 0 else z15
        right = cvall[:, t0 + 1024:t0 + 1039] if i < NCORES - 1 else z15
        cvin = np.concatenate([z15, cvall[:, :LC], z15, left, cvall[:, t0:t0 + 1024], right], axis=1)
        sel = lambda a: np.ascontiguousarray(np.concatenate([a[:, :LC], a[:, t0:t0 + 1024]], axis=1))
        in_maps.append({
            "gT": sel(rest_full[5120:]), "cvin": np.ascontiguousarray(cvin),
            "aT": sel(aT_full), "hyT": sel(hyT_full),
            "xin": np.ascontiguousarray(np.concatenate([ctx, x[i * 1024:(i + 1) * 1024]], axis=0)),
            "w_da": inp["w_da_out"][l], "w_hy": inp["w_hy_out"][l], "w_cv": inp["w_cv_out"][l], "w_out": inp["w_out"][l],
            "w_r": inp["w_router"][l], "b_r": np.ascontiguousarray(inp["b_router"][l].reshape(1, 32)),
            "cvp": np.ascontiguousarray(cvp.reshape(128, 8 * 34)),
            "modT": np.ascontiguousarray(modT[l].reshape(128, 192)), "ng2T": fm(inp["norm_g"][l, 2], 16),
            "g1row": g1row, "ng1row": np.ascontiguousarray(inp["norm_g"][l, 1].reshape(1, D)),
            "identF": identF, "onesF": onesF,
        })
    return run(build_k4(), in_maps)


EPC = NE // NCORES
SW_LIMIT = 7.0
SW_ALPHA = 1.702


def build_k5():
    P = Prog()
    S = P.S
    fTd = P.din("fT", [D, NTOK], BF16)
    wg4d = P.din("wg4", [128, NKT * EPC])
    wgTd = P.din("wgT4", [EPC, NTOK])
    wgud = P.din("w_gu", [EPC, D, 2 * DE]); bgud = P.din("b_guT", [128, EPC * 16])
    wdnd = P.din("w_dn", [EPC, DE, D]); bdnd = P.din("b_dn", [EPC, D])
    part = P.dout("part", [NTOK, D])

    wg4 = P.sb("wg4", [128, NKT * EPC]); bgu = P.sb("bgu", [128, EPC * 16]); bdn = P.sb("bdn", [EPC, D])
    S.dma("sp", wg4[:], wg4d, writes=[wg4.b]); S.dma("sp", bgu[:], bgud, writes=[bgu.b]); S.dma("sp", bdn[:], bdnd, writes=[bdn.b])

    gus = [P.dscratch("gus%d" % e, [128, 16 * 2 * DE], BF16) for e in range(EPC)]
    dns = [P.dscratch("dns%d" % e, [128, 8 * D], BF16) for e in range(EPC)]
    for t_ in gus + dns:
        t_.b.multi = True
    P.push()
    st = [P.sb("st", [128, 16, 512], BF16) for _ in range(3)]
    ns = 0
    for e in range(EPC):
        guv0 = gus[e].t.rearrange("p (k c) -> p k c", k=16)
        dnv0 = dns[e].t.rearrange("p (k c) -> p k c", k=8)
        for cb in range(4):
            s_ = st[ns % 3]; ns += 1
            S.dma("pool", s_[:], wgud[e].rearrange("(k p) c -> p k c", p=128)[:, :, cb * 512:(cb + 1) * 512], writes=[s_.b])
            S.dma("sp", guv0[:, :, cb * 512:(cb + 1) * 512], s_[:], reads=[s_.b], writes=[gus[e].b])
        for cb in range(4):
            s_ = st[ns % 3]; ns += 1
            S.dma("pool", s_[:, 0:8, :], wdnd[e].rearrange("(k p) c -> p k c", p=128)[:, :, cb * 512:(cb + 1) * 512], writes=[s_.b])
            S.dma("sp", dnv0[:, :, cb * 512:(cb + 1) * 512], s_[:, 0:8, :], reads=[s_.b], writes=[dns[e].b])
    P.pop()

    fTv = fTd.rearrange("(k p) t -> p k t", p=128)
    fgs = [P.sb("fg", [128, 16, 512], BF16) for _ in range(2)]
    wgts = [P.sb("wgt", [EPC, 512]) for _ in range(2)]
    acc = P.sb("acc", [128, 4, D])
    accb = [[Buf("acc%d_%d" % (t, cb)) for cb in range(4)] for t in range(4)]
    gs = P.sb("gs", [128, 8, 512]); gsb = [Buf("gs%d" % j) for j in range(8)]
    actT = P.sb("actT", [128, 8, 512], BF16); actb = Buf("actT", multi=True)
    wgu_t = [P.sb("wgu", [128, 16, 512], BF16) for _ in range(3)]
    wdn_t = [P.sb("wdn", [128, 8, 512], BF16) for _ in range(3)]
    t1s = [P.sb("t1", [128, 512]) for _ in range(2)]
    sgs = [P.sb("sg", [128, 512]) for _ in range(2)]
    pg = [P.ps("pg") for _ in range(3)]
    pd = [P.ps("pd") for _ in range(3)]
    pb = P.ps("pb")
    npg = 0; npd = 0; nwu = 0; nwd = 0; nt1 = 0
    groups = [(t0, min(t0 + 512, NTOK)) for t0 in range(0, NTOK, 512)]
    for gi, (t0, t1) in enumerate(groups):
        n = t1 - t0
        ntile = n // 128
        fg = fgs[gi % 2]; wgt = wgts[gi % 2]
        S.dma("sp", fg[:, :, 0:n], fTv[:, :, t0:t1], writes=[fg.b])
        S.dma("sp", wgt[:, 0:n], wgTd[:, t0:t1], writes=[wgt.b])
        for tl in range(ntile):
            for cb in range(4):
                S.do("pe", [wgt.b, bdn.b], [pb.b], "matmul", pb[:], wgt[:, tl * 128:(tl + 1) * 128], bdn[:, cb * 512:(cb + 1) * 512], start=True, stop=True)
                S.do("act", [pb.b], [accb[tl][cb]], "activation", out=acc[:, tl, cb * 512:(cb + 1) * 512], in_=pb[:], func=AF.Copy)
        for e in range(EPC):
            guv = gus[e].t.rearrange("p (k c) -> p k c", k=16)
            dnv = dns[e].t.rearrange("p (k c) -> p k c", k=8)
            for cb in range(4):
                W = wgu_t[nwu % 3]; nwu += 1
                S.dma("sp", W[:], guv[:, :, cb * 512:(cb + 1) * 512], reads=[gus[e].b], writes=[W.b])
                for c4 in range(4):
                    ch = cb * 4 + c4
                    ps = pg[npg % 3]; npg += 1
                    for k in range(16):
                        S.do("pe", [W.b, fg.b], [ps.b], "matmul", ps[:, 0:n], W[:, k, c4 * 128:(c4 + 1) * 128], fg[:, k, 0:n], start=(k == 0), stop=(k == 15))
                    bcol = bgu[:, e * 16 + ch:e * 16 + ch + 1]
                    t_ = t1s[nt1 % 2]; sg = sgs[nt1 % 2]; nt1 += 1
                    if ch < 8:
                        S.do("dve", [ps.b, bgu.b], [t_.b], "tensor_scalar", t_[:, 0:n], ps[:, 0:n], bcol, SW_LIMIT, ALU.add, ALU.min)
                        S.do("act", [t_.b], [sg.b], "activation", out=sg[:, 0:n], in_=t_[:, 0:n], func=AF.Sigmoid, scale=SW_ALPHA)
                        S.do("dve", [t_.b, sg.b], [gsb[ch]], "tensor_tensor", gs[:, ch, 0:n], t_[:, 0:n], sg[:, 0:n], ALU.mult)
                    else:
                        j = ch - 8
                        S.do("dve", [ps.b, bgu.b], [t_.b], "tensor_scalar", t_[:, 0:n], ps[:, 0:n], bcol, SW_LIMIT, ALU.add, ALU.min)
                        S.do("dve", [t_.b], [t_.b], "tensor_scalar", t_[:, 0:n], t_[:, 0:n], -SW_LIMIT, 1.0, ALU.max, ALU.add)
                        S.do("dve", [t_.b, gsb[j]], [actb], "tensor_tensor", actT[:, j, 0:n], gs[:, j, 0:n], t_[:, 0:n], ALU.mult)
            for cb in range(4):
                Wd = wdn_t[nwd % 3]; nwd += 1
                S.dma("sp", Wd[:], dnv[:, :, cb * 512:(cb + 1) * 512], reads=[dns[e].b], writes=[Wd.b])
                for tl in range(ntile):
                    ps = pd[npd % 3]; npd += 1
                    for k in range(8):
                        S.do("pe", [actb, Wd.b], [ps.b], "matmul", ps[:], actT[:, k, tl * 128:(tl + 1) * 128], Wd[:, k, :], start=(k == 0), stop=(k == 7))
                    tg = (t0 // 128) + tl
                    a_ = acc[:, tl, cb * 512:(cb + 1) * 512]
                    S.do("dve", [ps.b, wg4.b, accb[tl][cb]], [accb[tl][cb]], "scalar_tensor_tensor", a_, ps[:], wg4[:, tg * EPC + e:tg * EPC + e + 1], a_, ALU.mult, ALU.add)
        rd = [accb[tl][cb] for tl in range(ntile) for cb in range(4)]
        P.outs.append(S.dma("sp", part[t0:t1, :].rearrange("(t p) d -> p t d", p=128), acc[:, 0:ntile, :], reads=rd))
    return P.finish()


def run_k5(inp, l, fT_full, wg_full):
    in_maps = []
    for c in range(NCORES):
        es = slice(c * EPC, (c + 1) * EPC)
        wg = wg_full[:, es]
        wg4 = wg.reshape(NKT, 128, EPC).transpose(1, 0, 2).reshape(128, NKT * EPC)
        bgu = inp["b_gu"][l][es].reshape(EPC, 16, 128).transpose(2, 0, 1).reshape(128, EPC * 16)
        in_maps.append({
            "fT": fT_full, "wg4": np.ascontiguousarray(wg4), "wgT4": np.ascontiguousarray(wg.T),
            "w_gu": inp["w_gu"][l][es], "b_guT": np.ascontiguousarray(bgu),
            "w_dn": inp["w_dn"][l][es], "b_dn": np.ascontiguousarray(inp["b_dn"][l][es]),
        })
    res = run(build_k5(), in_maps)
    return [res[c]["part"] for c in range(NCORES)]


NT6 = 1024 + 32


def build_k6():
    P = Prog()
    S = P.S
    pd_ = P.din("parts", [NCORES, NT6, D])
    x1d = P.din("x1", [NT6, D])
    g2rd = P.din("g2row", [2, D]); ng3rd = P.din("ng3row", [1, D])
    out = P.dout("x2", [NT6, D])
    g2r = [P.sb("g2r", [128, D]) for _ in range(2)]
    ng3r = P.sb("ng3r", [128, D])
    S.dma("sp", ng3r[:], ng3rd.partition_broadcast(128), writes=[ng3r.b])
    for j in range(2):
        S.dma("sp", g2r[j][:], g2rd[j:j + 1, :].partition_broadcast(128), writes=[g2r[j].b])
        S.do("dve", [g2r[j].b, ng3r.b], [g2r[j].b], "tensor_tensor", g2r[j][:], g2r[j][:], ng3r[:], ALU.mult)
    pts = [P.sb("pt", [128, D]) for _ in range(4)]
    fs = [P.sb("f", [128, D]) for _ in range(2)]
    xts = [P.sb("xt", [128, D]) for _ in range(2)]
    junk = P.sb("junk", [128, D])
    ss = P.sb("ss", [128, 1]); rstd = P.sb("rstd", [128, 1])
    npt = 0
    tiles = [(t * 128, 128, 0) for t in range(8)] + [(1024, 32, 1)]
    for ti, (r0, nr, j) in enumerate(tiles):
        f = fs[ti % 2]; xt = xts[ti % 2]
        S.dma("sp", f[0:nr, :], pd_[0, r0:r0 + nr, :], writes=[f.b])
        S.dma("sp", xt[0:nr, :], x1d[r0:r0 + nr, :], writes=[xt.b])
        for c in range(1, NCORES):
            pt = pts[npt % 4]; npt += 1
            S.dma("sp", pt[0:nr, :], pd_[c, r0:r0 + nr, :], writes=[pt.b])
            eng = "dve" if c % 2 else "pool"
            S.do(eng, [f.b, pt.b], [f.b], "tensor_tensor", f[0:nr, :], f[0:nr, :], pt[0:nr, :], ALU.add)
        S.do("act", [f.b], [junk.b, ss.b], "activation", out=junk[0:nr, :], in_=f[0:nr, :], func=AF.Square, accum_out=ss[0:nr, :])
        S.do("dve", [ss.b], [rstd.b], "tensor_scalar", rstd[0:nr, :], ss[0:nr, :], 1.0 / D, EPS, ALU.mult, ALU.add)
        S.do("act", [rstd.b], [rstd.b], "activation", out=rstd[0:nr, :], in_=rstd[0:nr, :], func=AF.Sqrt)
        S.do("dve", [rstd.b], [rstd.b], "reciprocal", rstd[0:nr, :], rstd[0:nr, :])
        S.do("dve", [f.b, rstd.b, g2r[j].b], [f.b], "scalar_tensor_tensor", f[0:nr, :], f[0:nr, :], rstd[0:nr, 0:1], g2r[j][0:nr, :], ALU.mult, ALU.mult)
        S.do("dve", [f.b, xt.b], [xt.b], "tensor_tensor", xt[0:nr, :], xt[0:nr, :], f[0:nr, :], ALU.add)
        P.outs.append(S.dma("sp", out[r0:r0 + nr, :], xt[0:nr, :], reads=[xt.b]))
    return P.finish()


def run_k6(inp, l, parts, x1_lat, x1_ctx, modT):
    rows = mod_rows(modT[l])
    g2row = np.ascontiguousarray(rows[:, 5 * D:6 * D])
    ng3row = np.ascontiguousarray(inp["norm_g"][l, 3].reshape(1, D))
    in_maps = []
    for i in range(NCORES):
        sel = lambda a_lat, a_ctx: np.concatenate([a_lat[i * 1024:(i + 1) * 1024], a_ctx[i * 32:(i + 1) * 32]], axis=0)
        pp = np.stack([sel(p[LC:], p[:LC]) for p in parts], axis=0)
        in_maps.append({"parts": np.ascontiguousarray(pp), "x1": np.ascontiguousarray(sel(x1_lat, x1_ctx)),
                        "g2row": g2row, "ng3row": ng3row})
    res = run(build_k6(), in_maps)
    x2 = np.concatenate([res[i]["x2"][:1024] for i in range(NCORES)], axis=0)
    ctx2 = np.concatenate([res[i]["x2"][1024:] for i in range(NCORES)], axis=0)
    return x2, ctx2


def kernel(**inputs):
    inp = {k: np.asarray(v) for k, v in inputs.items()}
    x = np.ascontiguousarray(inp["x"][0])
    ctx = np.ascontiguousarray(inp["ctx"][0])
    modT = run_k0(inp)
    for l in range(DEPTH):
        r1 = run_k1(inp, l, x, ctx, modT)
        aT = run_k2(inp, l, r1)
        rest_full = np.concatenate([r1[0]["restT"][:, :LC]] + [r1[i]["restT"][:, LC:] for i in range(NCORES)], axis=1)
        del r1
        hyT = run_k3(inp, l, rest_full)
        r4 = run_k4(inp, l, x, ctx, modT, rest_full, aT, hyT)
        del rest_full
        fT = np.ascontiguousarray(np.concatenate([r4[0]["fT"][:, :LC]] + [r4[i]["fT"][:, LC:] for i in range(NCORES)], axis=1))
        wg = np.concatenate([r4[0]["wg"][:LC]] + [r4[i]["wg"][LC:] for i in range(NCORES)], axis=0)
        x1_lat = np.concatenate([r4[i]["x1"][LC:] for i in range(NCORES)], axis=0)
        x1_ctx = r4[0]["x1"][:LC]
        del r4
        parts = run_k5(inp, l, fT, wg)
        x, ctx = run_k6(inp, l, parts, x1_lat, x1_ctx, modT)
        del parts
    return np.ascontiguousarray(x[None].astype(np.float32))
```

```python
import math
from contextlib import ExitStack
import numpy as np
import ml_dtypes
import concourse.bass as bass
import concourse.mybir as mybir
from concourse.bass_utils import run_bass_kernel_spmd

F32 = mybir.dt.float32
BF16 = mybir.dt.bfloat16
AF = mybir.ActivationFunctionType
ALU = mybir.AluOpType
AX = mybir.AxisListType

NCORES = 8
D = 2048
SEQ = 8192
LC = 256
DEPTH = 2
EPS = 1e-6
IN_COLS = 14336
OFF_Q, OFF_K, OFF_V, OFF_HY, OFF_CV, OFF_GATE = 0, 1024, 2048, 3072, 6144, 8192
NE = 32
DE = 1024


class Buf:
    __slots__ = ("name", "w", "r", "multi")

    def __init__(self, name="", multi=False):
        self.name = name
        self.w = []
        self.r = []
        self.multi = multi


class Sched:
    ENG = ("pe", "dve", "act", "pool", "sp")

    def __init__(self, nc, es, n_dma_sems=24):
        self.nc = nc
        self.es = es
        self.ops = []
        self.n_dma_sems = n_dma_sems
        self.last = {}
        self.dmas_since = []
        self.pending = {}

    def barrier(self):
        deps = set(self.last.values()) | set(self.dmas_since)
        self.dmas_since = []
        for e in self.ENG:
            self.pending[e] = set(deps) | self.pending.get(e, set())

    def _deps(self, reads, writes):
        deps = set()
        for b in reads:
            deps.update(b.w)
        for b in writes:
            if not (b.multi and not b.r):
                deps.update(b.w)
            deps.update(b.r)
        return deps

    def _record(self, idx, reads, writes):
        for b in writes:
            if b.multi and not b.r:
                b.w.append(idx)
            else:
                b.w = [idx]
            b.r = []
        for b in reads:
            b.r.append(idx)

    def op(self, eng, fn, reads=(), writes=()):
        deps = self._deps(reads, writes) | self.pending.pop(eng, set())
        idx = len(self.ops)
        self.last[eng] = idx
        self.ops.append(dict(eng=eng, fn=fn, deps=deps, kind="c"))
        self._record(idx, reads, writes)
        return idx

    def do(self, eng, reads, writes, method, *a, **kw):
        return self.op(eng, lambda e: getattr(e, method)(*a, **kw), reads=reads, writes=writes)

    def dma(self, q, out, in_, reads=(), writes=(), **kw):
        deps = self._deps(reads, writes) | self.pending.pop(q, set())
        idx = len(self.ops)
        self.last[q] = idx
        self.dmas_since.append(idx)
        self.ops.append(dict(eng=q, fn=lambda e: e.dma_start(out=out, in_=in_, **kw), deps=deps, kind="d"))
        self._record(idx, reads, writes)
        return idx

    def emit(self, final_waits=()):
        nc = self.nc
        engs = {"pe": nc.tensor, "dve": nc.vector, "act": nc.scalar, "pool": nc.gpsimd, "sp": nc.sync}
        ops = self.ops
        for i, o in enumerate(ops):
            best = {}
            red = set()
            for d in o["deps"]:
                po = ops[d]
                if po["kind"] == "c":
                    if po["eng"] == "pe" and o["eng"] == "pe" and o["kind"] == "c":
                        continue
                    if best.get(po["eng"], -1) < d:
                        best[po["eng"]] = d
                else:
                    red.add(d)
            red.update(best.values())
            o["deps"] = red
        needed = [False] * len(ops)
        for o in ops:
            for d in o["deps"]:
                if ops[d]["kind"] == "c":
                    needed[d] = True
        for d in final_waits:
            if ops[d]["kind"] == "c":
                needed[d] = True
        csem = {e: self.es.enter_context(nc.semaphore("cs_" + e)) for e in self.ENG}
        dsem = [self.es.enter_context(nc.semaphore("ds%d" % i)) for i in range(self.n_dma_sems)]
        ccnt = {e: 0 for e in self.ENG}
        dcnt = [0] * self.n_dma_sems
        ev = [None] * len(ops)
        known = {e: {} for e in self.ENG}
        nd = 0
        nwaits = 0

        def wait(e, key, val):
            nonlocal nwaits
            if known[e].get(key, 0) >= val:
                return
            sem = csem[key] if isinstance(key, str) else dsem[key]
            engs[e].wait_ge(sem, val)
            known[e][key] = val
            nwaits += 1

        for i, o in enumerate(ops):
            e = o["eng"]
            for d in sorted(o["deps"]):
                key, val = ev[d]
                wait(e, key, val)
            if o["kind"] == "c":
                ins = o["fn"](engs[e])
                if needed[i]:
                    ccnt[e] += 1
                    ins.then_inc(csem[e], 1)
                    ev[i] = (e, ccnt[e])
            else:
                k = nd % self.n_dma_sems
                nd += 1
                wait(e, k, dcnt[k])
                ins = o["fn"](engs[e])
                dcnt[k] += 16
                ins.then_inc(dsem[k], 16)
                ev[i] = (k, dcnt[k])
        for d in final_waits:
            key, val = ev[d]
            wait("sp", key, val)
        self.stats = dict(n_ops=len(ops), ccnt=dict(ccnt), nwaits=nwaits)


class T:
    def __init__(self, t, name):
        self.t = t
        self.b = Buf(name)

    def __getitem__(self, k):
        return self.t[k]


class Prog:
    def __init__(self, name="k"):
        self.nc = bass.Bass("TRN2", target_bir_lowering=False)
        self.es = ExitStack()
        self.S = Sched(self.nc, self.es)
        self.outs = []
        self.n = 0
        self.stack = [self.es]

    def push(self):
        self.stack.append(ExitStack())

    def pop(self):
        self.S.barrier()
        self.stack.pop().close()

    def din(self, name, shape, dt=F32):
        return self.nc.dram_tensor(name, list(shape), dt, kind="ExternalInput").ap()

    def dout(self, name, shape, dt=F32):
        return self.nc.dram_tensor(name, list(shape), dt, kind="ExternalOutput").ap()

    def dscratch(self, name, shape, dt=F32):
        return T(self.nc.dram_tensor(name, list(shape), dt, kind="Internal").ap(), name)

    def sb(self, name, shape, dt=F32):
        self.n += 1
        return T(self.stack[-1].enter_context(self.nc.sbuf_tensor("%s_%d" % (name, self.n), list(shape), dt)), name)

    def ps(self, name, shape=(128, 512), dt=F32):
        self.n += 1
        return T(self.stack[-1].enter_context(self.nc.psum_tensor("%s_%d" % (name, self.n), list(shape), dt)), name)

    def finish(self):
        self.S.emit(final_waits=self.outs)
        self.es.close()
        return self.nc


def run(prog_nc, in_maps):
    import time
    t0 = time.time()
    res = run_bass_kernel_spmd(prog_nc, in_maps, core_ids=list(range(NCORES)))
    print("[run] launch %.1fs" % (time.time() - t0), flush=True)
    return res.results


def fm(vec, nchunk):
    return np.ascontiguousarray(np.asarray(vec).reshape(nchunk, 128).T)


def build_k0():
    P = Prog()
    S = P.S
    ccT = P.din("ccT", [128, 32])
    w = P.din("w", [DEPTH, 128, 16, 1536])
    bT = P.din("bT", [128, DEPTH * 12])
    out = P.dout("modT", [128, DEPTH * 12 * 2])
    cc = P.sb("cc", [128, 32])
    sl = P.sb("sl", [128, 32])
    bt = P.sb("bt", [128, DEPTH * 12])
    res = P.sb("res", [128, DEPTH * 12 * 2])
    S.dma("sp", cc[:], ccT, writes=[cc.b])
    S.dma("sp", bt[:], bT, writes=[bt.b])
    S.op("act", lambda e: e.activation(out=sl[:], in_=cc[:], func=AF.Silu), reads=[cc.b], writes=[sl.b])
    wts = []
    for l in range(DEPTH):
        for kg in range(4):
            wt = P.sb("w", [128, 4, 1536])
            S.dma("sp", wt[:], w[l, :, kg * 4:(kg + 1) * 4, :], writes=[wt.b])
            wts.append(wt)
    pss = [P.ps("ps", [128, 2]) for _ in range(4)]
    n = 0
    for l in range(DEPTH):
        for j in range(12):
            ps = pss[n % 4]
            for k in range(16):
                wt = wts[l * 4 + k // 4]
                S.op("pe", lambda e, wt=wt, k=k, j=j, ps=ps: e.matmul(
                    ps[:], wt[:, k % 4, j * 128:(j + 1) * 128], sl[:, 2 * k:2 * k + 2],
                    start=(k == 0), stop=(k == 15)), reads=[wt.b, sl.b], writes=[ps.b])
            idx = l * 12 + j
            S.op("dve", lambda e, ps=ps, idx=idx: e.tensor_scalar(
                res[:, 2 * idx:2 * idx + 2], ps[:], bt[:, idx:idx + 1], None, ALU.add),
                reads=[ps.b, bt.b], writes=[res.b])
            n += 1
    P.outs.append(S.dma("sp", out, res[:], reads=[res.b]))
    return P.finish()


def run_k0(inp):
    cc = np.stack([inp["c"][0], inp["c_ctx"]], axis=-1)
    ccT = np.ascontiguousarray(cc.reshape(16, 128, 2).transpose(1, 0, 2).reshape(128, 32))
    in_maps = []
    for i in range(NCORES):
        w = inp["w_ada"][:, :, i * 1536:(i + 1) * 1536]
        w = np.ascontiguousarray(w.reshape(DEPTH, 16, 128, 1536).transpose(0, 2, 1, 3))
        b = inp["b_ada"][:, i * 1536:(i + 1) * 1536].reshape(DEPTH, 12, 128)
        bT = np.ascontiguousarray(b.transpose(2, 0, 1).reshape(128, DEPTH * 12))
        in_maps.append({"ccT": ccT, "w": w, "bT": bT})
    res = run(build_k0(), in_maps)
    modT = np.zeros((DEPTH, 128, 96, 2), np.float32)
    for i in range(NCORES):
        r = res[i]["modT"].reshape(128, DEPTH, 12, 2)
        for l in range(DEPTH):
            modT[l, :, i * 12:(i + 1) * 12, :] = r[:, l]
    return modT


NT1 = 1280
GROUPS1 = [(0, 256), (256, 768), (768, 1280)]


def rope_tables(core):
    t = core * 1024 + np.arange(1024)
    row = (t // 64).astype(np.float32)
    col = (t % 64).astype(np.float32)
    inv = (10000.0 ** (-np.arange(0, 32, 2, dtype=np.float32) / 32)).astype(np.float32)
    cosT = np.zeros((128, 1024), np.float32)
    sinT = np.zeros((128, 1024), np.float32)
    prot = np.zeros((128, 128), np.float32)
    for p in range(128):
        d = p % 64
        pos = row if d < 32 else col
        e = d % 32
        ang = (pos * inv[e % 16]).astype(np.float32)
        cosT[p] = np.cos(ang)
        sinT[p] = -np.sin(ang) if e < 16 else np.sin(ang)
        partner = p + 16 if e < 16 else p - 16
        prot[partner, p] = 1.0
    return cosT, sinT, prot


def rms_rstd(S, P, xt, junk, ss, rstd, width=D):
    S.op("act", lambda e: e.activation(out=junk[:], in_=xt[:], func=AF.Square, accum_out=ss[:]),
         reads=[xt.b], writes=[junk.b, ss.b])
    S.op("dve", lambda e: e.tensor_scalar(rstd[:], ss[:], 1.0 / width, EPS, ALU.mult, ALU.add),
         reads=[ss.b], writes=[rstd.b])
    S.op("act", lambda e: e.activation(out=rstd[:], in_=rstd[:], func=AF.Sqrt),
         reads=[rstd.b], writes=[rstd.b])
    S.op("dve", lambda e: e.reciprocal(rstd[:], rstd[:]),
         reads=[rstd.b], writes=[rstd.b])


def build_k1():
    P = Prog()
    S = P.S
    xin = P.din("xin", [NT1, D])
    modT = P.din("modT", [128, 96 * 2])
    ng0T = P.din("ng0T", [128, 16])
    w_in = P.din("w_in", [D, IN_COLS])
    cosd = P.din("cosT", [128, 1024])
    sind = P.din("sinT", [128, 1024])
    protd = P.din("prot", [128, 128])
    bgd = P.din("bgT", [128, 48])
    identd = P.din("ident", [128, 128], BF16)
    qkT = P.dout("qkT", [2048, NT1], BF16)
    vout = P.dout("v", [NT1, 1024], BF16)
    restT = P.dout("restT", [IN_COLS - 3072, NT1])

    mod = P.sb("mod", [128, 192]); ng0 = P.sb("ng0", [128, 16])
    cos = P.sb("cos", [128, 1024]); sin = P.sb("sin", [128, 1024])
    prot = P.sb("prot", [128, 128]); bg = P.sb("bg", [128, 48]); ident = P.sb("ident", [128, 128], BF16)
    for t_, d_ in ((mod, modT), (ng0, ng0T), (cos, cosd), (sin, sind), (prot, protd), (bg, bgd), (ident, identd)):
        S.dma("sp", t_[:], d_, writes=[t_.b])
    s1 = P.sb("s1", [128, 32])
    sh1 = P.sb("sh1", [128, 32])
    modv = mod.t[:].rearrange("p (c j) -> p c j", j=2)
    for j in range(2):
        S.op("dve", lambda e, j=j: e.tensor_scalar(s1[:, j * 16:(j + 1) * 16], modv[:, 16:32, j], 1.0, None, ALU.add),
             reads=[mod.b], writes=[s1.b])
        S.op("dve", lambda e, j=j: e.tensor_tensor(s1[:, j * 16:(j + 1) * 16], s1[:, j * 16:(j + 1) * 16], ng0[:], ALU.mult),
             reads=[s1.b, ng0.b], writes=[s1.b])
        S.op("dve", lambda e, j=j: e.tensor_copy(sh1[:, j * 16:(j + 1) * 16], modv[:, 0:16, j]),
             reads=[mod.b], writes=[sh1.b])

    hT = P.sb("hT", [128, 16, NT1], BF16)
    hTb = [Buf("hT%d" % g, multi=True) for g in range(3)]
    grp_of_tile = lambda tt: 0 if tt < 2 else (1 if tt < 6 else 2)
    xts = [P.sb("xt", [128, D]) for _ in range(2)]
    junk = P.sb("junk", [128, D])
    xns = [P.sb("xn", [128, D], BF16) for _ in range(2)]
    sss = [P.sb("ss", [128, 1]) for _ in range(2)]
    rstds = [P.sb("rstd", [128, 1]) for _ in range(2)]
    tps = [P.ps("tp", [128, 512], BF16) for _ in range(2)]
    ntp = 0
    for tt in range(NT1 // 128):
        xt, xn, ss, rstd = xts[tt % 2], xns[tt % 2], sss[tt % 2], rstds[tt % 2]
        j = 1 if tt < 2 else 0
        S.dma("sp", xt[:], xin[tt * 128:(tt + 1) * 128, :], writes=[xt.b])
        rms_rstd(S, P, xt, junk, ss, rstd)
        S.op("act", lambda e, xt=xt, xn=xn, rstd=rstd: e.activation(out=xn[:], in_=xt[:], func=AF.Copy, scale=rstd[:, 0:1]),
             reads=[xt.b, rstd.b], writes=[xn.b])
        for k4 in range(4):
            tp = tps[ntp % 2]; ntp += 1
            for kk in range(4):
                k = k4 * 4 + kk
                S.op("pe", lambda e, tp=tp, kk=kk, k=k, xn=xn: e.transpose(tp[:, kk * 128:(kk + 1) * 128], xn[:, k * 128:(k + 1) * 128], ident[:]),
                     reads=[xn.b, ident.b], writes=[tp.b])
            for kk in range(4):
                k = k4 * 4 + kk
                S.op("dve", lambda e, tp=tp, kk=kk, k=k, j=j, tt=tt: e.tensor_scalar(
                    hT[:, k, tt * 128:(tt + 1) * 128], tp[:, kk * 128:(kk + 1) * 128],
                    s1[:, j * 16 + k:j * 16 + k + 1], sh1[:, j * 16 + k:j * 16 + k + 1], ALU.mult, ALU.add),
                    reads=[tp.b, s1.b, sh1.b], writes=[hTb[grp_of_tile(tt)]])

    wv = w_in.rearrange("(k p) c -> p k c", p=128)
    wts = [P.sb("wt", [128, 16, 512], BF16) for _ in range(3)]
    accs = [P.ps("acc") for _ in range(4)]
    rots = [P.ps("rot") for _ in range(2)]
    stg = [P.sb("stg", [128, 512]) for _ in range(4)]
    stgb = [P.sb("stgb", [128, 512], BF16) for _ in range(3)]
    tmp1 = [P.sb("tmp1", [128, 512]) for _ in range(2)]
    na = 0; ns = 0; nsb = 0; nr = 0
    for cb in range(IN_COLS // 512):
        wt = wts[cb % 3]
        S.dma("pool", wt[:], wv[:, :, cb * 512:(cb + 1) * 512], writes=[wt.b])
        if cb in (4, 5):
            for tt in range(NT1 // 128):
                acc = accs[na % 4]; na += 1
                for k in range(16):
                    S.op("pe", lambda e, acc=acc, k=k, tt=tt, wt=wt: e.matmul(
                        acc[:], hT[:, k, tt * 128:(tt + 1) * 128], wt[:, k, :], start=(k == 0), stop=(k == 15)),
                        reads=[hTb[grp_of_tile(tt)], wt.b], writes=[acc.b])
                sb_ = stgb[nsb % 3]; nsb += 1
                S.op("act", lambda e, acc=acc, sb_=sb_: e.activation(out=sb_[:], in_=acc[:], func=AF.Copy),
                     reads=[acc.b], writes=[sb_.b])
                P.outs.append(S.dma("sp", vout[tt * 128:(tt + 1) * 128, (cb - 4) * 512:(cb - 3) * 512], sb_[:], reads=[sb_.b]))
            continue
        for c4 in range(4):
            ch = cb * 4 + c4
            for g, (g0, g1) in enumerate(GROUPS1):
                n = g1 - g0
                acc = accs[na % 4]; na += 1
                for k in range(16):
                    S.op("pe", lambda e, acc=acc, k=k, c4=c4, wt=wt, g0=g0, g1=g1, n=n: e.matmul(
                        acc[:, 0:n], wt[:, k, c4 * 128:(c4 + 1) * 128], hT[:, k, g0:g1], start=(k == 0), stop=(k == 15)),
                        reads=[hTb[g], wt.b], writes=[acc.b])
                if ch < 16:
                    sb_ = stgb[nsb % 3]; nsb += 1
                    if g == 0:
                        S.op("act", lambda e, acc=acc, sb_=sb_, n=n: e.activation(out=sb_[:, 0:n], in_=acc[:, 0:n], func=AF.Copy),
                             reads=[acc.b], writes=[sb_.b])
                    else:
                        a = stg[ns % 4]; ns += 1
                        S.op("act", lambda e, acc=acc, a=a, n=n: e.activation(out=a[:, 0:n], in_=acc[:, 0:n], func=AF.Copy),
                             reads=[acc.b], writes=[a.b])
                        rot = rots[nr % 2]; t1 = tmp1[nr % 2]; nr += 1
                        S.op("pe", lambda e, rot=rot, a=a, n=n: e.matmul(rot[:, 0:n], prot[:], a[:, 0:n], start=True, stop=True),
                             reads=[prot.b, a.b], writes=[rot.b])
                        c0 = g0 - 256
                        S.op("dve", lambda e, t1=t1, a=a, n=n, c0=c0: e.tensor_tensor(t1[:, 0:n], a[:, 0:n], cos[:, c0:c0 + n], ALU.mult),
                             reads=[a.b, cos.b], writes=[t1.b])
                        S.op("dve", lambda e, rot=rot, a=a, n=n, c0=c0: e.tensor_tensor(a[:, 0:n], rot[:, 0:n], sin[:, c0:c0 + n], ALU.mult),
                             reads=[rot.b, sin.b], writes=[a.b])
                        S.op("dve", lambda e, t1=t1, a=a, sb_=sb_, n=n: e.tensor_tensor(sb_[:, 0:n], t1[:, 0:n], a[:, 0:n], ALU.add),
                             reads=[t1.b, a.b], writes=[sb_.b])
                    P.outs.append(S.dma("sp", qkT[ch * 128:(ch + 1) * 128, g0:g1], sb_[:, 0:n], reads=[sb_.b]))
                else:
                    a = stg[ns % 4]; ns += 1
                    if ch < 64:
                        S.op("act", lambda e, acc=acc, a=a, n=n: e.activation(out=a[:, 0:n], in_=acc[:, 0:n], func=AF.Copy),
                             reads=[acc.b], writes=[a.b])
                    else:
                        gi = ch - 64
                        S.op("act", lambda e, acc=acc, a=a, n=n, gi=gi: e.activation(
                            out=a[:, 0:n], in_=acc[:, 0:n], func=AF.Sigmoid, bias=bg[:, gi:gi + 1]),
                            reads=[acc.b, bg.b], writes=[a.b])
                    r0 = (ch - 24) * 128
                    P.outs.append(S.dma("sp", restT[r0:r0 + 128, g0:g1], a[:, 0:n], reads=[a.b]))
    return P.finish()


def run_k1(inp, l, x, ctx, modT):
    ident = np.eye(128, dtype=np.float32).astype(ml_dtypes.bfloat16)
    in_maps = []
    for i in range(NCORES):
        cosT, sinT, prot = rope_tables(i)
        in_maps.append({
            "xin": np.ascontiguousarray(np.concatenate([ctx, x[i * 1024:(i + 1) * 1024]], axis=0)),
            "modT": np.ascontiguousarray(modT[l].reshape(128, 192)),
            "ng0T": fm(inp["norm_g"][l, 0], 16),
            "w_in": inp["w_in"][l],
            "cosT": cosT, "sinT": sinT, "prot": prot,
            "bgT": fm(inp["b_gate"][l], 48),
            "ident": ident,
        })
    return run(build_k1(), in_maps)


NTOK = LC + SEQ
NKT = NTOK // 128


def build_k2():
    P = Prog()
    S = P.S
    qd = P.din("qT2", [128, NTOK], BF16)
    kd = P.din("kT2", [128, NTOK], BF16)
    vd = P.din("vh", [NTOK, 128], BF16)
    lamd = P.din("lamp", [1, 256])
    lcd = P.din("lamc", [128, 2])
    gd = P.din("subg", [1, 128])
    identd = P.din("ident", [128, 128], BF16)
    aout = P.dout("aT", [128, NTOK], BF16)

    q = P.sb("q", [128, NTOK], BF16); k = P.sb("k", [128, NTOK], BF16)
    v = P.sb("v", [128, NKT, 129], BF16)
    lamp = P.sb("lamp", [128, 256]); lamc = P.sb("lamc", [128, 2]); g = P.sb("g", [128, 128])
    ident = P.sb("ident", [128, 128], BF16)
    S.dma("sp", q[:], qd, writes=[q.b])
    S.dma("sp", k[:], kd, writes=[k.b])
    S.op("pool", lambda e: e.memset(v[:, :, 128:129], 1.0), writes=[v.b])
    S.dma("sp", v[:, :, 0:128], vd.rearrange("(t p) d -> p t d", p=128), writes=[v.b])
    S.dma("sp", lamp[:], lamd.partition_broadcast(128), writes=[lamp.b])
    S.dma("sp", g[:], gd.partition_broadcast(128), writes=[g.b])
    S.dma("sp", lamc[:], lcd, writes=[lamc.b])
    S.dma("sp", ident[:], identd, writes=[ident.b])
    pr = P.sb("pr", [128, 128]); sm = P.sb("sm", [128, 2]); nlam = P.sb("nlam", [128, 1])
    S.op("dve", lambda e: e.tensor_tensor(pr[:, 0:64], lamp[:, 0:64], lamp[:, 64:128], ALU.mult), reads=[lamp.b], writes=[pr.b])
    S.op("dve", lambda e: e.tensor_tensor(pr[:, 64:128], lamp[:, 128:192], lamp[:, 192:256], ALU.mult), reads=[lamp.b, pr.b], writes=[pr.b])
    S.op("dve", lambda e: e.tensor_reduce(sm[:], pr[:].rearrange("p (a b) -> p a b", a=2), AX.X, ALU.add), reads=[pr.b], writes=[sm.b])
    S.op("act", lambda e: e.activation(out=sm[:], in_=sm[:], func=AF.Exp), reads=[sm.b], writes=[sm.b])
    S.op("dve", lambda e: e.tensor_tensor(nlam[:], sm[:, 1:2], sm[:, 0:1], ALU.subtract), reads=[sm.b], writes=[nlam.b])
    S.op("dve", lambda e: e.tensor_tensor(nlam[:], nlam[:], lamc[:, 0:1], ALU.subtract), reads=[nlam.b, lamc.b], writes=[nlam.b])
    S.op("dve", lambda e: e.tensor_scalar(g[:], g[:], lamc[:, 1:2], None, ALU.mult), reads=[g.b, lamc.b], writes=[g.b])

    sps = [P.ps("s") for _ in range(3)]
    ops_ = [[P.ps("o", [128, 512]) for _ in range(2)] for _ in range(2)]
    tps = P.ps("tp", [128, 512], BF16)
    pts = [P.sb("pT", [128, 512], BF16) for _ in range(3)]
    o1 = P.sb("o1", [128, 128]); dif = P.sb("dif", [128, 128]); junk = P.sb("junk", [128, 128])
    rs = P.sb("rs", [128, 2]); ss = P.sb("ss", [128, 1]); rstd = P.sb("rstd", [128, 1])
    ab = P.sb("ab", [128, 512], BF16); aTs = [P.sb("aTs", [128, 512], BF16) for _ in range(2)]
    nsp = 0; npt = 0
    groups = [(0, 256, 2)] + [(256 + 512 * gi, 256 + 512 * (gi + 1), NKT) for gi in range(SEQ // 512)]
    for gi, (q0, q1, nkt) in enumerate(groups):
        nq = q1 - q0
        nsub = nq // 128
        units = [(kt, j) for kt in range(nkt) for j in range(2)]
        bufs = []
        for _ in units:
            bufs.append((sps[nsp % 3], pts[npt % 3])); nsp += 1; npt += 1

        def qk(u):
            kt, j = units[u]; sp, pT = bufs[u]
            S.do("pe", [k.b, q.b], [sp.b], "matmul", sp[:, 0:nq], k[j * 64:(j + 1) * 64, kt * 128:(kt + 1) * 128],
                 q[j * 64:(j + 1) * 64, q0:q1], start=True, stop=True)
            S.do("act", [sp.b], [pT.b], "activation", out=pT[:, 0:nq], in_=sp[:, 0:nq], func=AF.Exp, scale=0.125)

        def pv(u):
            kt, j = units[u]; sp, pT = bufs[u]
            for qs in range(nsub):
                o_ = ops_[j][qs // 2]
                S.do("pe", [pT.b, v.b], [o_.b], "matmul", o_[:, (qs % 2) * 129:(qs % 2) * 129 + 129], pT[:, qs * 128:(qs + 1) * 128], v[:, kt, :],
                     start=(kt == 0 and qs % 2 == 0), stop=(kt == nkt - 1), skip_group_check=True)

        qk(0)
        if len(units) > 1:
            qk(1)
        for u in range(len(units)):
            if u + 2 < len(units):
                qk(u + 2)
            pv(u)
        for qs in range(nsub):
            oa = ops_[0][qs // 2]; ob = ops_[1][qs // 2]; h_ = qs % 2
            S.op("dve", lambda e, oa=oa, h_=h_: e.reciprocal(rs[:, 0:1], oa[:, h_ * 129 + 128:h_ * 129 + 129]), reads=[oa.b], writes=[rs.b])
            S.op("dve", lambda e, ob=ob, h_=h_: e.reciprocal(rs[:, 1:2], ob[:, h_ * 129 + 128:h_ * 129 + 129]), reads=[ob.b, rs.b], writes=[rs.b])
            S.op("dve", lambda e: e.tensor_tensor(rs[:, 1:2], rs[:, 1:2], nlam[:], ALU.mult), reads=[rs.b, nlam.b], writes=[rs.b])
            S.op("dve", lambda e, oa=oa, h_=h_: e.tensor_scalar(o1[:], oa[:, h_ * 129:h_ * 129 + 128], rs[:, 0:1], None, ALU.mult),
                 reads=[oa.b, rs.b], writes=[o1.b])
            S.op("dve", lambda e, ob=ob, h_=h_: e.scalar_tensor_tensor(dif[:], ob[:, h_ * 129:h_ * 129 + 128], rs[:, 1:2], o1[:], ALU.mult, ALU.add),
                 reads=[ob.b, rs.b, o1.b], writes=[dif.b])
            rms_rstd(S, P, dif, junk, ss, rstd, width=128)
            S.op("dve", lambda e: e.tensor_scalar(dif[:], dif[:], rstd[:, 0:1], None, ALU.mult), reads=[dif.b, rstd.b], writes=[dif.b])
            S.op("dve", lambda e, qs=qs: e.tensor_tensor(ab[:, qs * 128:(qs + 1) * 128], dif[:], g[:], ALU.mult),
                 reads=[dif.b, g.b], writes=[ab.b])
            S.op("pe", lambda e, qs=qs: e.transpose(tps[:, qs * 128:(qs + 1) * 128], ab[:, qs * 128:(qs + 1) * 128], ident[:]),
                 reads=[ab.b, ident.b], writes=[tps.b])
        aT = aTs[gi % 2]
        S.op("act", lambda e, aT=aT, nq=nq: e.activation(out=aT[:, 0:nq], in_=tps[:, 0:nq], func=AF.Copy), reads=[tps.b], writes=[aT.b])
        P.outs.append(S.dma("sp", aout[:, q0:q1], aT[:, 0:nq], reads=[aT.b]))
    return P.finish()


def lam_init_of(l):
    return 0.8 - 0.6 * math.exp(-0.3 * l)


def run_k2(inp, l, r1):
    ident = np.eye(128, dtype=np.float32).astype(ml_dtypes.bfloat16)
    qk = np.concatenate([r1[0]["qkT"][:, :LC]] + [r1[i]["qkT"][:, LC:] for i in range(NCORES)], axis=1)
    v = np.concatenate([r1[0]["v"][:LC]] + [r1[i]["v"][LC:] for i in range(NCORES)], axis=0)
    li = lam_init_of(l)
    lamc = np.tile(np.array([[li, 1.0 - li]], np.float32), (128, 1))
    in_maps = []
    for h in range(NCORES):
        in_maps.append({
            "qT2": np.ascontiguousarray(qk[h * 128:(h + 1) * 128]),
            "kT2": np.ascontiguousarray(qk[1024 + h * 128:1024 + (h + 1) * 128]),
            "vh": np.ascontiguousarray(v[:, h * 128:(h + 1) * 128]),
            "lamp": np.ascontiguousarray(inp["da_lambda"][l].reshape(1, 256)),
            "lamc": lamc,
            "subg": np.ascontiguousarray(inp["da_subln_g"][l].reshape(1, 128)),
            "ident": ident,
        })
    res = run(build_k2(), in_maps)
    return np.concatenate([res[h]["aT"] for h in range(NCORES)], axis=0)


HY_EMB = 33
TWO_PI = 2.0 * math.pi


def hyena_tables(n, core):
    m = np.arange(2 * n)
    pos = np.where(m < n, n - 1 - m, m - n)
    t = np.linspace(0.0, 1.0, n, dtype=np.float32)[pos]
    w = (2.0 * math.pi * pos.astype(np.float32) / n).astype(np.float32)
    bands = np.linspace(1e-4, 15, 16, dtype=np.float32)
    z = np.concatenate([t[None, :], np.cos(bands[:, None] * w[None, :]), -np.sin(bands[:, None] * w[None, :])], axis=0)
    lo = math.log(1e-2) / 1.5
    hi = math.log(1e-2) / 0.3
    deltas = np.abs(np.linspace(lo, hi, 1024, dtype=np.float32))[core * 128:(core + 1) * 128]
    dec = np.exp(-t[None, :] * deltas[:, None])
    return z.astype(np.float32), dec.astype(np.float32)


def build_k3(dbg=False):
    P = Prog()
    S = P.S
    N, NC_ = SEQ, LC
    ud = P.din("uT", [3, 128, NTOK])
    swd = P.din("swT", [128, 9]); sbd = P.din("sbT", [128, 3])
    zL = P.din("zL", [HY_EMB, 2 * N]); zC = P.din("zC", [HY_EMB, 2 * NC_])
    dL = P.din("dL", [128, 2 * N]); dC = P.din("dC", [128, 2 * NC_])
    w1d = P.din("w1", [HY_EMB, 64]); w2d = P.din("w2", [64, 64]); w3d = P.din("w3c", [64, 512])
    pd = P.din("mlp", [64, 3])
    hbd = P.din("hbT", [128, 2])
    identd = P.din("identF", [128, 128]); jmd = P.din("jm", [128, 128])
    out = P.dout("hyT", [128, NTOK])
    kdL = [P.dscratch("kdL%d" % o, [128, 2 * N], BF16) for o in range(2)]
    kdC = [P.dscratch("kdC%d" % o, [128, 2 * NC_], BF16) for o in range(2)]

    sw = P.sb("sw", [128, 9]); sbb = P.sb("sbb", [128, 3]); hb = P.sb("hb", [128, 2])
    ident = P.sb("ident", [128, 128]); jm = P.sb("jm", [128, 128])
    w1 = P.sb("w1", [HY_EMB, 64]); w2 = P.sb("w2", [64, 64]); w3 = P.sb("w3", [64, 512]); mp = P.sb("mp", [64, 3])
    for t_, d_ in ((sw, swd), (sbb, sbd), (hb, hbd), (ident, identd), (jm, jmd), (w1, w1d), (w2, w2d), (w3, w3d), (mp, pd)):
        S.dma("sp", t_[:], d_, writes=[t_.b])
    fb = P.sb("fb", [64, 2])
    S.op("dve", lambda e: e.tensor_scalar(fb[:], mp[:, 0:2], mp[:, 2:3], None, ALU.mult), reads=[mp.b], writes=[fb.b])

    def filters(n, zd, dd, kd):
        W = 2 * n
        P.push()
        kf = [P.sb("kf", [128, W]) for _ in range(2)]
        kfb = [Buf("kfb%d" % o, multi=True) for o in range(2)]
        nb = max(1, W // 512)
        bw = W // nb
        zts = [P.sb("zt", [HY_EMB, bw]) for _ in range(2)]
        dts = [P.sb("dt", [128, bw]) for _ in range(2)]
        hs = [[P.sb("h", [64, bw]) for _ in range(2)] for _ in range(2)]
        t1 = P.sb("t1", [64, bw]); t2 = P.sb("t2", [64, bw])
        pa = [P.ps("pa") for _ in range(2)]
        pb = [P.ps("pb") for _ in range(2)]

        def sin_layer(ps, h, li):
            S.op("dve", lambda e: e.tensor_scalar(h[:], ps[0:64, 0:bw], mp[:, 2:3], fb[:, li:li + 1], ALU.mult, ALU.add),
                 reads=[ps.b, mp.b, fb.b], writes=[h.b])
            S.op("dve", lambda e: e.tensor_scalar(t1[:], h[:], math.pi, -TWO_PI, ALU.is_gt, ALU.mult), reads=[h.b], writes=[t1.b])
            S.op("dve", lambda e: e.tensor_scalar(t2[:], h[:], -math.pi, TWO_PI, ALU.is_lt, ALU.mult), reads=[h.b], writes=[t2.b])
            S.op("dve", lambda e: e.tensor_tensor(t1[:], t1[:], t2[:], ALU.add), reads=[t1.b, t2.b], writes=[t1.b])
            S.op("dve", lambda e: e.tensor_tensor(h[:], h[:], t1[:], ALU.add), reads=[h.b, t1.b], writes=[h.b])
            S.op("act", lambda e: e.activation(out=h[:], in_=h[:], func=AF.Sin), reads=[h.b], writes=[h.b])

        for b in range(nb):
            zt = zts[b % 2]; dt_ = dts[b % 2]; c0 = b * bw
            S.dma("sp", zt[:], zd[:, c0:c0 + bw], writes=[zt.b])
            S.dma("sp", dt_[:], dd[:, c0:c0 + bw], writes=[dt_.b])
            p1 = pa[b % 2]; h1 = hs[0][b % 2]; h2 = hs[1][b % 2]
            S.op("pe", lambda e, p1=p1, zt=zt: e.matmul(p1[0:64, 0:bw], w1[:], zt[:], start=True, stop=True),
                 reads=[w1.b, zt.b], writes=[p1.b])
            sin_layer(p1, h1, 0)
            S.op("pe", lambda e, p1=p1, h1=h1: e.matmul(p1[0:64, 0:bw], w2[:], h1[:], start=True, stop=True),
                 reads=[w2.b, h1.b], writes=[p1.b])
            sin_layer(p1, h2, 1)
            for o in range(2):
                if bw <= n:
                    segs = [(0, bw, 0 if c0 < n else 1)]
                else:
                    segs = [(0, n, 0), (n, bw, 1)]
                p3 = pb[o]
                for (a0, a1, dr) in segs:
                    od = o * 2 + dr
                    S.op("pe", lambda e, p3=p3, h2=h2, od=od, a0=a0, a1=a1: e.matmul(
                        p3[:, a0:a1], w3[:, od * 128:(od + 1) * 128], h2[:, a0:a1], start=True, stop=True),
                        reads=[w3.b, h2.b], writes=[p3.b])
                S.op("dve", lambda e, p3=p3, o=o, dt_=dt_, c0=c0: e.tensor_tensor(kf[o][:, c0:c0 + bw], p3[:, 0:bw], dt_[:], ALU.mult),
                     reads=[p3.b, dt_.b], writes=[kfb[o]])
        cw = min(W, 2048)
        ncw = W // cw
        junk = P.sb("junk", [128, cw]); part = P.sb("part", [128, 8]); tot = P.sb("tot", [128, 1])
        obs = [P.sb("ob", [128, cw], BF16) for _ in range(2)]
        for o in range(2):
            for i in range(ncw):
                S.op("act", lambda e, o=o, i=i: e.activation(out=junk[:], in_=kf[o][:, i * cw:(i + 1) * cw], func=AF.Abs, accum_out=part[:, i:i + 1]),
                     reads=[kfb[o]], writes=[junk.b, part.b])
            S.op("dve", lambda e: e.tensor_reduce(tot[:], part[:, 0:ncw], AX.X, ALU.add), reads=[part.b], writes=[tot.b])
            S.op("dve", lambda e: e.reciprocal(tot[:], tot[:]), reads=[tot.b], writes=[tot.b])
            for i in range(ncw):
                ob = obs[i % 2]
                S.op("act", lambda e, o=o, i=i, ob=ob: e.activation(out=ob[:], in_=kf[o][:, i * cw:(i + 1) * cw], func=AF.Copy, scale=tot[:, 0:1]),
                     reads=[kfb[o], tot.b], writes=[ob.b])
                S.dma("sp", kd[o][:, i * cw:(i + 1) * cw], ob[:], reads=[ob.b], writes=[kd[o].b])
        P.pop()

    filters(N, zL, dL, kdL)
    filters(NC_, zC, dC, kdC)
    if dbg == 1:
        dbo = P.dout("dbg", [128, 2 * NC_], BF16)
        dbt = P.sb("dbt", [128, 2 * NC_], BF16)
        S.dma("sp", dbt[:], kdC[0][:], reads=[kdC[0].b], writes=[dbt.b])
        P.outs.append(S.dma("sp", dbo, dbt[:], reads=[dbt.b]))

    A = P.sb("A", [128, NTOK])
    B = P.sb("B", [128, NTOK])
    U = P.sb("U", [128, 2048])
    Zt = P.sb("Zt", [128, 128, 64], BF16)
    Yt = P.sb("Yt", [128, 64, 128])
    Ytb = Buf("Ytb", multi=True)
    Gs = [P.sb("G", [128, 2 * N - 128], BF16) for _ in range(2)]
    tmp = P.sb("tmp", [128, 512])
    tpi = [P.ps("tpi") for _ in range(2)]
    yb = [P.ps("yb") for _ in range(2)]
    tpo = [P.ps("tpo") for _ in range(2)]
    SEGS = [(0, NC_), (NC_, NTOK)]

    def short_conv(which, dst):
        CH = 2046
        for (s0, s1) in SEGS:
            c = s0
            while c < s1:
                e_ = min(c + CH, s1)
                lo = max(c - 1, s0); hi = min(e_ + 1, s1)
                nl = hi - lo
                S.dma("sp", U[:, 0:nl], ud[which, :, lo:hi], writes=[U.b])
                off = c - lo
                nn = e_ - c
                S.op("dve", lambda e, off=off, nn=nn, c=c: e.tensor_scalar(
                    dst[:, c:c + nn], U[:, off:off + nn], sw[:, which * 3 + 1:which * 3 + 2], sbb[:, which:which + 1], ALU.mult, ALU.add),
                    reads=[U.b, sw.b, sbb.b], writes=[dst.b])
                tl = c if off == 1 else c + 1
                S.op("dve", lambda e, tl=tl, e_=e_, lo=lo: e.scalar_tensor_tensor(
                    dst[:, tl:e_], U[:, tl - 1 - lo:e_ - 1 - lo], sw[:, which * 3:which * 3 + 1], dst[:, tl:e_], ALU.mult, ALU.add),
                    reads=[U.b, sw.b, dst.b], writes=[dst.b])
                tr = e_ if hi == e_ + 1 else e_ - 1
                S.op("dve", lambda e, c=c, tr=tr, lo=lo: e.scalar_tensor_tensor(
                    dst[:, c:tr], U[:, c + 1 - lo:tr + 1 - lo], sw[:, which * 3 + 2:which * 3 + 3], dst[:, c:tr], ALU.mult, ALU.add),
                    reads=[U.b, sw.b, dst.b], writes=[dst.b])
                c = e_

    ng = [0]

    def long_conv(o, X, gate, order=(1, 0), dbg_stop=False):
        for si in order:
            (s0, s1), kd = SEGS[si], (kdC[o], kdL[o])[si]
            n = s1 - s0
            nbk = n // 128
            for J0 in range(0, nbk, 4):
                nj = min(4, nbk - J0)
                tp = tpi[(J0 // 4) % 2]
                for jj in range(nj):
                    J = J0 + jj
                    S.op("pe", lambda e, tp=tp, jj=jj, J=J, s0=s0: e.transpose(tp[:, jj * 128:(jj + 1) * 128], X[:, s0 + J * 128:s0 + (J + 1) * 128], ident[:]),
                         reads=[X.b, ident.b], writes=[tp.b])
                eng = "act" if (J0 // 4) % 2 else "dve"
                src = tp[:, 0:nj * 128].rearrange("p (j c) -> p j c", j=nj)
                dst = Zt[:, :, J0:J0 + nj].rearrange("p c j -> p j c")
                if eng == "act":
                    S.op("act", lambda e, src=src, dst=dst: e.activation(out=dst, in_=src, func=AF.Copy), reads=[tp.b], writes=[Zt.b])
                else:
                    S.op("dve", lambda e, src=src, dst=dst: e.tensor_copy(dst, src), reads=[tp.b], writes=[Zt.b])
            for c in range(128):
                G = Gs[ng[0] % 2]; ng[0] += 1
                gw = 2 * n - 128
                S.dma("sp", G[:, 0:gw], bass.AP(kd.t.tensor, c * 2 * n, [[1, 128], [1, gw]]), reads=[kd.b], writes=[G.b])
                ybk = yb[(c // 8) % 2]
                a0 = (c % 8) * 64
                deltas = [0] + [d for d in range(-(nbk - 1), nbk) if d != 0]
                for di, dl in enumerate(deltas):
                    if dl >= 0:
                        i0, i1, j0, j1 = dl, nbk, 0, nbk - dl
                    else:
                        i0, i1, j0, j1 = 0, nbk + dl, -dl, nbk
                    S.op("pe", lambda e, ybk=ybk, a0=a0, i0=i0, i1=i1, j0=j0, j1=j1, G=G, n=n, dl=dl, c=c, di=di, nd=len(deltas): e.matmul(
                        ybk[:, a0 + i0:a0 + i1], G[:, n - 128 - 128 * dl:n - 128 * dl], Zt[:, c, j0:j1],
                        start=(di == 0), stop=(di == nd - 1), skip_group_check=True),
                        reads=[G.b, Zt.b], writes=[ybk.b])
                if c % 8 == 7:
                    c0 = c - 7
                    src = ybk[:, 0:512].rearrange("p (c i) -> p c i", c=8)[:, :, 0:nbk]
                    dst = Yt[:, 0:nbk, c0:c0 + 8].rearrange("p i c -> p c i")
                    if (c // 8) % 2:
                        S.op("act", lambda e, src=src, dst=dst: e.activation(out=dst, in_=src, func=AF.Copy), reads=[ybk.b], writes=[Ytb])
                    else:
                        S.op("dve", lambda e, src=src, dst=dst: e.tensor_copy(dst, src), reads=[ybk.b], writes=[Ytb])
            if dbg_stop:
                d1 = P.dout("dbgZ", [128, 128 * 64], BF16); d2 = P.dout("dbgY", [128, 64 * 128])
                P.outs.append(S.dma("sp", d1, Zt[:].rearrange("p c j -> p (c j)"), reads=[Zt.b]))
                P.outs.append(S.dma("sp", d2, Yt[:].rearrange("p i c -> p (i c)"), reads=[Ytb]))
                return
            for I0 in range(0, nbk, 4):
                ni = min(4, nbk - I0)
                tp = tpo[(I0 // 4) % 2]
                for ii in range(ni):
                    S.op("pe", lambda e, tp=tp, ii=ii, I0=I0: e.matmul(tp[:, ii * 128:(ii + 1) * 128], Yt[:, I0 + ii, :], jm[:], start=True, stop=True),
                         reads=[Ytb, jm.b], writes=[tp.b])
                t0 = s0 + I0 * 128; wd = ni * 128
                S.op("dve", lambda e, tp=tp, t0=t0, wd=wd: e.scalar_tensor_tensor(
                    tmp[:, 0:wd], X[:, t0:t0 + wd], hb[:, o:o + 1], tp[:, 0:wd], ALU.mult, ALU.add),
                    reads=[X.b, hb.b, tp.b], writes=[tmp.b])
                S.op("dve", lambda e, t0=t0, wd=wd: e.tensor_tensor(X[:, t0:t0 + wd], tmp[:, 0:wd], gate[:, t0:t0 + wd], ALU.mult),
                     reads=[tmp.b, gate.b, X.b], writes=[X.b])

    short_conv(2, A)
    short_conv(0, B)
    if dbg == 4:
        for c in range(3):
            G = Gs[c % 2]
            S.dma("sp", G[:, 0:2 * N - 128], bass.AP(kdL[0].t.tensor, c * 2 * N, [[1, 128], [1, 2 * N - 128]]), reads=[kdL[0].b], writes=[G.b])
            dbo = P.dout("dbgG%d" % c, [128, 2 * N - 128], BF16)
            P.outs.append(S.dma("sp", dbo, G[:, 0:2 * N - 128], reads=[G.b]))
        dbk = P.dout("dbgK", [128, 2 * N], BF16)
        P.outs.append(S.dma("sp", dbk, kdL[0][:], reads=[kdL[0].b]))
        return P.finish()
    if dbg == 2:
        dbo = P.dout("dbgA", [128, NTOK]); dbo2 = P.dout("dbgB", [128, NTOK])
        P.outs.append(S.dma("sp", dbo, A[:], reads=[A.b]))
        P.outs.append(S.dma("sp", dbo2, B[:], reads=[B.b]))
        return P.finish()
    if dbg == 5:
        long_conv(0, A, B, order=(0, 1), dbg_stop=True)
        return P.finish()
    long_conv(0, A, B)
    if dbg == 3:
        dbo = P.dout("dbgA", [128, NTOK])
        P.outs.append(S.dma("sp", dbo, A[:], reads=[A.b]))
        return P.finish()
    short_conv(1, B)
    long_conv(1, A, B)
    P.outs.append(S.dma("sp", out, A[:], reads=[A.b]))
    return P.finish()


def run_k3(inp, l, restT_full, dbg=False):
    identF = np.eye(128, dtype=np.float32)
    jm = np.ascontiguousarray(identF[::-1])
    in_maps = []
    for c in range(NCORES):
        zl, dl = hyena_tables(SEQ, c)
        zc, dc = hyena_tables(LC, c)
        u = np.stack([restT_full[j * 1024 + c * 128:j * 1024 + (c + 1) * 128] for j in range(3)], axis=0)
        sw = np.stack([inp["hy_short_w"][l][:, j * 1024 + c * 128:j * 1024 + (c + 1) * 128].T for j in range(3)], axis=1)
        sb_ = np.stack([inp["hy_short_b"][l][j * 1024 + c * 128:j * 1024 + (c + 1) * 128] for j in range(3)], axis=1)
        w3 = inp["hy_w3"][l].reshape(64, 2, 2, 1024)[:, :, :, c * 128:(c + 1) * 128].reshape(64, 512)
        mlp = np.stack([inp["hy_b1"][l], inp["hy_b2"][l], inp["hy_freq"][l]], axis=1)
        in_maps.append({
            "uT": np.ascontiguousarray(u), "swT": np.ascontiguousarray(sw.reshape(128, 9)), "sbT": np.ascontiguousarray(sb_),
            "zL": zl, "zC": zc, "dL": dl, "dC": dc,
            "w1": np.ascontiguousarray(inp["hy_w1"][l]), "w2": np.ascontiguousarray(inp["hy_w2"][l]), "w3c": np.ascontiguousarray(w3),
            "mlp": np.ascontiguousarray(mlp), "hbT": np.ascontiguousarray(inp["hy_bias"][l][:, c * 128:(c + 1) * 128].T),
            "identF": identF, "jm": jm,
        })
    res = run(build_k3(dbg), in_maps)
    if dbg:
        return res
    return np.concatenate([res[c]["hyT"] for c in range(NCORES)], axis=0)


CW4 = 1340


def build_k4():
    P = Prog()
    S = P.S
    gTd = P.din("gT", [6144, NT1])
    cvd = P.din("cvin", [2048, CW4])
    aTd = P.din("aT", [1024, NT1], BF16)
    hyd = P.din("hyT", [1024, NT1])
    xind = P.din("xin", [NT1, D])
    wbr = [P.din(n_, [1024, D]) for n_ in ("w_da", "w_hy", "w_cv")]
    woutd = P.din("w_out", [D, D])
    wrd = P.din("w_r", [D, 32]); brd = P.din("b_r", [1, 32])
    cvpd = P.din("cvp", [128, 8 * 34])
    modd = P.din("modT", [128, 192]); ng2d = P.din("ng2T", [128, 16])
    g1rd = P.din("g1row", [2, D]); ng1rd = P.din("ng1row", [1, D])
    identd = P.din("identF", [128, 128]); onesd = P.din("onesF", [128, 128])
    x1o = P.dout("x1", [NT1, D]); fTo = P.dout("fT", [D, NT1], BF16); wgo = P.dout("wg", [NT1, 32])

    ident = P.sb("ident", [128, 128]); ones = P.sb("ones", [128, 128])
    mod = P.sb("mod", [128, 192]); ng2 = P.sb("ng2", [128, 16]); cvp = P.sb("cvp", [128, 8, 34])
    S.dma("sp", ident[:], identd, writes=[ident.b]); S.dma("sp", ones[:], onesd, writes=[ones.b])
    S.dma("sp", mod[:], modd, writes=[mod.b]); S.dma("sp", ng2[:], ng2d, writes=[ng2.b])
    S.dma("sp", cvp[:].rearrange("p a b -> p (a b)"), cvpd, writes=[cvp.b])
    s2 = P.sb("s2", [128, 32]); sh2 = P.sb("sh2", [128, 32])
    modv = mod.t[:].rearrange("p (c j) -> p c j", j=2)
    for j in range(2):
        S.do("dve", [mod.b], [s2.b], "tensor_scalar", s2[:, j * 16:(j + 1) * 16], modv[:, 64:80, j], 1.0, None, ALU.add)
        S.do("dve", [s2.b, ng2.b], [s2.b], "tensor_tensor", s2[:, j * 16:(j + 1) * 16], s2[:, j * 16:(j + 1) * 16], ng2[:], ALU.mult)
        S.do("dve", [mod.b], [sh2.b], "tensor_copy", sh2[:, j * 16:(j + 1) * 16], modv[:, 48:64, j])

    ypT = P.sb("ypT", [128, 16, NT1], BF16)
    ypTb = [Buf("ypT%d" % g, multi=True) for g in range(3)]
    P.push()
    cvT = P.sb("cvT", [128, 8, NT1], BF16)
    cvTb = Buf("cvTb", multi=True)

    P.push()
    C = P.sb("C", [128, 8, NT1])
    Cb = [Buf("C%d" % ch) for ch in range(8)]
    ats = [P.sb("at", [128, CW4]) for _ in range(2)]
    gts = [P.sb("gt", [128, CW4]) for _ in range(2)]
    for ch in range(8):
        at = ats[ch % 2]; gt = gts[ch % 2]
        S.dma("sp", at[:], cvd[ch * 128:(ch + 1) * 128, :], writes=[at.b])
        S.dma("sp", gt[:], cvd[1024 + ch * 128:1024 + (ch + 1) * 128, :], writes=[gt.b])
        S.do("act", [gt.b], [gt.b], "activation", out=gt[:], in_=gt[:], func=AF.Sigmoid)
        S.do("dve", [at.b, gt.b], [at.b], "tensor_tensor", at[:], at[:], gt[:], ALU.mult)
        eng = "dve"
        for (o0, n, i0) in ((0, 256, 0), (256, 1024, 286)):
            acc = C[:, ch, o0:o0 + n]
            S.do(eng, [at.b, cvp.b], [Cb[ch]], "tensor_scalar", acc, at[:, i0:i0 + n], cvp[:, ch, 0:1], cvp[:, ch, 31:32], ALU.mult, ALU.add)
            for k in range(1, 31):
                S.do(eng, [at.b, cvp.b, Cb[ch]], [Cb[ch]], "scalar_tensor_tensor", acc, at[:, i0 + k:i0 + k + n], cvp[:, ch, k:k + 1], acc, ALU.mult, ALU.add)
    sps = P.ps("sps"); qps = P.ps("qps")
    sq = [P.sb("sq", [128, 512]) for _ in range(2)]
    mean = P.sb("mean", [128, 512]); rstd = P.sb("rstdc", [128, 512]); msq = P.sb("msq", [128, 512])
    tt_ = [P.sb("tt", [128, 512]) for _ in range(2)]
    for g, (g0, g1) in enumerate(GROUPS1):
        n = g1 - g0
        for ch in range(8):
            S.do("pe", [ones.b, Cb[ch]], [sps.b], "matmul", sps[:, 0:n], ones[:], C[:, ch, g0:g1], start=(ch == 0), stop=(ch == 7))
        for ch in range(8):
            q_ = sq[ch % 2]
            S.do("act", [Cb[ch]], [q_.b], "activation", out=q_[:, 0:n], in_=C[:, ch, g0:g1], func=AF.Square)
            S.do("pe", [ones.b, q_.b], [qps.b], "matmul", qps[:, 0:n], ones[:], q_[:, 0:n], start=(ch == 0), stop=(ch == 7))
        S.do("dve", [sps.b], [mean.b], "tensor_scalar", mean[:, 0:n], sps[:, 0:n], 1.0 / 1024, None, ALU.mult)
        S.do("dve", [mean.b], [msq.b], "tensor_tensor", msq[:, 0:n], mean[:, 0:n], mean[:, 0:n], ALU.mult)
        S.do("dve", [qps.b, msq.b], [rstd.b], "scalar_tensor_tensor", rstd[:, 0:n], qps[:, 0:n], 1.0 / 1024, msq[:, 0:n], ALU.mult, ALU.subtract)
        S.do("dve", [rstd.b], [rstd.b], "tensor_scalar", rstd[:, 0:n], rstd[:, 0:n], EPS, None, ALU.add)
        S.do("act", [rstd.b], [rstd.b], "activation", out=rstd[:, 0:n], in_=rstd[:, 0:n], func=AF.Sqrt)
        S.do("dve", [rstd.b], [rstd.b], "reciprocal", rstd[:, 0:n], rstd[:, 0:n])
        for ch in range(8):
            t_ = tt_[ch % 2]
            S.do("dve", [Cb[ch], mean.b], [t_.b], "tensor_tensor", t_[:, 0:n], C[:, ch, g0:g1], mean[:, 0:n], ALU.subtract)
            S.do("dve", [t_.b, rstd.b], [t_.b], "tensor_tensor", t_[:, 0:n], t_[:, 0:n], rstd[:, 0:n], ALU.mult)
            S.do("act", [t_.b, cvp.b], [cvTb], "activation", out=cvT[:, ch, g0:g1], in_=t_[:, 0:n], func=AF.Silu,
                 scale=cvp[:, ch, 32:33], bias=cvp[:, ch, 33:34])
    P.pop()

    P.push()
    aT = P.sb("aT", [128, 8, NT1], BF16); hyT = P.sb("hyT", [128, 8, NT1], BF16)
    S.dma("sp", aT[:], aTd.rearrange("(k p) t -> p k t", p=128), writes=[aT.b])
    S.dma("pool", hyT[:], hyd.rearrange("(k p) t -> p k t", p=128), writes=[hyT.b])
    acts = [(aT, aT.b), (hyT, hyT.b), (cvT, cvTb)]
    wbs = [[P.sb("wb", [128, 8, 512], BF16) for _ in range(3)] for _ in range(2)]
    gbs = [[P.sb("gb", [128, NT1]) for _ in range(3)] for _ in range(2)]
    pacc = [P.ps("pacc") for _ in range(6)]
    m1 = P.sb("m1", [128, 512]); m2 = P.sb("m2", [128, 512])
    npa = 0
    for cb in range(4):
        wset = wbs[cb % 2]
        for b in range(3):
            S.dma("pool", wset[b][:], wbr[b].rearrange("(k p) c -> p k c", p=128)[:, :, cb * 512:(cb + 1) * 512], writes=[wset[b].b])
        for c4 in range(4):
            j = cb * 4 + c4
            gset = gbs[j % 2]
            for b in range(3):
                S.dma("sp", gset[b][:], gTd[(b * 16 + j) * 128:(b * 16 + j + 1) * 128, :], writes=[gset[b].b])
            for g, (g0, g1) in enumerate(GROUPS1):
                n = g1 - g0
                pp = []
                for b in range(3):
                    pa_ = pacc[npa % 6]; npa += 1
                    at_, ab_ = acts[b]
                    for k in range(8):
                        S.do("pe", [wset[b].b, ab_], [pa_.b], "matmul", pa_[:, 0:n], wset[b][:, k, c4 * 128:(c4 + 1) * 128], at_[:, k, g0:g1],
                             start=(k == 0), stop=(k == 7))
                    pp.append(pa_)
                S.do("dve", [pp[0].b, gset[0].b], [m1.b], "tensor_tensor", m1[:, 0:n], pp[0][:, 0:n], gset[0][:, g0:g1], ALU.mult)
                S.do("dve", [pp[1].b, gset[1].b], [m2.b], "tensor_tensor", m2[:, 0:n], pp[1][:, 0:n], gset[1][:, g0:g1], ALU.mult)
                S.do("dve", [m1.b, m2.b], [m1.b], "tensor_tensor", m1[:, 0:n], m1[:, 0:n], m2[:, 0:n], ALU.add)
                S.do("dve", [pp[2].b, gset[2].b], [m2.b], "tensor_tensor", m2[:, 0:n], pp[2][:, 0:n], gset[2][:, g0:g1], ALU.mult)
                S.do("dve", [m1.b, m2.b], [ypTb[g]], "tensor_tensor", ypT[:, j, g0:g1], m1[:, 0:n], m2[:, 0:n], ALU.add)
    P.pop()
    P.pop()

    P.push()
    wout = P.sb("wout", [128, 16, D], BF16)
    woutb = [Buf("wout%d" % i) for i in range(4)]
    for cb in range(4):
        S.dma("pool", wout[:, :, cb * 512:(cb + 1) * 512], woutd.rearrange("(k p) c -> p k c", p=128)[:, :, cb * 512:(cb + 1) * 512], writes=[woutb[cb]])
    g1r = [P.sb("g1r", [128, D]) for _ in range(2)]
    ng1r = P.sb("ng1r", [128, D])
    S.dma("sp", ng1r[:], ng1rd.partition_broadcast(128), writes=[ng1r.b])
    for j in range(2):
        S.dma("sp", g1r[j][:], g1rd[j:j + 1, :].partition_broadcast(128), writes=[g1r[j].b])
        S.do("dve", [g1r[j].b, ng1r.b], [g1r[j].b], "tensor_tensor", g1r[j][:], g1r[j][:], ng1r[:], ALU.mult)
    wr = P.sb("wr", [128, 16, 32]); br = P.sb("br", [128, 32])
    S.dma("sp", wr[:], wrd.rearrange("(k p) e -> p k e", p=128), writes=[wr.b])
    S.dma("sp", br[:], brd.partition_broadcast(128), writes=[br.b])
    accs = [P.ps("acc") for _ in range(4)]
    tps = [P.ps("tp") for _ in range(2)]
    lps = P.ps("lps", [128, 32])
    xts = [P.sb("xt", [128, D]) for _ in range(2)]
    x1ts = [P.sb("x1t", [128, D]) for _ in range(2)]
    xn = P.sb("xn", [128, D]); junk = xn
    ssp = P.sb("ssp", [128, 4]); ss = P.sb("ss", [128, 1]); rstd1 = P.sb("rstd1", [128, 1]); rstd2 = P.sb("rstd2", [128, 1])
    t5 = P.sb("t5", [128, 512])
    f32T = [P.sb("f32T", [128, 16, 128]) for _ in range(2)]
    fbT = [P.sb("fbT", [128, 16, 128], BF16) for _ in range(2)]
    lg = P.sb("lg", [128, 32]); top8 = P.sb("top8", [128, 8]); msk = P.sb("msk", [128, 32]); ex = P.sb("ex", [128, 32])
    nmx = P.sb("nmx", [128, 1]); sm = P.sb("sm", [128, 1]); wgt = [P.sb("wgt", [128, 32]) for _ in range(2)]
    fTv = fTo.rearrange("(k p) t -> p k t", p=128)
    ntp = 0
    for tt in range(NT1 // 128):
        j = 1 if tt < 2 else 0
        g = 0 if tt < 2 else (1 if tt < 6 else 2)
        xt = xts[tt % 2]; x1t = x1ts[tt % 2]
        S.dma("sp", xt[:], xind[tt * 128:(tt + 1) * 128, :], writes=[xt.b])
        for cb in range(4):
            for k in range(16):
                S.do("pe", [ypTb[g], woutb[cb]], [accs[cb].b], "matmul", accs[cb][:], ypT[:, k, tt * 128:(tt + 1) * 128], wout[:, k, cb * 512:(cb + 1) * 512],
                     start=(k == 0), stop=(k == 15))
            S.do("act", [accs[cb].b], [junk.b, ssp.b], "activation", out=junk[:, 0:512], in_=accs[cb][:], func=AF.Square, accum_out=ssp[:, cb:cb + 1])
        S.do("dve", [ssp.b], [ss.b], "tensor_reduce", ss[:], ssp[:], AX.X, ALU.add)
        S.do("dve", [ss.b], [rstd1.b], "tensor_scalar", rstd1[:], ss[:], 1.0 / D, EPS, ALU.mult, ALU.add)
        S.do("act", [rstd1.b], [rstd1.b], "activation", out=rstd1[:], in_=rstd1[:], func=AF.Sqrt)
        S.do("dve", [rstd1.b], [rstd1.b], "reciprocal", rstd1[:], rstd1[:])
        for cb in range(4):
            S.do("dve", [accs[cb].b, rstd1.b, g1r[j].b], [t5.b], "scalar_tensor_tensor", t5[:], accs[cb][:], rstd1[:, 0:1], g1r[j][:, cb * 512:(cb + 1) * 512], ALU.mult, ALU.mult)
            S.do("dve", [t5.b, xt.b], [x1t.b], "tensor_tensor", x1t[:, cb * 512:(cb + 1) * 512], t5[:], xt[:, cb * 512:(cb + 1) * 512], ALU.add)
        P.outs.append(S.dma("sp", x1o[tt * 128:(tt + 1) * 128, :], x1t[:], reads=[x1t.b]))
        S.do("act", [x1t.b], [junk.b, ss.b], "activation", out=junk[:], in_=x1t[:], func=AF.Square, accum_out=ss[:])
        S.do("dve", [ss.b], [rstd2.b], "tensor_scalar", rstd2[:], ss[:], 1.0 / D, EPS, ALU.mult, ALU.add)
        S.do("act", [rstd2.b], [rstd2.b], "activation", out=rstd2[:], in_=rstd2[:], func=AF.Sqrt)
        S.do("dve", [rstd2.b], [rstd2.b], "reciprocal", rstd2[:], rstd2[:])
        S.do("act", [x1t.b, rstd2.b], [xn.b], "activation", out=xn[:], in_=x1t[:], func=AF.Copy, scale=rstd2[:, 0:1])
        f32 = f32T[tt % 2]; fb = fbT[tt % 2]
        for k4 in range(4):
            tp = tps[ntp % 2]; ntp += 1
            for kk in range(4):
                k = k4 * 4 + kk
                S.do("pe", [xn.b, ident.b], [tp.b], "transpose", tp[:, kk * 128:(kk + 1) * 128], xn[:, k * 128:(k + 1) * 128], ident[:])
            for kk in range(4):
                k = k4 * 4 + kk
                S.do("dve", [tp.b, s2.b, sh2.b], [f32.b], "tensor_scalar", f32[:, k, :], tp[:, kk * 128:(kk + 1) * 128],
                     s2[:, j * 16 + k:j * 16 + k + 1], sh2[:, j * 16 + k:j * 16 + k + 1], ALU.mult, ALU.add)
        S.do("act", [f32.b], [fb.b], "activation", out=fb[:], in_=f32[:], func=AF.Copy)
        P.outs.append(S.dma("sp", fTv[:, :, tt * 128:(tt + 1) * 128], fb[:], reads=[fb.b]))
        for k in range(16):
            S.do("pe", [f32.b, wr.b], [lps.b], "matmul", lps[:], f32[:, k, :], wr[:, k, :], start=(k == 0), stop=(k == 15))
        S.do("dve", [lps.b, br.b], [lg.b], "tensor_tensor", lg[:], lps[:], br[:], ALU.add)
        S.do("dve", [lg.b], [top8.b], "max", top8[:], lg[:])
        S.do("dve", [lg.b, top8.b], [msk.b], "tensor_scalar", msk[:], lg[:], top8[:, 3:4], None, ALU.is_ge)
        S.do("dve", [top8.b], [nmx.b], "tensor_scalar", nmx[:], top8[:, 0:1], -1.0, None, ALU.mult)
        S.do("act", [lg.b, nmx.b], [ex.b], "activation", out=ex[:], in_=lg[:], func=AF.Exp, bias=nmx[:, 0:1])
        S.do("dve", [ex.b, msk.b], [ex.b], "tensor_tensor", ex[:], ex[:], msk[:], ALU.mult)
        S.do("dve", [ex.b], [sm.b], "tensor_reduce", sm[:], ex[:], AX.X, ALU.add)
        S.do("dve", [sm.b], [sm.b], "reciprocal", sm[:], sm[:])
        w_ = wgt[tt % 2]
        S.do("dve", [ex.b, sm.b], [w_.b], "tensor_scalar", w_[:], ex[:], sm[:, 0:1], None, ALU.mult)
        P.outs.append(S.dma("sp", wgo[tt * 128:(tt + 1) * 128, :], w_[:], reads=[w_.b]))
    P.pop()
    return P.finish()


def mod_rows(modT_l):
    return np.ascontiguousarray(modT_l.transpose(2, 1, 0).reshape(2, 12288))


def run_k4(inp, l, x, ctx, modT, rest_full, aT_full, hyT_full):
    identF = np.eye(128, dtype=np.float32)
    onesF = np.ones((128, 128), np.float32)
    rows = mod_rows(modT[l])
    g1row = np.ascontiguousarray(rows[:, 2 * D:3 * D])
    cvp = np.zeros((128, 8, 34), np.float32)
    cvp[:, :, 0:31] = inp["cv_dw_w"][l].reshape(31, 8, 128).transpose(2, 1, 0)
    cvp[:, :, 31] = fm(inp["cv_dw_b"][l], 8)
    cvp[:, :, 32] = fm(inp["cv_ln_g"][l], 8)
    cvp[:, :, 33] = fm(inp["cv_ln_b"][l], 8)
    cvall = rest_full[3072:5120]
    z15 = np.zeros((2048, 15), np.float32)
    in_maps = []
    for i in range(NCORES):
        t0 = LC + i * 1024
        left = cvall[:, t0 - 15:t0] if i > 0 else z15
        right = cvall[:, t0 + 1024:t0 + 1039] if i < NCORES - 1 else z15
        cvin = np.concatenate([z15, cvall[:, :LC], z15, left, cvall[:, t0:t0 + 1024], right], axis=1)
        sel = lambda a: np.ascontiguousarray(np.concatenate([a[:, :LC], a[:, t0:t0 + 1024]], axis=1))
        in_maps.append({
            "gT": sel(rest_full[5120:]), "cvin": np.ascontiguousarray(cvin),
            "aT": sel(aT_full), "hyT": sel(hyT_full),
            "xin": np.ascontiguousarray(np.concatenate([ctx, x[i * 1024:(i + 1) * 1024]], axis=0)),
            "w_da": inp["w_da_out"][l], "w_hy": inp["w_hy_out"][l], "w_cv": inp["w_cv_out"][l], "w_out": inp["w_out"][l],
            "w_r": inp["w_router"][l], "b_r": np.ascontiguousarray(inp["b_router"][l].reshape(1, 32)),
            "cvp": np.ascontiguousarray(cvp.reshape(128, 8 * 34)),
            "modT": np.ascontiguousarray(modT[l].reshape(128, 192)), "ng2T": fm(inp["norm_g"][l, 2], 16),
            "g1row": g1row, "ng1row": np.ascontiguousarray(inp["norm_g"][l, 1].reshape(1, D)),
            "identF": identF, "onesF": onesF,
        })
    return run(build_k4(), in_maps)


EPC = NE // NCORES
SW_LIMIT = 7.0
SW_ALPHA = 1.702


def build_k5():
    P = Prog()
    S = P.S
    fTd = P.din("fT", [D, NTOK], BF16)
    wg4d = P.din("wg4", [128, NKT * EPC])
    wgTd = P.din("wgT4", [EPC, NTOK])
    wgud = P.din("w_gu", [EPC, D, 2 * DE]); bgud = P.din("b_guT", [128, EPC * 16])
    wdnd = P.din("w_dn", [EPC, DE, D]); bdnd = P.din("b_dn", [EPC, D])
    part = P.dout("part", [NTOK, D])

    wg4 = P.sb("wg4", [128, NKT * EPC]); bgu = P.sb("bgu", [128, EPC * 16]); bdn = P.sb("bdn", [EPC, D])
    S.dma("sp", wg4[:], wg4d, writes=[wg4.b]); S.dma("sp", bgu[:], bgud, writes=[bgu.b]); S.dma("sp", bdn[:], bdnd, writes=[bdn.b])

    gus = [P.dscratch("gus%d" % e, [128, 16 * 2 * DE], BF16) for e in range(EPC)]
    dns = [P.dscratch("dns%d" % e, [128, 8 * D], BF16) for e in range(EPC)]
    for t_ in gus + dns:
        t_.b.multi = True
    P.push()
    st = [P.sb("st", [128, 16, 512], BF16) for _ in range(3)]
    ns = 0
    for e in range(EPC):
        guv0 = gus[e].t.rearrange("p (k c) -> p k c", k=16)
        dnv0 = dns[e].t.rearrange("p (k c) -> p k c", k=8)
        for cb in range(4):
            s_ = st[ns % 3]; ns += 1
            S.dma("pool", s_[:], wgud[e].rearrange("(k p) c -> p k c", p=128)[:, :, cb * 512:(cb + 1) * 512], writes=[s_.b])
            S.dma("sp", guv0[:, :, cb * 512:(cb + 1) * 512], s_[:], reads=[s_.b], writes=[gus[e].b])
        for cb in range(4):
            s_ = st[ns % 3]; ns += 1
            S.dma("pool", s_[:, 0:8, :], wdnd[e].rearrange("(k p) c -> p k c", p=128)[:, :, cb * 512:(cb + 1) * 512], writes=[s_.b])
            S.dma("sp", dnv0[:, :, cb * 512:(cb + 1) * 512], s_[:, 0:8, :], reads=[s_.b], writes=[dns[e].b])
    P.pop()

    fTv = fTd.rearrange("(k p) t -> p k t", p=128)
    fgs = [P.sb("fg", [128, 16, 512], BF16) for _ in range(2)]
    wgts = [P.sb("wgt", [EPC, 512]) for _ in range(2)]
    acc = P.sb("acc", [128, 4, D])
    accb = [[Buf("acc%d_%d" % (t, cb)) for cb in range(4)] for t in range(4)]
    gs = P.sb("gs", [128, 8, 512]); gsb = [Buf("gs%d" % j) for j in range(8)]
    actT = P.sb("actT", [128, 8, 512], BF16); actb = Buf("actT", multi=True)
    wgu_t = [P.sb("wgu", [128, 16, 512], BF16) for _ in range(3)]
    wdn_t = [P.sb("wdn", [128, 8, 512], BF16) for _ in range(3)]
    t1s = [P.sb("t1", [128, 512]) for _ in range(2)]
    sgs = [P.sb("sg", [128, 512]) for _ in range(2)]
    pg = [P.ps("pg") for _ in range(3)]
    pd = [P.ps("pd") for _ in range(3)]
    pb = P.ps("pb")
    npg = 0; npd = 0; nwu = 0; nwd = 0; nt1 = 0
    groups = [(t0, min(t0 + 512, NTOK)) for t0 in range(0, NTOK, 512)]
    for gi, (t0, t1) in enumerate(groups):
        n = t1 - t0
        ntile = n // 128
        fg = fgs[gi % 2]; wgt = wgts[gi % 2]
        S.dma("sp", fg[:, :, 0:n], fTv[:, :, t0:t1], writes=[fg.b])
        S.dma("sp", wgt[:, 0:n], wgTd[:, t0:t1], writes=[wgt.b])
        for tl in range(ntile):
            for cb in range(4):
                S.do("pe", [wgt.b, bdn.b], [pb.b], "matmul", pb[:], wgt[:, tl * 128:(tl + 1) * 128], bdn[:, cb * 512:(cb + 1) * 512], start=True, stop=True)
                S.do("act", [pb.b], [accb[tl][cb]], "activation", out=acc[:, tl, cb * 512:(cb + 1) * 512], in_=pb[:], func=AF.Copy)
        for e in range(EPC):
            guv = gus[e].t.rearrange("p (k c) -> p k c", k=16)
            dnv = dns[e].t.rearrange("p (k c) -> p k c", k=8)
            for cb in range(4):
                W = wgu_t[nwu % 3]; nwu += 1
                S.dma("sp", W[:], guv[:, :, cb * 512:(cb + 1) * 512], reads=[gus[e].b], writes=[W.b])
                for c4 in range(4):
                    ch = cb * 4 + c4
                    ps = pg[npg % 3]; npg += 1
                    for k in range(16):
                        S.do("pe", [W.b, fg.b], [ps.b], "matmul", ps[:, 0:n], W[:, k, c4 * 128:(c4 + 1) * 128], fg[:, k, 0:n], start=(k == 0), stop=(k == 15))
                    bcol = bgu[:, e * 16 + ch:e * 16 + ch + 1]
                    t_ = t1s[nt1 % 2]; sg = sgs[nt1 % 2]; nt1 += 1
                    if ch < 8:
                        S.do("dve", [ps.b, bgu.b], [t_.b], "tensor_scalar", t_[:, 0:n], ps[:, 0:n], bcol, SW_LIMIT, ALU.add, ALU.min)
                        S.do("act", [t_.b], [sg.b], "activation", out=sg[:, 0:n], in_=t_[:, 0:n], func=AF.Sigmoid, scale=SW_ALPHA)
                        S.do("dve", [t_.b, sg.b], [gsb[ch]], "tensor_tensor", gs[:, ch, 0:n], t_[:, 0:n], sg[:, 0:n], ALU.mult)
                    else:
                        j = ch - 8
                        S.do("dve", [ps.b, bgu.b], [t_.b], "tensor_scalar", t_[:, 0:n], ps[:, 0:n], bcol, SW_LIMIT, ALU.add, ALU.min)
                        S.do("dve", [t_.b], [t_.b], "tensor_scalar", t_[:, 0:n], t_[:, 0:n], -SW_LIMIT, 1.0, ALU.max, ALU.add)
                        S.do("dve", [t_.b, gsb[j]], [actb], "tensor_tensor", actT[:, j, 0:n], gs[:, j, 0:n], t_[:, 0:n], ALU.mult)
            for cb in range(4):
                Wd = wdn_t[nwd % 3]; nwd += 1
                S.dma("sp", Wd[:], dnv[:, :, cb * 512:(cb + 1) * 512], reads=[dns[e].b], writes=[Wd.b])
                for tl in range(ntile):
                    ps = pd[npd % 3]; npd += 1
                    for k in range(8):
                        S.do("pe", [actb, Wd.b], [ps.b], "matmul", ps[:], actT[:, k, tl * 128:(tl + 1) * 128], Wd[:, k, :], start=(k == 0), stop=(k == 7))
                    tg = (t0 // 128) + tl
                    a_ = acc[:, tl, cb * 512:(cb + 1) * 512]
                    S.do("dve", [ps.b, wg4.b, accb[tl][cb]], [accb[tl][cb]], "scalar_tensor_tensor", a_, ps[:], wg4[:, tg * EPC + e:tg * EPC + e + 1], a_, ALU.mult, ALU.add)
        rd = [accb[tl][cb] for tl in range(ntile) for cb in range(4)]
        P.outs.append(S.dma("sp", part[t0:t1, :].rearrange("(t p) d -> p t d", p=128), acc[:, 0:ntile, :], reads=rd))
    return P.finish()


def run_k5(inp, l, fT_full, wg_full):
    in_maps = []
    for c in range(NCORES):
        es = slice(c * EPC, (c + 1) * EPC)
        wg = wg_full[:, es]
        wg4 = wg.reshape(NKT, 128, EPC).transpose(1, 0, 2).reshape(128, NKT * EPC)
        bgu = inp["b_gu"][l][es].reshape(EPC, 16, 128).transpose(2, 0, 1).reshape(128, EPC * 16)
        in_maps.append({
            "fT": fT_full, "wg4": np.ascontiguousarray(wg4), "wgT4": np.ascontiguousarray(wg.T),
            "w_gu": inp["w_gu"][l][es], "b_guT": np.ascontiguousarray(bgu),
            "w_dn": inp["w_dn"][l][es], "b_dn": np.ascontiguousarray(inp["b_dn"][l][es]),
        })
    res = run(build_k5(), in_maps)
    return [res[c]["part"] for c in range(NCORES)]


NT6 = 1024 + 32


def build_k6():
    P = Prog()
    S = P.S
    pd_ = P.din("parts", [NCORES, NT6, D])
    x1d = P.din("x1", [NT6, D])
    g2rd = P.din("g2row", [2, D]); ng3rd = P.din("ng3row", [1, D])
    out = P.dout("x2", [NT6, D])
    g2r = [P.sb("g2r", [128, D]) for _ in range(2)]
    ng3r = P.sb("ng3r", [128, D])
    S.dma("sp", ng3r[:], ng3rd.partition_broadcast(128), writes=[ng3r.b])
    for j in range(2):
        S.dma("sp", g2r[j][:], g2rd[j:j + 1, :].partition_broadcast(128), writes=[g2r[j].b])
        S.do("dve", [g2r[j].b, ng3r.b], [g2r[j].b], "tensor_tensor", g2r[j][:], g2r[j][:], ng3r[:], ALU.mult)
    pts = [P.sb("pt", [128, D]) for _ in range(4)]
    fs = [P.sb("f", [128, D]) for _ in range(2)]
    xts = [P.sb("xt", [128, D]) for _ in range(2)]
    junk = P.sb("junk", [128, D])
    ss = P.sb("ss", [128, 1]); rstd = P.sb("rstd", [128, 1])
    npt = 0
    tiles = [(t * 128, 128, 0) for t in range(8)] + [(1024, 32, 1)]
    for ti, (r0, nr, j) in enumerate(tiles):
        f = fs[ti % 2]; xt = xts[ti % 2]
        S.dma("sp", f[0:nr, :], pd_[0, r0:r0 + nr, :], writes=[f.b])
        S.dma("sp", xt[0:nr, :], x1d[r0:r0 + nr, :], writes=[xt.b])
        for c in range(1, NCORES):
            pt = pts[npt % 4]; npt += 1
            S.dma("sp", pt[0:nr, :], pd_[c, r0:r0 + nr, :], writes=[pt.b])
            eng = "dve" if c % 2 else "pool"
            S.do(eng, [f.b, pt.b], [f.b], "tensor_tensor", f[0:nr, :], f[0:nr, :], pt[0:nr, :], ALU.add)
        S.do("act", [f.b], [junk.b, ss.b], "activation", out=junk[0:nr, :], in_=f[0:nr, :], func=AF.Square, accum_out=ss[0:nr, :])
        S.do("dve", [ss.b], [rstd.b], "tensor_scalar", rstd[0:nr, :], ss[0:nr, :], 1.0 / D, EPS, ALU.mult, ALU.add)
        S.do("act", [rstd.b], [rstd.b], "activation", out=rstd[0:nr, :], in_=rstd[0:nr, :], func=AF.Sqrt)
        S.do("dve", [rstd.b], [rstd.b], "reciprocal", rstd[0:nr, :], rstd[0:nr, :])
        S.do("dve", [f.b, rstd.b, g2r[j].b], [f.b], "scalar_tensor_tensor", f[0:nr, :], f[0:nr, :], rstd[0:nr, 0:1], g2r[j][0:nr, :], ALU.mult, ALU.mult)
        S.do("dve", [f.b, xt.b], [xt.b], "tensor_tensor", xt[0:nr, :], xt[0:nr, :], f[0:nr, :], ALU.add)
        P.outs.append(S.dma("sp", out[r0:r0 + nr, :], xt[0:nr, :], reads=[xt.b]))
    return P.finish()


def run_k6(inp, l, parts, x1_lat, x1_ctx, modT):
    rows = mod_rows(modT[l])
    g2row = np.ascontiguousarray(rows[:, 5 * D:6 * D])
    ng3row = np.ascontiguousarray(inp["norm_g"][l, 3].reshape(1, D))
    in_maps = []
    for i in range(NCORES):
        sel = lambda a_lat, a_ctx: np.concatenate([a_lat[i * 1024:(i + 1) * 1024], a_ctx[i * 32:(i + 1) * 32]], axis=0)
        pp = np.stack([sel(p[LC:], p[:LC]) for p in parts], axis=0)
        in_maps.append({"parts": np.ascontiguousarray(pp), "x1": np.ascontiguousarray(sel(x1_lat, x1_ctx)),
                        "g2row": g2row, "ng3row": ng3row})
    res = run(build_k6(), in_maps)
    x2 = np.concatenate([res[i]["x2"][:1024] for i in range(NCORES)], axis=0)
    ctx2 = np.concatenate([res[i]["x2"][1024:] for i in range(NCORES)], axis=0)
    return x2, ctx2


def kernel(**inputs):
    inp = {k: np.asarray(v) for k, v in inputs.items()}
    x = np.ascontiguousarray(inp["x"][0])
    ctx = np.ascontiguousarray(inp["ctx"][0])
    modT = run_k0(inp)
    for l in range(DEPTH):
        r1 = run_k1(inp, l, x, ctx, modT)
        aT = run_k2(inp, l, r1)
        rest_full = np.concatenate([r1[0]["restT"][:, :LC]] + [r1[i]["restT"][:, LC:] for i in range(NCORES)], axis=1)
        del r1
        hyT = run_k3(inp, l, rest_full)
        r4 = run_k4(inp, l, x, ctx, modT, rest_full, aT, hyT)
        del rest_full
        fT = np.ascontiguousarray(np.concatenate([r4[0]["fT"][:, :LC]] + [r4[i]["fT"][:, LC:] for i in range(NCORES)], axis=1))
        wg = np.concatenate([r4[0]["wg"][:LC]] + [r4[i]["wg"][LC:] for i in range(NCORES)], axis=0)
        x1_lat = np.concatenate([r4[i]["x1"][LC:] for i in range(NCORES)], axis=0)
        x1_ctx = r4[0]["x1"][:LC]
        del r4
        parts = run_k5(inp, l, fT, wg)
        x, ctx = run_k6(inp, l, parts, x1_lat, x1_ctx, modT)
        del parts
    return np.ascontiguousarray(x[None].astype(np.float32))
```
